# Optimizing a Trainium2 kernel written in Bass

```python
import jax, jax.numpy as jnp
from jax import lax
import numpy as np

D_MODEL = 1024
BATCH = 4
SEQ = 8192
DEPTH = 2

CHUNK = 64
D_MIX = D_MODEL
D_MLSTM = D_MIX // 2
N_MLSTM_HEADS = 4
DH_MLSTM = D_MLSTM // N_MLSTM_HEADS
D_LRU = D_MIX - D_MLSTM
N_LRU_BLOCKS = 8
DB_LRU = D_LRU // N_LRU_BLOCKS
CONV_WIDTH = 4
LRU_C = 8.0
D_FF = 2816
N_EXPERTS = 8
TOP_K = 2
N_DENSE = (DEPTH + 1) // 2
N_MOE = DEPTH // 2
EPS = 1e-6
SPLITS = (D_MLSTM, 2 * D_MLSTM, 3 * D_MLSTM, 4 * D_MLSTM,
          4 * D_MLSTM + N_MLSTM_HEADS, 4 * D_MLSTM + 2 * N_MLSTM_HEADS,
          4 * D_MLSTM + 2 * N_MLSTM_HEADS + D_LRU)
D_IN = 4 * D_MLSTM + 2 * N_MLSTM_HEADS + 2 * D_LRU

kernel_name = 'hybrid_mlstm_rglru_moe_trunk'


def _rmsnorm(x, g):
    xf = x.astype(jnp.float32)
    xf = xf * lax.rsqrt(jnp.mean(xf * xf, axis=-1, keepdims=True) + EPS)
    return xf.astype(x.dtype) * g


def _causal_conv(x, w, b):
    S = x.shape[1]
    W = w.shape[0]
    xp = jnp.pad(x, ((0, 0), (W - 1, 0), (0, 0)))
    y = b
    for j in range(W):
        y = y + xp[:, j:j + S, :] * w[j]
    return y


def _mlstm_chunkwise(q, k, v, i_pre, f_pre):
    B, S, H, Dh = q.shape
    NC = S // CHUNK
    f32 = jnp.float32

    def to_chunks(t):
        t = t.astype(f32).reshape((B, NC, CHUNK, H) + t.shape[3:])
        return jnp.moveaxis(t, 3, 1)

    q = to_chunks(q)
    k = to_chunks(k) * (Dh ** -0.5)
    v = to_chunks(v)
    ig = to_chunks(i_pre)
    b = jnp.cumsum(jax.nn.log_sigmoid(to_chunks(f_pre)), axis=-1)
    b_last = b[..., -1]

    w_end = b_last[..., None] - b + ig
    m_loc = jnp.max(w_end, axis=-1)
    e_end = jnp.exp(w_end - m_loc[..., None])
    C_loc = jnp.einsum('bhcsd,bhcse->bhcde', v * e_end[..., None], k)
    n_loc = jnp.einsum('bhcs,bhcse->bhce', e_end, k)

    def step(carry, inp):
        C, n, m = carry
        Cl, nl, ml, bl = inp
        m_new = jnp.maximum(bl + m, ml)
        a = jnp.exp(bl + m - m_new)
        g = jnp.exp(ml - m_new)
        C_new = a[..., None, None] * C + g[..., None, None] * Cl
        n_new = a[..., None] * n + g[..., None] * nl
        return (C_new, n_new, m_new), (C, n, m)

    init = (jnp.zeros((B, H, Dh, Dh), f32), jnp.zeros((B, H, Dh), f32), jnp.zeros((B, H), f32))
    xs = (jnp.moveaxis(C_loc, 2, 0), jnp.moveaxis(n_loc, 2, 0),
          jnp.moveaxis(m_loc, 2, 0), jnp.moveaxis(b_last, 2, 0))
    _, (C_prev, n_prev, m_prev) = lax.scan(step, init, xs)
    C_prev = jnp.moveaxis(C_prev, 0, 2)
    n_prev = jnp.moveaxis(n_prev, 0, 2)
    m_prev = jnp.moveaxis(m_prev, 0, 2)

    causal = jnp.tril(jnp.ones((CHUNK, CHUNK), dtype=bool))
    Dmat = jnp.where(causal, b[..., :, None] - b[..., None, :] + ig[..., None, :], -jnp.inf)
    inter_log = b + m_prev[..., None]
    m_t = jnp.maximum(inter_log, jnp.max(Dmat, axis=-1))
    scores = jnp.einsum('bhctd,bhcsd->bhcts', q, k) * jnp.exp(Dmat - m_t[..., None])
    inter_scale = jnp.exp(inter_log - m_t)
    num = jnp.einsum('bhcts,bhcsd->bhctd', scores, v) \
        + inter_scale[..., None] * jnp.einsum('bhcde,bhcte->bhctd', C_prev, q)
    den = jnp.sum(scores, axis=-1) + inter_scale * jnp.einsum('bhce,bhcte->bhct', n_prev, q)
    h = num / jnp.maximum(jnp.abs(den), jnp.exp(-m_t))[..., None]
    return jnp.moveaxis(h, 1, 3).reshape(B, S, H * Dh)


def _rglru(xl, gl, w_conv, b_conv, w_a, b_a, w_x, b_x, lam):
    B, S, _ = xl.shape
    f32 = jnp.float32
    xc = _causal_conv(xl, w_conv, b_conv)
    xb = xc.reshape(B, S, N_LRU_BLOCKS, DB_LRU)
    r = jax.nn.sigmoid((jnp.einsum('bsnd,nde->bsne', xb, w_a).reshape(B, S, D_LRU) + b_a).astype(f32))
    i = jax.nn.sigmoid((jnp.einsum('bsnd,nde->bsne', xb, w_x).reshape(B, S, D_LRU) + b_x).astype(f32))
    log_a = -LRU_C * r * jax.nn.softplus(-lam.astype(f32))
    a = jnp.exp(log_a)
    u = jnp.sqrt(-jnp.expm1(2.0 * log_a)) * (i * xc.astype(f32))

    def combine(left, right):
        a1, b1 = left
        a2, b2 = right
        return a1 * a2, a2 * b1 + b2

    _, hs = lax.associative_scan(combine, (a, u), axis=1)
    return hs.astype(xl.dtype) * jax.nn.gelu(gl)


def _hybrid_mixer(h, w_in, w_conv_qk, b_conv_qk, b_gates, w_conv_lru, b_conv_lru,
                  w_lru_a, b_lru_a, w_lru_x, b_lru_x, lru_lambda, g_mix_out, w_out):
    B, S, _ = h.shape
    z = h @ w_in
    q, k, v, o, ig, fg, xl, gl = jnp.split(z, SPLITS, axis=-1)
    qk = jax.nn.silu(_causal_conv(jnp.concatenate([q, k], axis=-1), w_conv_qk, b_conv_qk))
    q, k = jnp.split(qk, 2, axis=-1)
    gates = jnp.concatenate([ig, fg], axis=-1) + b_gates
    ig, fg = jnp.split(gates, 2, axis=-1)
    shp = (B, S, N_MLSTM_HEADS, DH_MLSTM)
    hm = _mlstm_chunkwise(q.reshape(shp), k.reshape(shp), v.reshape(shp), ig, fg).astype(h.dtype)
    hm = _rmsnorm(hm.reshape(shp), g_mix_out[:D_MLSTM].reshape(N_MLSTM_HEADS, DH_MLSTM)).reshape(B, S, D_MLSTM)
    hm = hm * jax.nn.sigmoid(o)
    hl = _rglru(xl, gl, w_conv_lru, b_conv_lru, w_lru_a, b_lru_a, w_lru_x, b_lru_x, lru_lambda)
    hl = _rmsnorm(hl, g_mix_out[D_MLSTM:])
    return jnp.concatenate([hm, hl], axis=-1) @ w_out


def _swiglu(h, w_gate, w_up, w_down):
    return (jax.nn.silu(h @ w_gate) * (h @ w_up)) @ w_down


def _moe(h, w_router, b_router, w_g, w_u, w_d):
    logits = (h @ w_router + b_router).astype(jnp.float32)
    top_v, top_i = lax.top_k(logits, TOP_K)
    top_p = jax.nn.softmax(top_v, axis=-1)
    gates = jnp.sum(jax.nn.one_hot(top_i, N_EXPERTS, dtype=jnp.float32) * top_p[..., None], axis=-2).astype(h.dtype)
    y = jnp.zeros_like(h)
    for e in range(N_EXPERTS):
        y = y + gates[..., e:e + 1] * _swiglu(h, w_g[e], w_u[e], w_d[e])
    return y


def setup_inputs(seed: int = 0) -> dict:
    key = jax.random.key(seed)
    ks = jax.random.split(key, 32)
    f32 = jnp.float32

    def nrm(k, shape, scale):
        return jax.random.normal(k, shape, f32) * scale

    D, H = D_MODEL, N_MLSTM_HEADS
    a0 = jax.random.uniform(ks[20], (DEPTH, D_LRU), f32, 0.9, 0.999)
    b_in = nrm(ks[21], (DEPTH, H), 0.1)
    b_fg = 3.0 + 3.0 * jax.random.uniform(ks[22], (DEPTH, H), f32)
    return {
        'x': nrm(ks[0], (BATCH, SEQ, D), 1.0),
        'c': nrm(ks[1], (BATCH, D), 1.0),
        'w_ada': nrm(ks[2], (DEPTH, D, 6 * D), 0.02),
        'b_ada': nrm(ks[3], (DEPTH, 6 * D), 0.02),
        'g_norm_mix': 1.0 + nrm(ks[4], (DEPTH, D), 0.02),
        'g_norm_ffn': 1.0 + nrm(ks[5], (DEPTH, D), 0.02),
        'w_in': nrm(ks[6], (DEPTH, D, D_IN), D ** -0.5),
        'w_conv_qk': nrm(ks[7], (DEPTH, CONV_WIDTH, 2 * D_MLSTM), CONV_WIDTH ** -0.5),
        'b_conv_qk': nrm(ks[8], (DEPTH, 2 * D_MLSTM), 0.02),
        'b_gates': jnp.concatenate([b_in, b_fg], axis=-1),
        'w_conv_lru': nrm(ks[9], (DEPTH, CONV_WIDTH, D_LRU), CONV_WIDTH ** -0.5),
        'b_conv_lru': nrm(ks[10], (DEPTH, D_LRU), 0.02),
        'w_lru_a': nrm(ks[11], (DEPTH, N_LRU_BLOCKS, DB_LRU, DB_LRU), DB_LRU ** -0.5),
        'b_lru_a': nrm(ks[12], (DEPTH, D_LRU), 0.02),
        'w_lru_x': nrm(ks[13], (DEPTH, N_LRU_BLOCKS, DB_LRU, DB_LRU), DB_LRU ** -0.5),
        'b_lru_x': nrm(ks[14], (DEPTH, D_LRU), 0.02),
        'lru_lambda': jnp.log(a0) - jnp.log1p(-a0),
        'g_mix_out': 1.0 + nrm(ks[15], (DEPTH, D_MIX), 0.02),
        'w_out': nrm(ks[16], (DEPTH, D_MIX, D), D_MIX ** -0.5),
        'w_ff_gate': nrm(ks[17], (N_DENSE, D, D_FF), D ** -0.5),
        'w_ff_up': nrm(ks[18], (N_DENSE, D, D_FF), D ** -0.5),
        'w_ff_down': nrm(ks[19], (N_DENSE, D_FF, D), D_FF ** -0.5),
        'w_router': nrm(ks[23], (N_MOE, D, N_EXPERTS), D ** -0.5),
        'b_router': nrm(ks[24], (N_MOE, N_EXPERTS), 0.01),
        'w_exp_gate': nrm(ks[25], (N_MOE, N_EXPERTS, D, D_FF), D ** -0.5),
        'w_exp_up': nrm(ks[26], (N_MOE, N_EXPERTS, D, D_FF), D ** -0.5),
        'w_exp_down': nrm(ks[27], (N_MOE, N_EXPERTS, D_FF, D), D_FF ** -0.5),
        'g_final': 1.0 + nrm(ks[28], (D,), 0.02),
    }


def reference(x, c, w_ada, b_ada, g_norm_mix, g_norm_ffn, w_in, w_conv_qk, b_conv_qk, b_gates,
              w_conv_lru, b_conv_lru, w_lru_a, b_lru_a, w_lru_x, b_lru_x, lru_lambda, g_mix_out, w_out,
              w_ff_gate, w_ff_up, w_ff_down, w_router, b_router, w_exp_gate, w_exp_up, w_exp_down, g_final):
    cond = jax.nn.silu(c)
    for l in range(DEPTH):
        mod = cond @ w_ada[l] + b_ada[l]
        sh_m, sc_m, gt_m, sh_f, sc_f, gt_f = jnp.split(mod[:, None, :], 6, axis=-1)
        h = _rmsnorm(x, g_norm_mix[l]) * (1 + sc_m) + sh_m
        x = x + gt_m * _hybrid_mixer(h, w_in[l], w_conv_qk[l], b_conv_qk[l], b_gates[l],
                                      w_conv_lru[l], b_conv_lru[l], w_lru_a[l], b_lru_a[l],
                                      w_lru_x[l], b_lru_x[l], lru_lambda[l], g_mix_out[l], w_out[l])
        h = _rmsnorm(x, g_norm_ffn[l]) * (1 + sc_f) + sh_f
        j = l // 2
        if l % 2 == 0:
            y = _swiglu(h, w_ff_gate[j], w_ff_up[j], w_ff_down[j])
        else:
            y = _moe(h, w_router[j], b_router[j], w_exp_gate[j], w_exp_up[j], w_exp_down[j])
        x = x + gt_f * y
    return _rmsnorm(x, g_final)
```

```python
import contextlib
import math
import numpy as np
import concourse.bass as bass
import concourse.mybir as mybir
from concourse.bass_utils import run_bass_kernel_spmd

F32 = mybir.dt.float32
BF16 = mybir.dt.bfloat16
ALU = mybir.AluOpType
AF = mybir.ActivationFunctionType
AX = mybir.AxisListType

D_MODEL = 1024
D_FF = 2816
NE = 8
EPS = 1e-6
D_IN = 3080
SW = 560
C_Q, C_K, C_V, C_O, C_IG, C_FG, C_XL, C_GL = 0, 512, 1024, 1536, 2048, 2052, 2056, 2568
JGW = 256
NJG = D_FF // JGW

SAME_ENGINE_SYNC = True
EPOCH = 16000


class Tl:
    __slots__ = ("t", "w", "r", "name")

    def __init__(self, t, name):
        self.t = t
        self.w = None
        self.r = {}
        self.name = name

    def __getitem__(self, k):
        return self.t[k]


class Ring:
    def __init__(self, tiles):
        self.tiles = tiles
        self.i = 0

    def next(self):
        t = self.tiles[self.i % len(self.tiles)]
        self.i += 1
        return t


class Prog:
    def __init__(self, nc, es, nslots=40):
        self.nc = nc
        self.es = es
        self.tes = es
        self.eng = {"pe": nc.tensor, "act": nc.scalar, "dve": nc.vector, "pool": nc.gpsimd, "sp": nc.sync}
        self.sems = {}
        self.cnt = {e: 0 for e in self.eng}
        self.seen = {e: {} for e in self.eng}
        self.nslots = nslots
        self.slot_sem = [es.enter_context(nc.semaphore(f"dq{i}")) for i in range(nslots)]
        self.slot_cnt = [0] * nslots
        self.next_slot = 0
        self.uid = 0

    def _esem(self, e, ep):
        k = (e, ep)
        if k not in self.sems:
            self.sems[k] = self.es.enter_context(self.nc.semaphore(f"s_{e}_{ep}"))
        return self.sems[k]

    def sb(self, shape, dtype, name=None):
        self.uid += 1
        name = f"{name or 't'}_{self.uid}"
        return Tl(self.tes.enter_context(self.nc.sbuf_tensor(name, list(shape), dtype)), name)

    def ps(self, shape, dtype, name=None):
        self.uid += 1
        name = f"{name or 'p'}_{self.uid}"
        return Tl(self.tes.enter_context(self.nc.psum_tensor(name, list(shape), dtype)), name)

    def pseudo(self, name):
        return Tl(None, name)

    def _wait(self, e, dep):
        kind, key, val = dep
        k = (kind, key)
        if self.seen[e].get(k, 0) >= val:
            return
        sem = self._esem(key[0], key[1]) if kind == "eng" else self.slot_sem[key]
        self.eng[e].wait_ge(sem, val)
        self.seen[e][k] = val

    def _deps(self, e, reads, writes, force_same=False):
        deps = []
        for t in reads:
            if t.w is not None:
                deps.append(t.w)
        for t in writes:
            if t.w is not None:
                deps.append(t.w)
            for k, v in t.r.items():
                deps.append((k[0], k[1], v))
        for d in deps:
            if d[0] == "eng" and d[1][0] == e and not force_same and (e == "pe" or not SAME_ENGINE_SYNC):
                continue
            self._wait(e, d)

    def _mark(self, me, reads, writes):
        k = (me[0], me[1])
        for t in reads:
            if t.r.get(k, 0) < me[2]:
                t.r[k] = me[2]
        for t in writes:
            t.w = me
            t.r = {}

    def _cur(self, e):
        c = self.cnt[e]
        ep = (c - 1) // EPOCH
        return ("eng", (e, ep), c - ep * EPOCH)

    def op(self, e, fn, reads=(), writes=()):
        self._deps(e, reads, writes)
        ins = fn()
        self.cnt[e] += 1
        me = self._cur(e)
        ins.then_inc(self._esem(e, me[1][1]), 1)
        self._mark(me, reads, writes)
        return me

    def dma(self, q, out, in_, reads=(), writes=(), slow=False):
        slot = self.next_slot
        self.next_slot = (slot + 1) % self.nslots
        if self.slot_cnt[slot] > 0:
            self._wait(q, ("dma", slot, 16 * self.slot_cnt[slot]))
        self._deps(q, reads, writes, force_same=True)
        if slow:
            ins = self.eng[q].dma_start(out=out, in_=in_, allow_slow_non_contiguous=True)
        else:
            ins = self.eng[q].dma_start(out=out, in_=in_)
        self.slot_cnt[slot] += 1
        ins.then_inc(self.slot_sem[slot], 16)
        me = ("dma", slot, 16 * self.slot_cnt[slot])
        self._mark(me, reads, writes)
        return me

    def barrier(self):
        deps = []
        for e in self.eng:
            if self.cnt[e] > 0:
                deps.append(self._cur(e))
        for s in range(self.nslots):
            if self.slot_cnt[s] > 0:
                deps.append(("dma", s, 16 * self.slot_cnt[s]))
        for e in self.eng:
            for d in deps:
                self._wait(e, d)


def vec_layout(NL):
    lay = {}
    off = 0

    def add(name, n):
        nonlocal off
        lay[name] = (off, n)
        off += n

    add("cond", 8)
    for l in range(NL):
        add(f"bada{l}", 48)
        add(f"gm{l}", 8)
        add(f"gf{l}", 8)
        add(f"wcqk{l}", 32)
        add(f"bcqk{l}", 8)
        add(f"wcl{l}", 16)
        add(f"bcl{l}", 4)
        add(f"bla{l}", 4)
        add(f"blx{l}", 4)
        add(f"lam{l}", 4)
        add(f"gml{l}", 4)
        add(f"bg{l}", 2)
    add("gfin", 8)
    return lay, off


def fm(v, nch):
    return np.ascontiguousarray(np.asarray(v, np.float32).reshape(nch, 128).T)


def build(T, NL, moe_layers=(1,)):
    NSUB = T // 512
    TT = min(1024, T)
    NTT = T // TT
    NB = TT // 512
    lay, NV = vec_layout(NL)

    nc = bass.Bass("TRN2", target_bir_lowering=False)
    DT = nc.dram_tensor
    xT = DT("xT", [1024, T], F32, kind="ExternalInput").ap()
    vecs_d = DT("vecs", [128, NV], F32, kind="ExternalInput").ap()
    consts_d = DT("consts", [128, 256 + 1024], F32, kind="ExternalInput").ap()
    gmixb_d = DT("gmixb", [NL, 128, 512], F32, kind="ExternalInput").ap()
    brb_d = DT("brb", [128, 8], F32, kind="ExternalInput").ap()
    w_ada_d = DT("w_ada", [NL, 1024, 6144], F32, kind="ExternalInput").ap()
    w_in_d = DT("w_in", [NL, 1024, D_IN], F32, kind="ExternalInput").ap()
    w_out_d = DT("w_out", [NL, 1024, 1024], F32, kind="ExternalInput").ap()
    w_la_d = DT("w_lru_a", [NL, 8, 64, 64], F32, kind="ExternalInput").ap()
    w_lx_d = DT("w_lru_x", [NL, 8, 64, 64], F32, kind="ExternalInput").ap()
    ffg_d = DT("ffg", [NJG, 128, 8, JGW], F32, kind="ExternalInput").ap()
    ffu_d = DT("ffu", [NJG, 128, 8, JGW], F32, kind="ExternalInput").ap()
    ffd_d = DT("ffd", [D_FF, 1024], F32, kind="ExternalInput").ap()
    w_r_d = DT("w_router", [1024, 8], F32, kind="ExternalInput").ap()
    exg_d = DT("exg", [NE, NJG, 128, 8, JGW], F32, kind="ExternalInput").ap()
    exu_d = DT("exu", [NE, NJG, 128, 8, JGW], F32, kind="ExternalInput").ap()
    exd_d = DT("exd", [NE, D_FF, 1024], F32, kind="ExternalInput").ap()
    st_in_d = DT("st_in", [NL, 128, SW], F32, kind="ExternalInput").ap()
    outT = DT("outT", [1024, T], F32, kind="ExternalOutput").ap()
    st_out_d = DT("st_out", [NL, 128, SW], F32, kind="ExternalOutput").ap()

    xT_v = xT.rearrange("(kc p) t -> p kc t", p=128)
    outT_v = outT.rearrange("(kc p) t -> p kc t", p=128)

    es = contextlib.ExitStack()
    P = Prog(nc, es)
    dx = [P.pseudo(f"dx{s}") for s in range(NSUB)]

    consts = P.sb([128, 256 + 1024], F32, "consts")
    vec = P.sb([128, NV], F32, "vec")
    P.dma("sp", consts[:], consts_d, writes=[consts])
    P.dma("sp", vec[:], vecs_d, writes=[vec])
    ident_f = consts[:, 0:128]
    mask_f = consts[:, 128:256]
    ident_b = P.sb([128, 128], BF16, "identb")
    P.op("dve", lambda: nc.vector.tensor_copy(out=ident_b[:], in_=ident_f), reads=[consts], writes=[ident_b])
    onesD = P.sb([128, 128], F32, "onesD")
    onesL = P.sb([128, 128], F32, "onesL")
    ones1 = P.sb([128, 512], F32, "ones1")
    epsc = P.sb([128, 1], F32, "epsc")
    P.op("dve", lambda: nc.vector.memset(onesD[:], 1.0 / 1024), writes=[onesD])
    P.op("dve", lambda: nc.vector.memset(onesL[:], 1.0 / 512), writes=[onesL])
    P.op("dve", lambda: nc.vector.memset(ones1[:], 1.0), writes=[ones1])
    P.op("dve", lambda: nc.vector.memset(epsc[:], EPS), writes=[epsc])

    def V(name, j=0, n=1):
        o, _ = lay[name]
        return vec[:, o + j:o + j + n]

    psf = Ring([P.ps([128, 512], F32, "psf") for _ in range(6)])
    psb = Ring([P.ps([128, 1024], BF16, "psb") for _ in range(2)])

    def mm(o_t, o_ap, l_ap, r_ap, reads, start=True, stop=True):
        P.op("pe", lambda: nc.tensor.matmul(o_ap, l_ap, r_ap, start=start, stop=stop), reads=reads, writes=[o_t])

    def act(o_t, o_ap, i_ap, func, reads, scale=None, bias=None, accum=None, extra_w=()):
        kw = {}
        if scale is not None:
            kw["scale"] = scale
        if bias is not None:
            kw["bias"] = bias
        if accum is not None:
            kw["accum_out"] = accum
        P.op("act", lambda: nc.scalar.activation(out=o_ap, in_=i_ap, func=func, **kw), reads=reads,
             writes=[o_t] + list(extra_w))

    def tt(e, o_t, o_ap, a_ap, b_ap, op, reads):
        eng = nc.vector if e == "dve" else nc.gpsimd
        P.op(e, lambda: eng.tensor_tensor(out=o_ap, in0=a_ap, in1=b_ap, op=op), reads=reads, writes=[o_t])

    def ts(o_t, o_ap, a_ap, s1, s2, op0, op1, reads, e="dve"):
        eng = nc.vector if e == "dve" else nc.gpsimd
        if op1 is None:
            P.op(e, lambda: eng.tensor_scalar(out=o_ap, in0=a_ap, scalar1=s1, scalar2=None, op0=op0), reads=reads, writes=[o_t])
        else:
            P.op(e, lambda: eng.tensor_scalar(out=o_ap, in0=a_ap, scalar1=s1, scalar2=s2, op0=op0, op1=op1), reads=reads, writes=[o_t])

    def stt(o_t, o_ap, a_ap, s, b_ap, op0, op1, reads):
        P.op("dve", lambda: nc.vector.scalar_tensor_tensor(out=o_ap, in0=a_ap, scalar=s, in1=b_ap, op0=op0, op1=op1),
             reads=reads, writes=[o_t])

    mod = P.sb([128, NL * 48], F32, "mod")
    derived = P.sb([128, NL * 64], F32, "derived")
    with contextlib.ExitStack() as es2:
        P.tes = es2
        cond = P.sb([128, 8], F32, "cond")
        act(cond, cond[:], V("cond", 0, 8), AF.Silu, [vec])
        wa_ring = Ring([P.sb([128, 8, 1024], F32, "wada") for _ in range(2)])
        pmod = psf.next()
        for l in range(NL):
            for sl in range(6):
                wa = wa_ring.next()
                P.dma("sp", wa[:], w_ada_d[l].rearrange("(kc p) n -> p kc n", p=128)[:, :, sl * 1024:(sl + 1) * 1024], writes=[wa])
                for j in range(8):
                    col = l * 48 + sl * 8 + j
                    for kc in range(8):
                        mm(pmod, pmod[:, col:col + 1], wa[:, kc, j * 128:(j + 1) * 128], cond[:, kc:kc + 1], [wa, cond],
                           start=(kc == 0), stop=(kc == 7))
        for l in range(NL):
            o = lay[f"bada{l}"][0]
            tt("dve", mod, mod[:, l * 48:(l + 1) * 48], pmod[:, l * 48:(l + 1) * 48], vec[:, o:o + 48], ALU.add, [pmod, vec])
            m0 = l * 48
            d0 = l * 64
            stt(derived, derived[:, d0:d0 + 8], mod[:, m0 + 8:m0 + 16], 1.0, V(f"gm{l}", 0, 8), ALU.add, ALU.mult, [mod, vec])
            P.op("dve", lambda: nc.vector.tensor_copy(out=derived[:, d0 + 8:d0 + 16], in_=mod[:, m0:m0 + 8]), reads=[mod], writes=[derived])
            P.op("dve", lambda: nc.vector.tensor_copy(out=derived[:, d0 + 16:d0 + 24], in_=mod[:, m0 + 16:m0 + 24]), reads=[mod], writes=[derived])
            stt(derived, derived[:, d0 + 24:d0 + 32], mod[:, m0 + 32:m0 + 40], 1.0, V(f"gf{l}", 0, 8), ALU.add, ALU.mult, [mod, vec])
            P.op("dve", lambda: nc.vector.tensor_copy(out=derived[:, d0 + 32:d0 + 40], in_=mod[:, m0 + 24:m0 + 32]), reads=[mod], writes=[derived])
            P.op("dve", lambda: nc.vector.tensor_copy(out=derived[:, d0 + 40:d0 + 48], in_=mod[:, m0 + 40:m0 + 48]), reads=[mod], writes=[derived])
            tmp4 = P.sb([128, 4], F32, "tmp4")
            act(tmp4, tmp4[:], V(f"lam{l}", 0, 4), AF.Exp, [vec], scale=-1.0)
            act(tmp4, tmp4[:], tmp4[:], AF.Ln, [tmp4], bias=1.0)
            ts(derived, derived[:, d0 + 48:d0 + 52], tmp4[:], -8.0, None, ALU.mult, None, [tmp4])
            ts(derived, derived[:, d0 + 52:d0 + 56], tmp4[:], -16.0, None, ALU.mult, None, [tmp4])
            ts(derived, derived[:, d0 + 56:d0 + 57], V(f"bg{l}", 1, 1), -1.0, None, ALU.mult, None, [vec])
        P.barrier()
    P.tes = es

    def Dv(l, j, n=1):
        return derived[:, l * 64 + j:l * 64 + j + n]

    def rms_rstd(x_t, x_ap_of_kc, nk, ones_t, sq_ring, rstd_t, reads_extra=()):
        ss = psf.next()
        for kc in range(nk):
            sq = sq_ring.next()
            act(sq, sq[:], x_ap_of_kc(kc), AF.Square, ([x_t] if x_t is not None else []) + list(reads_extra))
            mm(ss, ss[:], ones_t[:], sq[:], [ones_t, sq], start=(kc == 0), stop=(kc == nk - 1))
        act(rstd_t, rstd_t[:], ss[:], AF.Sqrt, [ss, epsc], bias=epsc[:, 0:1], scale=1.0)
        P.op("dve", lambda: nc.vector.reciprocal(out=rstd_t[:], in_=rstd_t[:]), reads=[rstd_t], writes=[rstd_t])

    def mixer_phase(l):
        with contextlib.ExitStack() as es2:
            P.tes = es2
            win = [P.sb([128, 2, D_IN], BF16, "win") for _ in range(4)]
            wout = [P.sb([128, 4, 1024], BF16, "wout") for _ in range(2)]
            w_in_v = w_in_d[l].rearrange("(kc p) n -> p kc n", p=128)
            w_out_v = w_out_d[l].rearrange("(kc p) n -> p kc n", p=128)
            for i in range(4):
                P.dma("pool", win[i][:], w_in_v[:, 2 * i:2 * i + 2, :], writes=[win[i]])
            for i in range(2):
                P.dma("pool", wout[i][:], w_out_v[:, 4 * i:4 * i + 4, :], writes=[wout[i]])
            bd_a = P.sb([128, 4, 128], BF16, "bda")
            bd_x = P.sb([128, 4, 128], BF16, "bdx")
            for bd, wd in ((bd_a, w_la_d), (bd_x, w_lx_d)):
                P.op("dve", lambda: nc.vector.memset(bd[:], 0.0), writes=[bd])
                wv = wd[l].rearrange("(c two) k m -> two k c m", two=2)
                P.dma("pool", bd[0:64, :, 0:64], wv[0], writes=[bd])
                P.dma("pool", bd[64:128, :, 64:128], wv[1], reads=[bd], writes=[bd])
            gmb = P.sb([128, 512], F32, "gmb")
            P.dma("sp", gmb[:], gmixb_d[l], writes=[gmb])

            def wcol(kc, c0, c1):
                return win[kc // 2][:, kc % 2, c0:c1]

            Cst = P.sb([128, 4, 129], F32, "Cst")
            Cbf = P.sb([128, 4, 129], BF16, "Cbf")
            lru_h = P.sb([128, 4], F32, "lruh")
            gcar = P.sb([4, 2], F32, "gcar")
            tails = P.sb([128, 12, 3], F32, "tails")
            cbuf = Ring([P.sb([128, 515], F32, "cbuf") for _ in range(3)])
            sti = st_in_d[l]
            P.dma("sp", Cst[:].rearrange("p h j -> p (h j)"), sti[:, 0:516], writes=[Cst])
            P.dma("sp", lru_h[:], sti[:, 516:520], writes=[lru_h])
            P.dma("sp", tails[:].rearrange("p c j -> p (c j)"), sti[:, 520:556], writes=[tails])
            P.op("dve", lambda: nc.vector.memset(gcar[:], 0.0), writes=[gcar])
            P.dma("sp", gcar[0:4, 1:2], sti[0:4, 556:557], reads=[gcar], writes=[gcar], slow=True)

            xring = Ring([P.sb([128, 8, 512], F32, "xt") for _ in range(1)])
            hT = [P.sb([128, 512], BF16, "hT") for _ in range(8)]
            tmp_ring = Ring([P.sb([128, 512], F32, "tmp") for _ in range(3)])
            sq_ring = tmp_ring
            rstd = P.sb([128, 512], F32, "rstd")
            qk_bf = [P.sb([128, 512], BF16, "qkbf") for _ in range(8)]
            acc_ring = Ring([P.sb([128, 512], F32, "acc") for _ in range(2)])
            vaug = [P.sb([128, 4, 129], BF16, "vaug") for _ in range(4)]
            for tb in range(4):
                P.op("dve", lambda: nc.vector.memset(vaug[tb][:], 1.0), writes=[vaug[tb]])
            go = [P.sb([128, 512], F32, "go") for _ in range(4)]
            nb = P.sb([4, 512], F32, "nb")
            wv_t = P.sb([4, 512], F32, "wv")
            mu = P.sb([4, 512], F32, "mu")
            e1 = P.sb([4, 512], F32, "e1")
            fl = P.sb([4, 512], F32, "fl")
            g_e = e1
            g_l = fl
            rp = P.sb([4, 4], F32, "rp")
            a_t = P.sb([4, 4], F32, "a_t")
            adiag = P.sb([4, 16], F32, "adiag")
            abc = P.sb([128, 16], F32, "abc")
            tsc = [P.sb([128, 8], F32, "tsc") for _ in range(4)]
            ke_ring = Ring([P.sb([128, 128], BF16, "ke") for _ in range(2)])
            stm_ring = Ring([P.sb([128, 128], BF16, "stm") for _ in range(2)])
            den_ring = Ring([P.sb([128, 1], F32, "den") for _ in range(2)])
            hm4_ring = Ring([P.sb([128, 512], F32, "hm4") for _ in range(1)])
            junk = P.sb([128, 128], F32, "junk")
            ssq_ring = Ring([P.sb([128, 4], F32, "ssq") for _ in range(2)])
            hmn_ring = Ring([P.sb([128, 512], BF16, "hmn") for _ in range(1)])
            hmix = [P.sb([128, 512], BF16, "hmix") for _ in range(8)]
            xcb_ring = Ring([P.sb([128, 512], BF16, "xcb") for _ in range(2)])
            lt = [P.sb([128, 512], F32, "lt") for _ in range(6)]
            hl = [P.sb([128, 512], F32, "hl") for _ in range(4)]
            rstl = P.sb([128, 512], F32, "rstl")

            for s in range(NSUB):
                t0 = s * 512
                xt = xring.next()
                if l == 0:
                    P.dma("sp", xt[:], xT_v[:, :, t0:t0 + 512], writes=[xt])
                else:
                    P.dma("sp", xt[:], outT_v[:, :, t0:t0 + 512], reads=[dx[s]], writes=[xt])
                rms_rstd(xt, lambda kc: xt[:, kc, :], 8, onesD, sq_ring, rstd)
                for kc in range(8):
                    tmp = tmp_ring.next()
                    tt("dve", tmp, tmp[:], xt[:, kc, :], rstd[:], ALU.mult, [xt, rstd])
                    act(hT[kc], hT[kc][:], tmp[:], AF.Identity, [tmp, derived], scale=Dv(l, 0 + kc), bias=Dv(l, 8 + kc))

                def proj_fm(ps, M, col0):
                    for kc in range(8):
                        mm(ps, ps[0:M, :], wcol(kc, col0, col0 + M), hT[kc][:], [win[kc // 2], hT[kc]],
                           start=(kc == 0), stop=(kc == 7))

                def conv(ps, ci, wname, bname, c, acc):
                    buf = cbuf.next()
                    act(buf, buf[:, 3:515], ps[:], AF.Copy, [ps])
                    P.op("dve", lambda: nc.vector.tensor_copy(out=buf[:, 0:3], in_=tails[:, ci, :]), reads=[tails, buf], writes=[buf])
                    wo = lay[wname][0] + 4 * c
                    bo = lay[bname][0] + c
                    ts(acc, acc[:], buf[:, 3:515], vec[:, wo + 3:wo + 4], vec[:, bo:bo + 1], ALU.mult, ALU.add, [buf, vec])
                    for j in (2, 1, 0):
                        stt(acc, acc[:], buf[:, j:j + 512], vec[:, wo + j:wo + j + 1], acc[:], ALU.mult, ALU.add, [buf, vec, acc])
                    P.op("dve", lambda: nc.vector.tensor_copy(out=tails[:, ci, :], in_=buf[:, 512:515]), reads=[buf, tails], writes=[tails])

                for c in range(8):
                    ps = psf.next()
                    proj_fm(ps, 128, C_Q + c * 128)
                    acc = acc_ring.next()
                    conv(ps, c, f"wcqk{l}", f"bcqk{l}", c, acc)
                    act(qk_bf[c], qk_bf[c][:], acc[:], AF.Silu, [acc])

                psi = psf.next()
                proj_fm(psi, 4, C_IG)
                psg = psf.next()
                proj_fm(psg, 4, C_FG)
                act(g_e, g_e[:], psg[0:4, :], AF.Exp, [psg, derived], scale=-1.0, bias=derived[0:4, l * 64 + 56:l * 64 + 57])
                act(g_l, g_l[:], g_e[:], AF.Ln, [g_e], bias=1.0)
                P.op("dve", lambda: nc.vector.tensor_tensor_scan(out=nb[:], data0=ones1[0:4, :], data1=g_l[:], initial=gcar[0:4, 0:1],
                                                                 op0=ALU.mult, op1=ALU.add), reads=[ones1, g_l, gcar], writes=[nb])
                bgo = lay[f"bg{l}"][0]
                stt(wv_t, wv_t[:], psi[0:4, :], vec[0:4, bgo:bgo + 1], nb[:], ALU.add, ALU.add, [psi, vec, nb])
                P.op("dve", lambda: nc.vector.tensor_tensor_scan(out=mu[:], data0=wv_t[:], data1=wv_t[:], initial=gcar[0:4, 1:2],
                                                                 op0=ALU.max, op1=ALU.max), reads=[wv_t, gcar], writes=[mu])
                mu_v = mu[:].rearrange("p (c t) -> p c t", t=128)
                Rb = mu_v[:, :, 127:128].to_broadcast([4, 4, 128])
                P.op("dve", lambda: nc.vector.tensor_copy(out=rp[:, 0:1], in_=gcar[0:4, 1:2]), reads=[gcar], writes=[rp])
                P.op("dve", lambda: nc.vector.tensor_copy(out=rp[:, 1:4].unsqueeze(2), in_=mu_v[:, 0:3, 127:128]), reads=[mu, rp], writes=[rp])
                tt("dve", a_t, a_t[:].unsqueeze(2), rp[:].unsqueeze(2), mu_v[:, :, 127:128], ALU.subtract, [rp, mu])
                act(a_t, a_t[:], a_t[:], AF.Exp, [a_t])
                tt("dve", adiag, adiag[:].rearrange("p (c h) -> p c h", h=4), a_t[:].unsqueeze(2).to_broadcast([4, 4, 4]),
                   consts[0:4, 0:4].unsqueeze(1).to_broadcast([4, 4, 4]), ALU.mult, [a_t, consts])
                pab = psf.next()
                mm(pab, pab[:, 0:16], ones1[0:4, 0:128], adiag[:], [ones1, adiag])
                P.op("dve", lambda: nc.vector.tensor_copy(out=abc[:], in_=pab[:, 0:16]), reads=[pab], writes=[abc])
                tt("dve", e1, e1[:].rearrange("p (c t) -> p c t", t=128), wv_t[:].rearrange("p (c t) -> p c t", t=128), Rb, ALU.subtract, [wv_t, mu])
                ts(e1, e1[:], e1[:], -0.5 * math.log(128.0), None, ALU.add, None, [e1])
                act(e1, e1[:], e1[:], AF.Exp, [e1])
                tt("dve", fl, fl[:].rearrange("p (c t) -> p c t", t=128), nb[:].rearrange("p (c t) -> p c t", t=128), Rb, ALU.subtract, [nb, mu])
                act(fl, fl[:], fl[:], AF.Exp, [fl])
                P.op("dve", lambda: nc.vector.tensor_copy(out=gcar[0:4, 0:1], in_=nb[:, 511:512]), reads=[nb, gcar], writes=[gcar])
                P.op("dve", lambda: nc.vector.tensor_copy(out=gcar[0:4, 1:2], in_=mu[:, 511:512]), reads=[mu, gcar], writes=[gcar])
                for tb in range(4):
                    pt = psf.next()
                    P.op("pe", lambda: nc.tensor.transpose(out=pt[:, 0:4], in_=e1[0:4, tb * 128:(tb + 1) * 128], identity=consts[0:4, 0:4]),
                         reads=[e1, consts], writes=[pt])
                    P.op("pe", lambda: nc.tensor.transpose(out=pt[:, 4:8], in_=fl[0:4, tb * 128:(tb + 1) * 128], identity=consts[0:4, 0:4]),
                         reads=[fl, consts], writes=[pt])
                    P.op("dve", lambda: nc.vector.tensor_copy(out=tsc[tb][:], in_=pt[:, 0:8]), reads=[pt], writes=[tsc[tb]])

                for tb in range(4):
                    blk = slice(tb * 128, (tb + 1) * 128)
                    psv = psf.next()
                    for kc in range(8):
                        mm(psv, psv[:], hT[kc][:, blk], wcol(kc, C_V, C_V + 512), [hT[kc], win[kc // 2]], start=(kc == 0), stop=(kc == 7))
                    act(vaug[tb], vaug[tb][:, :, 0:128], psv[:].rearrange("p (h d) -> p h d", d=128), AF.Copy, [psv])
                    pso = psf.next()
                    for kc in range(8):
                        mm(pso, pso[:], hT[kc][:, blk], wcol(kc, C_O, C_O + 512), [hT[kc], win[kc // 2]], start=(kc == 0), stop=(kc == 7))
                    act(go[tb], go[tb][:], pso[:], AF.Sigmoid, [pso])
                    tt("pool", go[tb], go[tb][:], go[tb][:], gmb[:], ALU.mult, [go[tb], gmb])

                for tb in range(4):
                    blk = slice(tb * 128, (tb + 1) * 128)
                    hm4 = hm4_ring.next()
                    ssq = ssq_ring.next()
                    for h in range(4):
                        kt = qk_bf[4 + h]
                        qt = qk_bf[h]
                        pk = psb.next()
                        P.op("pe", lambda: nc.tensor.transpose(out=pk[:, 0:128], in_=kt[:, blk], identity=ident_b[:]), reads=[kt, ident_b], writes=[pk])
                        ke = ke_ring.next()
                        act(ke, ke[:], pk[:, 0:128], AF.Copy, [pk, tsc[tb]], scale=tsc[tb][:, h:h + 1])
                        pS = psf.next()
                        mm(pS, pS[:, 0:128], kt[:, blk], qt[:, blk], [kt, qt])
                        stm = stm_ring.next()
                        stt(stm, stm[:], pS[:, 0:128], tsc[tb][:, h:h + 1], mask_f, ALU.mult, ALU.mult, [pS, tsc[tb], consts])
                        ts(Cst, Cst[:, h, :], Cst[:, h, :], abc[:, tb * 4 + h:tb * 4 + h + 1], None, ALU.mult, None, [Cst, abc])
                        act(Cbf, Cbf[:, h, :], Cst[:, h, :], AF.Copy, [Cst])
                        pO = psf.next()
                        mm(pO, pO[:, 0:129], stm[:], vaug[tb][:, h, :], [stm, vaug[tb]], start=True, stop=False)
                        mm(pO, pO[:, 0:129], qt[:, blk], Cbf[:, h, :], [qt, Cbf], start=False, stop=True)
                        pU = psf.next()
                        mm(pU, pU[:, 0:129], ke[:], vaug[tb][:, h, :], [ke, vaug[tb]])
                        tt("dve", Cst, Cst[:, h, :], Cst[:, h, :], pU[:, 0:129], ALU.add, [Cst, pU])
                        den = den_ring.next()
                        act(den, den[:], pO[:, 128:129], AF.Abs, [pO])
                        tt("dve", den, den[:], den[:], tsc[tb][:, 4 + h:5 + h], ALU.max, [den, tsc[tb]])
                        P.op("dve", lambda: nc.vector.reciprocal(out=den[:], in_=den[:]), reads=[den], writes=[den])
                        act(hm4, hm4[:, h * 128:(h + 1) * 128], pO[:, 0:128], AF.Copy, [pO, den], scale=den[:, 0:1])
                        act(junk, junk[:], hm4[:, h * 128:(h + 1) * 128], AF.Square, [hm4], accum=ssq[:, h:h + 1], extra_w=[ssq])
                    ts(ssq, ssq[:], ssq[:], 1.0 / 128, EPS, ALU.mult, ALU.add, [ssq])
                    act(ssq, ssq[:], ssq[:], AF.Sqrt, [ssq])
                    P.op("dve", lambda: nc.vector.reciprocal(out=ssq[:], in_=ssq[:]), reads=[ssq], writes=[ssq])
                    hmn = hmn_ring.next()
                    for h in range(4):
                        hs_ = slice(h * 128, (h + 1) * 128)
                        stt(hmn, hmn[:, hs_], hm4[:, hs_], ssq[:, h:h + 1], go[tb][:, hs_], ALU.mult, ALU.mult, [hm4, ssq, go[tb]])
                    for h in range(4):
                        hs_ = slice(h * 128, (h + 1) * 128)
                        ph = psb.next()
                        P.op("pe", lambda: nc.tensor.transpose(out=ph[:, 0:128], in_=hmn[:, hs_], identity=ident_b[:]), reads=[hmn, ident_b], writes=[ph])
                        act(hmix[h], hmix[h][:, blk], ph[:, 0:128], AF.Copy, [ph])

                pn = None
                for c in range(4):
                    ps = psf.next()
                    proj_fm(ps, 128, C_XL + c * 128)
                    xc, r_t, a_, a2, i_t, hs = lt
                    gel = r_t
                    conv(ps, 8 + c, f"wcl{l}", f"bcl{l}", c, xc)
                    xcb = xcb_ring.next()
                    act(xcb, xcb[:], xc[:], AF.Copy, [xc])
                    pa = psf.next()
                    mm(pa, pa[:], bd_a[:, c, :], xcb[:], [bd_a, xcb])
                    px = psf.next()
                    mm(px, px[:], bd_x[:, c, :], xcb[:], [bd_x, xcb])
                    act(r_t, r_t[:], pa[:], AF.Sigmoid, [pa, vec], bias=V(f"bla{l}", c))
                    act(a_, a_[:], r_t[:], AF.Exp, [r_t, derived], scale=Dv(l, 48 + c))
                    act(a2, a2[:], r_t[:], AF.Exp, [r_t, derived], scale=Dv(l, 52 + c))
                    ts(a2, a2[:], a2[:], -1.0, 1.0, ALU.mult, ALU.add, [a2])
                    ts(a2, a2[:], a2[:], 1e-30, None, ALU.max, None, [a2])
                    act(a2, a2[:], a2[:], AF.Sqrt, [a2])
                    act(i_t, i_t[:], px[:], AF.Sigmoid, [px, vec], bias=V(f"blx{l}", c))
                    tt("dve", i_t, i_t[:], i_t[:], xc[:], ALU.mult, [i_t, xc])
                    tt("dve", i_t, i_t[:], i_t[:], a2[:], ALU.mult, [i_t, a2])
                    P.op("dve", lambda: nc.vector.tensor_tensor_scan(out=hs[:], data0=a_[:], data1=i_t[:], initial=lru_h[:, c:c + 1],
                                                                     op0=ALU.mult, op1=ALU.add), reads=[a_, i_t, lru_h], writes=[hs])
                    P.op("dve", lambda: nc.vector.tensor_copy(out=lru_h[:, c:c + 1], in_=hs[:, 511:512]), reads=[hs, lru_h], writes=[lru_h])
                    psl = psf.next()
                    proj_fm(psl, 128, C_GL + c * 128)
                    act(gel, gel[:], psl[:], AF.Gelu_apprx_tanh, [psl])
                    tt("dve", hl[c], hl[c][:], hs[:], gel[:], ALU.mult, [hs, gel])
                rms_rstd(None, lambda c: hl[c][:], 4, onesL, sq_ring, rstl, reads_extra=hl)
                for c in range(4):
                    stt(hmix[4 + c], hmix[4 + c][:], hl[c][:], V(f"gml{l}", c), rstl[:], ALU.mult, ALU.mult, [hl[c], vec, rstl])

                for dc in range(8):
                    ps = psf.next()
                    for kc in range(8):
                        mm(ps, ps[:], wout[kc // 4][:, kc % 4, dc * 128:(dc + 1) * 128], hmix[kc][:], [wout[kc // 4], hmix[kc]],
                           start=(kc == 0), stop=(kc == 7))
                    stt(xt, xt[:, dc, :], ps[:], Dv(l, 16 + dc), xt[:, dc, :], ALU.mult, ALU.add, [ps, derived, xt])
                P.dma("sp", outT_v[:, :, t0:t0 + 512], xt[:], reads=[xt], writes=[dx[s]])

            sto = st_out_d[l]
            P.dma("sp", sto[:, 0:516], Cst[:].rearrange("p h j -> p (h j)"), reads=[Cst])
            P.dma("sp", sto[:, 516:520], lru_h[:], reads=[lru_h])
            P.dma("sp", sto[:, 520:556], tails[:].rearrange("p c j -> p (c j)"), reads=[tails])
            mend = P.sb([128, 4], F32, "mend")
            P.op("dve", lambda: nc.vector.memset(mend[:], 0.0), writes=[mend])
            tt("dve", mend, mend[0:4, 0:1], gcar[0:4, 1:2], gcar[0:4, 0:1], ALU.subtract, [gcar, mend])
            P.dma("sp", sto[:, 556:560], mend[:], reads=[mend])
            P.barrier()
        P.tes = es

    def ffn_phase(l, moe, final):
        with contextlib.ExitStack() as es2:
            P.tes = es2
            xF = P.sb([128, 8, TT], F32, "xF")
            hF = [P.sb([128, TT], BF16, "hF") for _ in range(8)]
            tmp_ring = Ring([P.sb([128, 512], F32, "tmp") for _ in range(3)])
            sq_ring = tmp_ring
            rstd = P.sb([128, 512], F32, "rstd")
            wg_ring = Ring([P.sb([128, 8, JGW], BF16, "wg") for _ in range(3)])
            wu_ring = Ring([P.sb([128, 8, JGW], BF16, "wu") for _ in range(3)])
            wd_ring = Ring([P.sb([128, 2, 1024], BF16, "wd") for _ in range(3)])
            sg_ring = Ring([P.sb([128, 512], F32, "sg") for _ in range(3)])
            act_ring = Ring([P.sb([128, 512], BF16, "actb") for _ in range(4 * NB)])
            if moe:
                hf32 = [P.sb([128, 512], F32, "hf32") for _ in range(8)]
                wr = P.sb([128, 8, 8], F32, "wr")
                P.dma("sp", wr[:], w_r_d.rearrange("(kc p) e -> p kc e", p=128), writes=[wr])
                brb = P.sb([128, 8], F32, "brb")
                P.dma("sp", brb[:], brb_d, writes=[brb])
                gTs = P.sb([8, TT], F32, "gTs")
                gb = [P.sb([128, TT], F32, "gb") for _ in range(NE)]
                lg = P.sb([128, 8], F32, "lg")
                lg2 = P.sb([128, 8], F32, "lg2")
                m1 = P.sb([128, 1], F32, "m1")
                m2 = P.sb([128, 1], F32, "m2")
                selt = P.sb([128, 8], F32, "selt")
                pe_ = P.sb([128, 8], F32, "pe_")
                dsum = P.sb([128, 1], F32, "dsum")
                sg2_ring = Ring([P.sb([128, 512], F32, "sg2") for _ in range(3)])
            n_exp = NE if moe else 1

            for tti in range(NTT):
                T0 = tti * TT
                blocks = [dx[(T0 // 512) + n] for n in range(NB)]
                P.dma("sp", xF[:], outT_v[:, :, T0:T0 + TT], reads=blocks, writes=[xF])
                for n in range(NB):
                    nb_ = slice(n * 512, (n + 1) * 512)
                    rms_rstd(xF, lambda kc: xF[:, kc, nb_], 8, onesD, sq_ring, rstd)
                    for kc in range(8):
                        tmp = tmp_ring.next()
                        tt("dve", tmp, tmp[:], xF[:, kc, nb_], rstd[:], ALU.mult, [xF, rstd])
                        if moe:
                            act(hf32[kc], hf32[kc][:], tmp[:], AF.Identity, [tmp, derived], scale=Dv(l, 24 + kc), bias=Dv(l, 32 + kc))
                            P.op("pool", lambda: nc.gpsimd.tensor_copy(out=hF[kc][:, nb_], in_=hf32[kc][:]), reads=[hf32[kc]], writes=[hF[kc]])
                        else:
                            act(hF[kc], hF[kc][:, nb_], tmp[:], AF.Identity, [tmp, derived], scale=Dv(l, 24 + kc), bias=Dv(l, 32 + kc))
                    if moe:
                        for tb in range(4):
                            tblk = slice(tb * 128, (tb + 1) * 128)
                            pr = psf.next()
                            for kc in range(8):
                                mm(pr, pr[:, 0:8], hf32[kc][:, tblk], wr[:, kc, :], [hf32[kc], wr], start=(kc == 0), stop=(kc == 7))
                            tt("dve", lg, lg[:], pr[:, 0:8], brb[:], ALU.add, [pr, brb])
                            P.op("dve", lambda: nc.vector.tensor_reduce(out=m1[:], in_=lg[:], axis=AX.X, op=ALU.max), reads=[lg], writes=[m1])
                            ts(selt, selt[:], lg[:], m1[:, 0:1], -1e30, ALU.is_ge, ALU.mult, [lg, m1])
                            tt("dve", lg2, lg2[:], lg[:], selt[:], ALU.add, [lg, selt])
                            P.op("dve", lambda: nc.vector.tensor_reduce(out=m2[:], in_=lg2[:], axis=AX.X, op=ALU.max), reads=[lg2], writes=[m2])
                            ts(selt, selt[:], lg[:], m2[:, 0:1], None, ALU.is_ge, None, [lg, m2])
                            ts(m1, m1[:], m1[:], -1.0, None, ALU.mult, None, [m1])
                            act(pe_, pe_[:], lg[:], AF.Exp, [lg, m1], bias=m1[:, 0:1], scale=1.0)
                            tt("dve", pe_, pe_[:], pe_[:], selt[:], ALU.mult, [pe_, selt])
                            P.op("dve", lambda: nc.vector.tensor_reduce(out=dsum[:], in_=pe_[:], axis=AX.X, op=ALU.add), reads=[pe_], writes=[dsum])
                            P.op("dve", lambda: nc.vector.reciprocal(out=dsum[:], in_=dsum[:]), reads=[dsum], writes=[dsum])
                            ts(pe_, pe_[:], pe_[:], dsum[:, 0:1], None, ALU.mult, None, [pe_, dsum])
                            pg = psf.next()
                            P.op("pe", lambda: nc.tensor.transpose(out=pg[0:8, 0:128], in_=pe_[:], identity=ident_f), reads=[pe_, consts], writes=[pg])
                            c0 = n * 512 + tb * 128
                            P.op("dve", lambda: nc.vector.tensor_copy(out=gTs[:, c0:c0 + 128], in_=pg[0:8, 0:128]), reads=[pg], writes=[gTs])
                        for e in range(NE):
                            pb = psf.next()
                            mm(pb, pb[:], consts[0:8, 256 + e * 128:256 + (e + 1) * 128], gTs[:, nb_], [consts, gTs])
                            act(gb[e], gb[e][:, nb_], pb[:], AF.Copy, [pb])

                stages = [(e, jg) for e in range(n_exp) for jg in range(NJG)]
                loaded = {}

                def load(k):
                    e, jg = stages[k]
                    wg = wg_ring.next()
                    wu = wu_ring.next()
                    wd = wd_ring.next()
                    if moe:
                        P.dma("pool", wg[:], exg_d[e, jg], writes=[wg])
                        P.dma("pool", wu[:], exu_d[e, jg], writes=[wu])
                        P.dma("pool", wd[:], exd_d[e, jg * JGW:(jg + 1) * JGW, :].rearrange("(c p) n -> p c n", p=128), writes=[wd])
                    else:
                        P.dma("pool", wg[:], ffg_d[jg], writes=[wg])
                        P.dma("pool", wu[:], ffu_d[jg], writes=[wu])
                        P.dma("pool", wd[:], ffd_d[jg * JGW:(jg + 1) * JGW, :].rearrange("(c p) n -> p c n", p=128), writes=[wd])
                    loaded[k] = (wg, wu, wd)

                load(0)
                if len(stages) > 1:
                    load(1)
                for k in range(len(stages)):
                    if k + 2 < len(stages):
                        load(k + 2)
                    e, jg = stages[k]
                    wg, wu, wd = loaded.pop(k)
                    acts = {}
                    for jj in range(2):
                        for n in range(NB):
                            nb_ = slice(n * 512, (n + 1) * 512)
                            pgp = psf.next()
                            for kc in range(8):
                                mm(pgp, pgp[:], wg[:, kc, jj * 128:(jj + 1) * 128], hF[kc][:, nb_], [wg, hF[kc]], start=(kc == 0), stop=(kc == 7))
                            pup = psf.next()
                            for kc in range(8):
                                mm(pup, pup[:], wu[:, kc, jj * 128:(jj + 1) * 128], hF[kc][:, nb_], [wu, hF[kc]], start=(kc == 0), stop=(kc == 7))
                            sg = sg_ring.next()
                            act(sg, sg[:], pgp[:], AF.Silu, [pgp])
                            ab = act_ring.next()
                            if moe:
                                sg2 = sg2_ring.next()
                                tt("dve", sg2, sg2[:], sg[:], pup[:], ALU.mult, [sg, pup])
                                tt("pool", ab, ab[:], sg2[:], gb[e][:, nb_], ALU.mult, [sg2, gb[e]])
                            else:
                                tt("dve", ab, ab[:], sg[:], pup[:], ALU.mult, [sg, pup])
                            acts[(jj, n)] = ab
                    for n in range(NB):
                        nb_ = slice(n * 512, (n + 1) * 512)
                        for dc in range(8):
                            py = psf.next()
                            for jj in range(2):
                                mm(py, py[:], wd[:, jj, dc * 128:(dc + 1) * 128], acts[(jj, n)][:], [wd, acts[(jj, n)]], start=(jj == 0), stop=(jj == 1))
                            stt(xF, xF[:, dc, nb_], py[:], Dv(l, 40 + dc), xF[:, dc, nb_], ALU.mult, ALU.add, [py, derived, xF])

                if final:
                    for n in range(NB):
                        nb_ = slice(n * 512, (n + 1) * 512)
                        rms_rstd(xF, lambda kc: xF[:, kc, nb_], 8, onesD, sq_ring, rstd)
                        for kc in range(8):
                            stt(xF, xF[:, kc, nb_], xF[:, kc, nb_], V("gfin", kc), rstd[:], ALU.mult, ALU.mult, [xF, vec, rstd])
                P.dma("sp", outT_v[:, :, T0:T0 + TT], xF[:], reads=[xF], writes=blocks)
            P.barrier()
        P.tes = es

    for l in range(NL):
        mixer_phase(l)
        ffn_phase(l, moe=(l in moe_layers), final=(l == NL - 1))
    P.barrier()
    es.close()
    return nc


_NC_CACHE = {}


def _prep_shared(inp, NL):
    sh = {}
    sh["w_ada"] = np.ascontiguousarray(inp["w_ada"], dtype=np.float32)
    sh["w_in"] = np.ascontiguousarray(inp["w_in"], dtype=np.float32)
    sh["w_out"] = np.ascontiguousarray(inp["w_out"], dtype=np.float32)
    sh["w_lru_a"] = np.ascontiguousarray(inp["w_lru_a"], dtype=np.float32)
    sh["w_lru_x"] = np.ascontiguousarray(inp["w_lru_x"], dtype=np.float32)

    def grp(w):
        return np.ascontiguousarray(w.reshape(8, 128, NJG, JGW).transpose(2, 1, 0, 3))

    sh["ffg"] = grp(np.asarray(inp["w_ff_gate"][0], np.float32))
    sh["ffu"] = grp(np.asarray(inp["w_ff_up"][0], np.float32))
    sh["ffd"] = np.ascontiguousarray(inp["w_ff_down"][0], dtype=np.float32)
    sh["w_router"] = np.ascontiguousarray(inp["w_router"][0], dtype=np.float32)
    sh["brb"] = np.ascontiguousarray(np.broadcast_to(np.asarray(inp["b_router"][0], np.float32)[None, :], (128, 8)))
    sh["exg"] = np.stack([grp(np.asarray(inp["w_exp_gate"][0, e], np.float32)) for e in range(NE)])
    sh["exu"] = np.stack([grp(np.asarray(inp["w_exp_up"][0, e], np.float32)) for e in range(NE)])
    sh["exd"] = np.ascontiguousarray(inp["w_exp_down"][0], dtype=np.float32)
    sh["gmixb"] = np.ascontiguousarray(np.broadcast_to(np.asarray(inp["g_mix_out"], np.float32)[:, None, :512], (NL, 128, 512)))
    consts = np.zeros((128, 256 + 1024), np.float32)
    consts[:, 0:128] = np.eye(128, dtype=np.float32)
    consts[:, 128:256] = np.triu(np.ones((128, 128), np.float32))
    for e in range(NE):
        consts[e, 256 + e * 128:256 + (e + 1) * 128] = 1.0
    sh["consts"] = consts
    return sh


def _vecs(inp, b, NL):
    lay, NV = vec_layout(NL)
    v = np.zeros((128, NV), np.float32)

    def put(name, arr):
        o, n = lay[name]
        assert arr.shape == (128, n), (name, arr.shape, n)
        v[:, o:o + n] = arr

    put("cond", fm(inp["c"][b], 8))
    for l in range(NL):
        put(f"bada{l}", fm(inp["b_ada"][l], 48))
        put(f"gm{l}", fm(inp["g_norm_mix"][l], 8))
        put(f"gf{l}", fm(inp["g_norm_ffn"][l], 8))
        w = np.asarray(inp["w_conv_qk"][l], np.float32)
        put(f"wcqk{l}", np.ascontiguousarray(w.T.reshape(8, 128, 4).transpose(1, 0, 2).reshape(128, 32)))
        put(f"bcqk{l}", fm(inp["b_conv_qk"][l], 8))
        w = np.asarray(inp["w_conv_lru"][l], np.float32)
        put(f"wcl{l}", np.ascontiguousarray(w.T.reshape(4, 128, 4).transpose(1, 0, 2).reshape(128, 16)))
        put(f"bcl{l}", fm(inp["b_conv_lru"][l], 4))
        put(f"bla{l}", fm(inp["b_lru_a"][l], 4))
        put(f"blx{l}", fm(inp["b_lru_x"][l], 4))
        put(f"lam{l}", fm(inp["lru_lambda"][l], 4))
        put(f"gml{l}", fm(inp["g_mix_out"][l][512:], 4))
        bg = np.zeros((128, 2), np.float32)
        bg[0:4, 0] = inp["b_gates"][l][0:4]
        bg[0:4, 1] = inp["b_gates"][l][4:8]
        put(f"bg{l}", bg)
    put("gfin", fm(inp["g_final"], 8))
    return v


def kernel(**inputs):
    x = np.asarray(inputs["x"], np.float32)
    B, S, D = x.shape
    NL = int(np.asarray(inputs["w_in"]).shape[0])
    n_cores = 2 * B
    T = S // 2
    key = (T, NL)
    if key not in _NC_CACHE:
        _NC_CACHE[key] = build(T, NL)
    nc = _NC_CACHE[key]
    sh = _prep_shared(inputs, NL)
    base = []
    for cid in range(n_cores):
        b, half = cid // 2, cid % 2
        m = dict(sh)
        m["xT"] = np.ascontiguousarray(x[b, half * T:(half + 1) * T, :].T)
        m["vecs"] = _vecs(inputs, b, NL)
        base.append(m)
    zeros_st = np.zeros((NL, 128, SW), np.float32)
    in_maps = [dict(m, st_in=zeros_st) for m in base]
    res = run_bass_kernel_spmd(nc, in_maps, core_ids=list(range(n_cores)))
    in_maps = []
    for cid in range(n_cores):
        st = zeros_st if cid % 2 == 0 else np.ascontiguousarray(res.results[cid - 1]["st_out"])
        in_maps.append(dict(base[cid], st_in=st))
    res = run_bass_kernel_spmd(nc, in_maps, core_ids=list(range(n_cores)))
    out = np.empty((B, S, D), np.float32)
    for cid in range(n_cores):
        b, half = cid // 2, cid % 2
        out[b, half * T:(half + 1) * T, :] = res.results[cid]["outT"].T
    return out
```

```python
import contextlib
import math
import numpy as np
import concourse.bass as bass
import concourse.mybir as mybir
from concourse.bass_utils import run_bass_kernel_spmd

F32 = mybir.dt.float32
BF16 = mybir.dt.bfloat16
ALU = mybir.AluOpType
AF = mybir.ActivationFunctionType
AX = mybir.AxisListType

D_MODEL = 1024
D_FF = 2816
NE = 8
EPS = 1e-6
D_IN = 3080
SW = 560
C_Q, C_K, C_V, C_O, C_IG, C_FG, C_XL, C_GL = 0, 512, 1024, 1536, 2048, 2052, 2056, 2568
JGW = 256
NJG = D_FF // JGW

SAME_ENGINE_SYNC = True
EPOCH = 16000
_SKIP_MIX = False
_SKIP_FFN = False


class Tl:
    __slots__ = ("t", "w", "r", "name")

    def __init__(self, t, name):
        self.t = t
        self.w = None
        self.r = {}
        self.name = name

    def __getitem__(self, k):
        return self.t[k]


class Ring:
    def __init__(self, tiles):
        self.tiles = tiles
        self.i = 0

    def next(self):
        t = self.tiles[self.i % len(self.tiles)]
        self.i += 1
        return t


class Prog:
    def __init__(self, nc, es, nslots=40):
        self.nc = nc
        self.es = es
        self.tes = es
        self.eng = {"pe": nc.tensor, "act": nc.scalar, "dve": nc.vector, "pool": nc.gpsimd, "sp": nc.sync}
        self.sems = {}
        self.cnt = {e: 0 for e in self.eng}
        self.seen = {e: {} for e in self.eng}
        self.nslots = nslots
        self.slot_sem = [es.enter_context(nc.semaphore(f"dq{i}")) for i in range(nslots)]
        self.slot_cnt = [0] * nslots
        self.next_slot = 0
        self.uid = 0

    def _esem(self, e, ep):
        k = (e, ep)
        if k not in self.sems:
            self.sems[k] = self.es.enter_context(self.nc.semaphore(f"s_{e}_{ep}"))
        return self.sems[k]

    def sb(self, shape, dtype, name=None):
        self.uid += 1
        name = f"{name or 't'}_{self.uid}"
        return Tl(self.tes.enter_context(self.nc.sbuf_tensor(name, list(shape), dtype)), name)

    def ps(self, shape, dtype, name=None):
        self.uid += 1
        name = f"{name or 'p'}_{self.uid}"
        return Tl(self.tes.enter_context(self.nc.psum_tensor(name, list(shape), dtype)), name)

    def pseudo(self, name):
        return Tl(None, name)

    def _wait(self, e, dep):
        kind, key, val = dep
        k = (kind, key)
        if self.seen[e].get(k, 0) >= val:
            return
        sem = self._esem(key[0], key[1]) if kind == "eng" else self.slot_sem[key]
        self.eng[e].wait_ge(sem, val)
        self.seen[e][k] = val

    def _deps(self, e, reads, writes, force_same=False):
        deps = []
        for t in reads:
            if t.w is not None:
                deps.append(t.w)
        for t in writes:
            if t.w is not None:
                deps.append(t.w)
            for k, v in t.r.items():
                deps.append((k[0], k[1], v))
        for d in deps:
            if d[0] == "eng" and d[1][0] == e and not force_same and (e == "pe" or not SAME_ENGINE_SYNC):
                continue
            self._wait(e, d)

    def _mark(self, me, reads, writes):
        k = (me[0], me[1])
        for t in reads:
            if t.r.get(k, 0) < me[2]:
                t.r[k] = me[2]
        for t in writes:
            t.w = me
            t.r = {}

    def _cur(self, e):
        c = self.cnt[e]
        ep = (c - 1) // EPOCH
        return ("eng", (e, ep), c - ep * EPOCH)

    def op(self, e, fn, reads=(), writes=()):
        self._deps(e, reads, writes)
        ins = fn()
        self.cnt[e] += 1
        me = self._cur(e)
        ins.then_inc(self._esem(e, me[1][1]), 1)
        self._mark(me, reads, writes)
        return me

    def dma(self, q, out, in_, reads=(), writes=(), slow=False):
        slot = self.next_slot
        self.next_slot = (slot + 1) % self.nslots
        if self.slot_cnt[slot] > 0:
            self._wait(q, ("dma", slot, 16 * self.slot_cnt[slot]))
        self._deps(q, reads, writes, force_same=True)
        if slow:
            ins = self.eng[q].dma_start(out=out, in_=in_, allow_slow_non_contiguous=True)
        else:
            ins = self.eng[q].dma_start(out=out, in_=in_)
        self.slot_cnt[slot] += 1
        ins.then_inc(self.slot_sem[slot], 16)
        me = ("dma", slot, 16 * self.slot_cnt[slot])
        self._mark(me, reads, writes)
        return me

    def barrier(self):
        deps = []
        for e in self.eng:
            if self.cnt[e] > 0:
                deps.append(self._cur(e))
        for s in range(self.nslots):
            if self.slot_cnt[s] > 0:
                deps.append(("dma", s, 16 * self.slot_cnt[s]))
        for e in self.eng:
            for d in deps:
                self._wait(e, d)


def vec_layout(NL):
    lay = {}
    off = 0

    def add(name, n):
        nonlocal off
        lay[name] = (off, n)
        off += n

    add("cond", 8)
    for l in range(NL):
        add(f"bada{l}", 48)
        add(f"gm{l}", 8)
        add(f"gf{l}", 8)
        add(f"wcqk{l}", 32)
        add(f"bcqk{l}", 8)
        add(f"wcl{l}", 16)
        add(f"bcl{l}", 4)
        add(f"bla{l}", 4)
        add(f"blx{l}", 4)
        add(f"lam{l}", 4)
        add(f"gml{l}", 4)
        add(f"bg{l}", 2)
    add("gfin", 8)
    add("flag", 1)
    return lay, off


def fm(v, nch):
    return np.ascontiguousarray(np.asarray(v, np.float32).reshape(nch, 128).T)


def build(T, NL, moe_layers=(1,), prefix=True):
    NSUB = T // 512
    TT = min(1024, T)
    NTT = T // TT
    NB = TT // 512
    lay, NV = vec_layout(NL)

    nc = bass.Bass("TRN2", target_bir_lowering=False)
    DT = nc.dram_tensor
    xT = DT("xT", [1024, T], F32, kind="ExternalInput").ap()
    xpT = DT("xpT", [1024, T], F32, kind="ExternalInput").ap()
    preT = DT("preT", [1024, T], F32, kind="Internal").ap()
    st_pre_d = DT("st_pre", [NL, 128, SW], F32, kind="Internal").ap()
    vecs_d = DT("vecs", [128, NV], F32, kind="ExternalInput").ap()
    consts_d = DT("consts", [128, 256 + 1024], F32, kind="ExternalInput").ap()
    gmixb_d = DT("gmixb", [NL, 128, 512], F32, kind="ExternalInput").ap()
    brb_d = DT("brb", [128, 8], F32, kind="ExternalInput").ap()
    w_ada_d = DT("w_ada", [NL, 1024, 6144], F32, kind="ExternalInput").ap()
    w_in_d = DT("w_in", [NL, 1024, D_IN], F32, kind="ExternalInput").ap()
    w_out_d = DT("w_out", [NL, 1024, 1024], F32, kind="ExternalInput").ap()
    w_la_d = DT("w_lru_a", [NL, 8, 64, 64], F32, kind="ExternalInput").ap()
    w_lx_d = DT("w_lru_x", [NL, 8, 64, 64], F32, kind="ExternalInput").ap()
    ffg_d = DT("ffg", [NJG, 128, 8, JGW], F32, kind="ExternalInput").ap()
    ffu_d = DT("ffu", [NJG, 128, 8, JGW], F32, kind="ExternalInput").ap()
    ffd_d = DT("ffd", [D_FF, 1024], F32, kind="ExternalInput").ap()
    w_r_d = DT("w_router", [1024, 8], F32, kind="ExternalInput").ap()
    exg_d = DT("exg", [NE, NJG, 128, 8, JGW], F32, kind="ExternalInput").ap()
    exu_d = DT("exu", [NE, NJG, 128, 8, JGW], F32, kind="ExternalInput").ap()
    exd_d = DT("exd", [NE, D_FF, 1024], F32, kind="ExternalInput").ap()
    st_in_d = DT("st_in", [NL, 128, SW], F32, kind="ExternalInput").ap()
    outT = DT("outT", [1024, T], F32, kind="ExternalOutput").ap()
    st_out_d = DT("st_out", [NL, 128, SW], F32, kind="ExternalOutput").ap()

    xT_v = xT.rearrange("(kc p) t -> p kc t", p=128)
    xpT_v = xpT.rearrange("(kc p) t -> p kc t", p=128)
    preT_v = preT.rearrange("(kc p) t -> p kc t", p=128)
    outT_v = outT.rearrange("(kc p) t -> p kc t", p=128)

    es = contextlib.ExitStack()
    P = Prog(nc, es)
    dx = [P.pseudo(f"dx{s}") for s in range(NSUB)]
    dpre = [P.pseudo(f"dpre{s}") for s in range(NSUB)]
    dstp = [P.pseudo(f"dstp{l}") for l in range(NL)]

    consts = P.sb([128, 256 + 1024], F32, "consts")
    vec = P.sb([128, NV], F32, "vec")
    P.dma("sp", consts[:], consts_d, writes=[consts])
    P.dma("sp", vec[:], vecs_d, writes=[vec])
    ident_f = consts[:, 0:128]
    mask_f = consts[:, 128:256]
    ident_b = P.sb([128, 128], BF16, "identb")
    P.op("dve", lambda: nc.vector.tensor_copy(out=ident_b[:], in_=ident_f), reads=[consts], writes=[ident_b])
    onesD = P.sb([128, 128], F32, "onesD")
    onesL = P.sb([128, 128], F32, "onesL")
    ones1 = P.sb([128, 512], F32, "ones1")
    epsc = P.sb([128, 1], F32, "epsc")
    P.op("dve", lambda: nc.vector.memset(onesD[:], 1.0 / 1024), writes=[onesD])
    P.op("dve", lambda: nc.vector.memset(onesL[:], 1.0 / 512), writes=[onesL])
    P.op("dve", lambda: nc.vector.memset(ones1[:], 1.0), writes=[ones1])
    P.op("dve", lambda: nc.vector.memset(epsc[:], EPS), writes=[epsc])

    def V(name, j=0, n=1):
        o, _ = lay[name]
        return vec[:, o + j:o + j + n]

    psf = Ring([P.ps([128, 512], F32, "psf") for _ in range(6)])
    psb = Ring([P.ps([128, 1024], BF16, "psb") for _ in range(2)])

    def mm(o_t, o_ap, l_ap, r_ap, reads, start=True, stop=True):
        P.op("pe", lambda: nc.tensor.matmul(o_ap, l_ap, r_ap, start=start, stop=stop), reads=reads, writes=[o_t])

    def act(o_t, o_ap, i_ap, func, reads, scale=None, bias=None, accum=None, extra_w=()):
        kw = {}
        if scale is not None:
            kw["scale"] = scale
        if bias is not None:
            kw["bias"] = bias
        if accum is not None:
            kw["accum_out"] = accum
        P.op("act", lambda: nc.scalar.activation(out=o_ap, in_=i_ap, func=func, **kw), reads=reads,
             writes=[o_t] + list(extra_w))

    def tt(e, o_t, o_ap, a_ap, b_ap, op, reads):
        eng = nc.vector if e == "dve" else nc.gpsimd
        P.op(e, lambda: eng.tensor_tensor(out=o_ap, in0=a_ap, in1=b_ap, op=op), reads=reads, writes=[o_t])

    def ts(o_t, o_ap, a_ap, s1, s2, op0, op1, reads, e="dve"):
        eng = nc.vector if e == "dve" else nc.gpsimd
        if op1 is None:
            P.op(e, lambda: eng.tensor_scalar(out=o_ap, in0=a_ap, scalar1=s1, scalar2=None, op0=op0), reads=reads, writes=[o_t])
        else:
            P.op(e, lambda: eng.tensor_scalar(out=o_ap, in0=a_ap, scalar1=s1, scalar2=s2, op0=op0, op1=op1), reads=reads, writes=[o_t])

    def stt(o_t, o_ap, a_ap, s, b_ap, op0, op1, reads):
        P.op("dve", lambda: nc.vector.scalar_tensor_tensor(out=o_ap, in0=a_ap, scalar=s, in1=b_ap, op0=op0, op1=op1),
             reads=reads, writes=[o_t])

    mod = P.sb([128, NL * 48], F32, "mod")
    derived = P.sb([128, NL * 64], F32, "derived")
    with contextlib.ExitStack() as es2:
        P.tes = es2
        cond = P.sb([128, 8], F32, "cond")
        act(cond, cond[:], V("cond", 0, 8), AF.Silu, [vec])
        wa_ring = Ring([P.sb([128, 8, 1024], F32, "wada") for _ in range(2)])
        pmod = psf.next()
        for l in range(NL):
            for sl in range(6):
                wa = wa_ring.next()
                P.dma("sp", wa[:], w_ada_d[l].rearrange("(kc p) n -> p kc n", p=128)[:, :, sl * 1024:(sl + 1) * 1024], writes=[wa])
                for j in range(8):
                    col = l * 48 + sl * 8 + j
                    for kc in range(8):
                        mm(pmod, pmod[:, col:col + 1], wa[:, kc, j * 128:(j + 1) * 128], cond[:, kc:kc + 1], [wa, cond],
                           start=(kc == 0), stop=(kc == 7))
        for l in range(NL):
            o = lay[f"bada{l}"][0]
            tt("dve", mod, mod[:, l * 48:(l + 1) * 48], pmod[:, l * 48:(l + 1) * 48], vec[:, o:o + 48], ALU.add, [pmod, vec])
            m0 = l * 48
            d0 = l * 64
            stt(derived, derived[:, d0:d0 + 8], mod[:, m0 + 8:m0 + 16], 1.0, V(f"gm{l}", 0, 8), ALU.add, ALU.mult, [mod, vec])
            P.op("dve", lambda: nc.vector.tensor_copy(out=derived[:, d0 + 8:d0 + 16], in_=mod[:, m0:m0 + 8]), reads=[mod], writes=[derived])
            P.op("dve", lambda: nc.vector.tensor_copy(out=derived[:, d0 + 16:d0 + 24], in_=mod[:, m0 + 16:m0 + 24]), reads=[mod], writes=[derived])
            stt(derived, derived[:, d0 + 24:d0 + 32], mod[:, m0 + 32:m0 + 40], 1.0, V(f"gf{l}", 0, 8), ALU.add, ALU.mult, [mod, vec])
            P.op("dve", lambda: nc.vector.tensor_copy(out=derived[:, d0 + 32:d0 + 40], in_=mod[:, m0 + 24:m0 + 32]), reads=[mod], writes=[derived])
            P.op("dve", lambda: nc.vector.tensor_copy(out=derived[:, d0 + 40:d0 + 48], in_=mod[:, m0 + 40:m0 + 48]), reads=[mod], writes=[derived])
            tmp4 = P.sb([128, 4], F32, "tmp4")
            act(tmp4, tmp4[:], V(f"lam{l}", 0, 4), AF.Exp, [vec], scale=-1.0)
            act(tmp4, tmp4[:], tmp4[:], AF.Ln, [tmp4], bias=1.0)
            ts(derived, derived[:, d0 + 48:d0 + 52], tmp4[:], -8.0, None, ALU.mult, None, [tmp4])
            ts(derived, derived[:, d0 + 52:d0 + 56], tmp4[:], -16.0, None, ALU.mult, None, [tmp4])
            ts(derived, derived[:, d0 + 56:d0 + 57], V(f"bg{l}", 1, 1), -1.0, None, ALU.mult, None, [vec])
        P.barrier()
    P.tes = es

    def Dv(l, j, n=1):
        return derived[:, l * 64 + j:l * 64 + j + n]

    def rms_rstd(x_t, x_ap_of_kc, nk, ones_t, sq_ring, rstd_t, reads_extra=()):
        ss = psf.next()
        for kc in range(nk):
            sq = sq_ring.next()
            act(sq, sq[:], x_ap_of_kc(kc), AF.Square, ([x_t] if x_t is not None else []) + list(reads_extra))
            mm(ss, ss[:], ones_t[:], sq[:], [ones_t, sq], start=(kc == 0), stop=(kc == nk - 1))
        act(rstd_t, rstd_t[:], ss[:], AF.Sqrt, [ss, epsc], bias=epsc[:, 0:1], scale=1.0)
        P.op("dve", lambda: nc.vector.reciprocal(out=rstd_t[:], in_=rstd_t[:]), reads=[rstd_t], writes=[rstd_t])

    def mixer_phase(l, src_v, src_blocks, dst_v, dst_blocks, st_src, st_src_tile, st_dst, st_dst_tile, use_flag):
        with contextlib.ExitStack() as es2:
            P.tes = es2
            win = [P.sb([128, 2, D_IN], BF16, "win") for _ in range(4)]
            wout = [P.sb([128, 4, 1024], BF16, "wout") for _ in range(2)]
            w_in_v = w_in_d[l].rearrange("(kc p) n -> p kc n", p=128)
            w_out_v = w_out_d[l].rearrange("(kc p) n -> p kc n", p=128)
            for i in range(4):
                P.dma("pool", win[i][:], w_in_v[:, 2 * i:2 * i + 2, :], writes=[win[i]])
            for i in range(2):
                P.dma("pool", wout[i][:], w_out_v[:, 4 * i:4 * i + 4, :], writes=[wout[i]])
            bd_a = P.sb([128, 4, 128], BF16, "bda")
            bd_x = P.sb([128, 4, 128], BF16, "bdx")
            for bd, wd in ((bd_a, w_la_d), (bd_x, w_lx_d)):
                P.op("dve", lambda: nc.vector.memset(bd[:], 0.0), writes=[bd])
                wv = wd[l].rearrange("(c two) k m -> two k c m", two=2)
                P.dma("pool", bd[0:64, :, 0:64], wv[0], writes=[bd])
                P.dma("pool", bd[64:128, :, 64:128], wv[1], reads=[bd], writes=[bd])
            gmb = P.sb([128, 512], F32, "gmb")
            P.dma("sp", gmb[:], gmixb_d[l], writes=[gmb])

            def wcol(kc, c0, c1):
                return win[kc // 2][:, kc % 2, c0:c1]

            Cst = P.sb([128, 4, 129], F32, "Cst")
            Cbf = P.sb([128, 4, 129], BF16, "Cbf")
            lru_h = P.sb([128, 4], F32, "lruh")
            gcar = P.sb([4, 2], F32, "gcar")
            tails = P.sb([128, 12, 3], F32, "tails")
            cbuf = Ring([P.sb([128, 515], F32, "cbuf") for _ in range(3)])
            sti = st_src
            srd = [st_src_tile] if st_src_tile is not None else []
            P.dma("sp", Cst[:].rearrange("p h j -> p (h j)"), sti[:, 0:516], reads=srd, writes=[Cst])
            P.dma("sp", lru_h[:], sti[:, 516:520], reads=srd, writes=[lru_h])
            P.dma("sp", tails[:].rearrange("p c j -> p (c j)"), sti[:, 520:556], reads=srd, writes=[tails])
            P.op("dve", lambda: nc.vector.memset(gcar[:], 0.0), writes=[gcar])
            P.dma("sp", gcar[0:4, 1:2], sti[0:4, 556:557], reads=[gcar] + srd, writes=[gcar], slow=True)
            if use_flag:
                fo = lay["flag"][0]
                ts(Cst, Cst[:].rearrange("p h j -> p (h j)"), Cst[:].rearrange("p h j -> p (h j)"), vec[:, fo:fo + 1], None, ALU.mult, None, [Cst, vec])
                ts(lru_h, lru_h[:], lru_h[:], vec[:, fo:fo + 1], None, ALU.mult, None, [lru_h, vec])
                ts(tails, tails[:].rearrange("p c j -> p (c j)"), tails[:].rearrange("p c j -> p (c j)"), vec[:, fo:fo + 1], None, ALU.mult, None, [tails, vec])
                ts(gcar, gcar[0:4, 1:2], gcar[0:4, 1:2], vec[0:4, fo:fo + 1], None, ALU.mult, None, [gcar, vec])

            xring = Ring([P.sb([128, 8, 512], F32, "xt") for _ in range(1)])
            hT = [P.sb([128, 512], BF16, "hT") for _ in range(8)]
            tmp_ring = Ring([P.sb([128, 512], F32, "tmp") for _ in range(3)])
            sq_ring = tmp_ring
            rstd = P.sb([128, 512], F32, "rstd")
            qk_bf = [P.sb([128, 512], BF16, "qkbf") for _ in range(8)]
            acc_ring = Ring([P.sb([128, 512], F32, "acc") for _ in range(2)])
            vaug = [P.sb([128, 4, 129], BF16, "vaug") for _ in range(4)]
            for tb in range(4):
                P.op("dve", lambda: nc.vector.memset(vaug[tb][:], 1.0), writes=[vaug[tb]])
            go = [P.sb([128, 512], F32, "go") for _ in range(4)]
            nb = P.sb([4, 512], F32, "nb")
            wv_t = P.sb([4, 512], F32, "wv")
            mu = P.sb([4, 512], F32, "mu")
            e1 = P.sb([4, 512], F32, "e1")
            fl = P.sb([4, 512], F32, "fl")
            g_e = e1
            g_l = fl
            rp = P.sb([4, 4], F32, "rp")
            a_t = P.sb([4, 4], F32, "a_t")
            adiag = P.sb([4, 16], F32, "adiag")
            abc = P.sb([128, 16], F32, "abc")
            tsc = [P.sb([128, 8], F32, "tsc") for _ in range(4)]
            ke_ring = Ring([P.sb([128, 128], BF16, "ke") for _ in range(2)])
            stm_ring = Ring([P.sb([128, 128], BF16, "stm") for _ in range(2)])
            den_ring = Ring([P.sb([128, 1], F32, "den") for _ in range(2)])
            hm4_ring = Ring([P.sb([128, 512], F32, "hm4") for _ in range(1)])
            junk = P.sb([128, 128], F32, "junk")
            ssq_ring = Ring([P.sb([128, 4], F32, "ssq") for _ in range(2)])
            hmn_ring = Ring([P.sb([128, 512], BF16, "hmn") for _ in range(1)])
            hmix = [P.sb([128, 512], BF16, "hmix") for _ in range(8)]
            xcb_ring = Ring([P.sb([128, 512], BF16, "xcb") for _ in range(2)])
            lt = [P.sb([128, 512], F32, "lt") for _ in range(6)]
            hl = [P.sb([128, 512], F32, "hl") for _ in range(4)]
            rstl = P.sb([128, 512], F32, "rstl")

            for s in range(NSUB):
                t0 = s * 512
                xt = xring.next()
                P.dma("sp", xt[:], src_v[:, :, t0:t0 + 512], reads=([src_blocks[s]] if src_blocks is not None else []), writes=[xt])
                rms_rstd(xt, lambda kc: xt[:, kc, :], 8, onesD, sq_ring, rstd)
                for kc in range(8):
                    tmp = tmp_ring.next()
                    tt("dve", tmp, tmp[:], xt[:, kc, :], rstd[:], ALU.mult, [xt, rstd])
                    act(hT[kc], hT[kc][:], tmp[:], AF.Identity, [tmp, derived], scale=Dv(l, 0 + kc), bias=Dv(l, 8 + kc))

                def proj_fm(ps, M, col0):
                    for kc in range(8):
                        mm(ps, ps[0:M, :], wcol(kc, col0, col0 + M), hT[kc][:], [win[kc // 2], hT[kc]],
                           start=(kc == 0), stop=(kc == 7))

                def conv(ps, ci, wname, bname, c, acc):
                    buf = cbuf.next()
                    act(buf, buf[:, 3:515], ps[:], AF.Copy, [ps])
                    P.op("dve", lambda: nc.vector.tensor_copy(out=buf[:, 0:3], in_=tails[:, ci, :]), reads=[tails, buf], writes=[buf])
                    wo = lay[wname][0] + 4 * c
                    bo = lay[bname][0] + c
                    ts(acc, acc[:], buf[:, 3:515], vec[:, wo + 3:wo + 4], vec[:, bo:bo + 1], ALU.mult, ALU.add, [buf, vec])
                    for j in (2, 1, 0):
                        stt(acc, acc[:], buf[:, j:j + 512], vec[:, wo + j:wo + j + 1], acc[:], ALU.mult, ALU.add, [buf, vec, acc])
                    P.op("dve", lambda: nc.vector.tensor_copy(out=tails[:, ci, :], in_=buf[:, 512:515]), reads=[buf, tails], writes=[tails])

                for c in range(8):
                    ps = psf.next()
                    proj_fm(ps, 128, C_Q + c * 128)
                    acc = acc_ring.next()
                    conv(ps, c, f"wcqk{l}", f"bcqk{l}", c, acc)
                    act(qk_bf[c], qk_bf[c][:], acc[:], AF.Silu, [acc])

                psi = psf.next()
                proj_fm(psi, 4, C_IG)
                psg = psf.next()
                proj_fm(psg, 4, C_FG)
                act(g_e, g_e[:], psg[0:4, :], AF.Exp, [psg, derived], scale=-1.0, bias=derived[0:4, l * 64 + 56:l * 64 + 57])
                act(g_l, g_l[:], g_e[:], AF.Ln, [g_e], bias=1.0)
                P.op("dve", lambda: nc.vector.tensor_tensor_scan(out=nb[:], data0=ones1[0:4, :], data1=g_l[:], initial=gcar[0:4, 0:1],
                                                                 op0=ALU.mult, op1=ALU.add), reads=[ones1, g_l, gcar], writes=[nb])
                bgo = lay[f"bg{l}"][0]
                stt(wv_t, wv_t[:], psi[0:4, :], vec[0:4, bgo:bgo + 1], nb[:], ALU.add, ALU.add, [psi, vec, nb])
                P.op("dve", lambda: nc.vector.tensor_tensor_scan(out=mu[:], data0=wv_t[:], data1=wv_t[:], initial=gcar[0:4, 1:2],
                                                                 op0=ALU.max, op1=ALU.max), reads=[wv_t, gcar], writes=[mu])
                mu_v = mu[:].rearrange("p (c t) -> p c t", t=128)
                Rb = mu_v[:, :, 127:128].to_broadcast([4, 4, 128])
                P.op("dve", lambda: nc.vector.tensor_copy(out=rp[:, 0:1], in_=gcar[0:4, 1:2]), reads=[gcar], writes=[rp])
                P.op("dve", lambda: nc.vector.tensor_copy(out=rp[:, 1:4].unsqueeze(2), in_=mu_v[:, 0:3, 127:128]), reads=[mu, rp], writes=[rp])
                tt("dve", a_t, a_t[:].unsqueeze(2), rp[:].unsqueeze(2), mu_v[:, :, 127:128], ALU.subtract, [rp, mu])
                act(a_t, a_t[:], a_t[:], AF.Exp, [a_t])
                tt("dve", adiag, adiag[:].rearrange("p (c h) -> p c h", h=4), a_t[:].unsqueeze(2).to_broadcast([4, 4, 4]),
                   consts[0:4, 0:4].unsqueeze(1).to_broadcast([4, 4, 4]), ALU.mult, [a_t, consts])
                pab = psf.next()
                mm(pab, pab[:, 0:16], ones1[0:4, 0:128], adiag[:], [ones1, adiag])
                P.op("dve", lambda: nc.vector.tensor_copy(out=abc[:], in_=pab[:, 0:16]), reads=[pab], writes=[abc])
                tt("dve", e1, e1[:].rearrange("p (c t) -> p c t", t=128), wv_t[:].rearrange("p (c t) -> p c t", t=128), Rb, ALU.subtract, [wv_t, mu])
                ts(e1, e1[:], e1[:], -0.5 * math.log(128.0), None, ALU.add, None, [e1])
                act(e1, e1[:], e1[:], AF.Exp, [e1])
                tt("dve", fl, fl[:].rearrange("p (c t) -> p c t", t=128), nb[:].rearrange("p (c t) -> p c t", t=128), Rb, ALU.subtract, [nb, mu])
                act(fl, fl[:], fl[:], AF.Exp, [fl])
                P.op("dve", lambda: nc.vector.tensor_copy(out=gcar[0:4, 0:1], in_=nb[:, 511:512]), reads=[nb, gcar], writes=[gcar])
                P.op("dve", lambda: nc.vector.tensor_copy(out=gcar[0:4, 1:2], in_=mu[:, 511:512]), reads=[mu, gcar], writes=[gcar])
                for tb in range(4):
                    pt = psf.next()
                    P.op("pe", lambda: nc.tensor.transpose(out=pt[:, 0:4], in_=e1[0:4, tb * 128:(tb + 1) * 128], identity=consts[0:4, 0:4]),
                         reads=[e1, consts], writes=[pt])
                    P.op("pe", lambda: nc.tensor.transpose(out=pt[:, 4:8], in_=fl[0:4, tb * 128:(tb + 1) * 128], identity=consts[0:4, 0:4]),
                         reads=[fl, consts], writes=[pt])
                    P.op("dve", lambda: nc.vector.tensor_copy(out=tsc[tb][:], in_=pt[:, 0:8]), reads=[pt], writes=[tsc[tb]])

                for tb in range(4):
                    blk = slice(tb * 128, (tb + 1) * 128)
                    psv = psf.next()
                    for kc in range(8):
                        mm(psv, psv[:], hT[kc][:, blk], wcol(kc, C_V, C_V + 512), [hT[kc], win[kc // 2]], start=(kc == 0), stop=(kc == 7))
                    act(vaug[tb], vaug[tb][:, :, 0:128], psv[:].rearrange("p (h d) -> p h d", d=128), AF.Copy, [psv])
                    pso = psf.next()
                    for kc in range(8):
                        mm(pso, pso[:], hT[kc][:, blk], wcol(kc, C_O, C_O + 512), [hT[kc], win[kc // 2]], start=(kc == 0), stop=(kc == 7))
                    act(go[tb], go[tb][:], pso[:], AF.Sigmoid, [pso])
                    tt("pool", go[tb], go[tb][:], go[tb][:], gmb[:], ALU.mult, [go[tb], gmb])

                for tb in range(4):
                    blk = slice(tb * 128, (tb + 1) * 128)
                    hm4 = hm4_ring.next()
                    ssq = ssq_ring.next()
                    for h in range(4):
                        kt = qk_bf[4 + h]
                        qt = qk_bf[h]
                        pk = psb.next()
                        P.op("pe", lambda: nc.tensor.transpose(out=pk[:, 0:128], in_=kt[:, blk], identity=ident_b[:]), reads=[kt, ident_b], writes=[pk])
                        ke = ke_ring.next()
                        act(ke, ke[:], pk[:, 0:128], AF.Copy, [pk, tsc[tb]], scale=tsc[tb][:, h:h + 1])
                        pS = psf.next()
                        mm(pS, pS[:, 0:128], kt[:, blk], qt[:, blk], [kt, qt])
                        stm = stm_ring.next()
                        stt(stm, stm[:], pS[:, 0:128], tsc[tb][:, h:h + 1], mask_f, ALU.mult, ALU.mult, [pS, tsc[tb], consts])
                        ts(Cst, Cst[:, h, :], Cst[:, h, :], abc[:, tb * 4 + h:tb * 4 + h + 1], None, ALU.mult, None, [Cst, abc])
                        act(Cbf, Cbf[:, h, :], Cst[:, h, :], AF.Copy, [Cst])
                        pO = psf.next()
                        mm(pO, pO[:, 0:129], stm[:], vaug[tb][:, h, :], [stm, vaug[tb]], start=True, stop=False)
                        mm(pO, pO[:, 0:129], qt[:, blk], Cbf[:, h, :], [qt, Cbf], start=False, stop=True)
                        pU = psf.next()
                        mm(pU, pU[:, 0:129], ke[:], vaug[tb][:, h, :], [ke, vaug[tb]])
                        tt("dve", Cst, Cst[:, h, :], Cst[:, h, :], pU[:, 0:129], ALU.add, [Cst, pU])
                        den = den_ring.next()
                        act(den, den[:], pO[:, 128:129], AF.Abs, [pO])
                        tt("dve", den, den[:], den[:], tsc[tb][:, 4 + h:5 + h], ALU.max, [den, tsc[tb]])
                        P.op("dve", lambda: nc.vector.reciprocal(out=den[:], in_=den[:]), reads=[den], writes=[den])
                        act(hm4, hm4[:, h * 128:(h + 1) * 128], pO[:, 0:128], AF.Copy, [pO, den], scale=den[:, 0:1])
                        act(junk, junk[:], hm4[:, h * 128:(h + 1) * 128], AF.Square, [hm4], accum=ssq[:, h:h + 1], extra_w=[ssq])
                    ts(ssq, ssq[:], ssq[:], 1.0 / 128, EPS, ALU.mult, ALU.add, [ssq])
                    act(ssq, ssq[:], ssq[:], AF.Sqrt, [ssq])
                    P.op("dve", lambda: nc.vector.reciprocal(out=ssq[:], in_=ssq[:]), reads=[ssq], writes=[ssq])
                    hmn = hmn_ring.next()
                    for h in range(4):
                        hs_ = slice(h * 128, (h + 1) * 128)
                        stt(hmn, hmn[:, hs_], hm4[:, hs_], ssq[:, h:h + 1], go[tb][:, hs_], ALU.mult, ALU.mult, [hm4, ssq, go[tb]])
                    for h in range(4):
                        hs_ = slice(h * 128, (h + 1) * 128)
                        ph = psb.next()
                        P.op("pe", lambda: nc.tensor.transpose(out=ph[:, 0:128], in_=hmn[:, hs_], identity=ident_b[:]), reads=[hmn, ident_b], writes=[ph])
                        act(hmix[h], hmix[h][:, blk], ph[:, 0:128], AF.Copy, [ph])

                pn = None
                for c in range(4):
                    ps = psf.next()
                    proj_fm(ps, 128, C_XL + c * 128)
                    xc, r_t, a_, a2, i_t, hs = lt
                    gel = r_t
                    conv(ps, 8 + c, f"wcl{l}", f"bcl{l}", c, xc)
                    xcb = xcb_ring.next()
                    act(xcb, xcb[:], xc[:], AF.Copy, [xc])
                    pa = psf.next()
                    mm(pa, pa[:], bd_a[:, c, :], xcb[:], [bd_a, xcb])
                    px = psf.next()
                    mm(px, px[:], bd_x[:, c, :], xcb[:], [bd_x, xcb])
                    act(r_t, r_t[:], pa[:], AF.Sigmoid, [pa, vec], bias=V(f"bla{l}", c))
                    act(a_, a_[:], r_t[:], AF.Exp, [r_t, derived], scale=Dv(l, 48 + c))
                    act(a2, a2[:], r_t[:], AF.Exp, [r_t, derived], scale=Dv(l, 52 + c))
                    ts(a2, a2[:], a2[:], -1.0, 1.0, ALU.mult, ALU.add, [a2])
                    ts(a2, a2[:], a2[:], 1e-30, None, ALU.max, None, [a2])
                    act(a2, a2[:], a2[:], AF.Sqrt, [a2])
                    act(i_t, i_t[:], px[:], AF.Sigmoid, [px, vec], bias=V(f"blx{l}", c))
                    tt("dve", i_t, i_t[:], i_t[:], xc[:], ALU.mult, [i_t, xc])
                    tt("dve", i_t, i_t[:], i_t[:], a2[:], ALU.mult, [i_t, a2])
                    P.op("dve", lambda: nc.vector.tensor_tensor_scan(out=hs[:], data0=a_[:], data1=i_t[:], initial=lru_h[:, c:c + 1],
                                                                     op0=ALU.mult, op1=ALU.add), reads=[a_, i_t, lru_h], writes=[hs])
                    P.op("dve", lambda: nc.vector.tensor_copy(out=lru_h[:, c:c + 1], in_=hs[:, 511:512]), reads=[hs, lru_h], writes=[lru_h])
                    psl = psf.next()
                    proj_fm(psl, 128, C_GL + c * 128)
                    act(gel, gel[:], psl[:], AF.Gelu_apprx_tanh, [psl])
                    tt("dve", hl[c], hl[c][:], hs[:], gel[:], ALU.mult, [hs, gel])
                rms_rstd(None, lambda c: hl[c][:], 4, onesL, sq_ring, rstl, reads_extra=hl)
                for c in range(4):
                    stt(hmix[4 + c], hmix[4 + c][:], hl[c][:], V(f"gml{l}", c), rstl[:], ALU.mult, ALU.mult, [hl[c], vec, rstl])

                for dc in range(8):
                    ps = psf.next()
                    for kc in range(8):
                        mm(ps, ps[:], wout[kc // 4][:, kc % 4, dc * 128:(dc + 1) * 128], hmix[kc][:], [wout[kc // 4], hmix[kc]],
                           start=(kc == 0), stop=(kc == 7))
                    stt(xt, xt[:, dc, :], ps[:], Dv(l, 16 + dc), xt[:, dc, :], ALU.mult, ALU.add, [ps, derived, xt])
                P.dma("sp", dst_v[:, :, t0:t0 + 512], xt[:], reads=[xt], writes=[dst_blocks[s]])

            sto = st_dst
            swr = [st_dst_tile] if st_dst_tile is not None else []
            P.dma("sp", sto[:, 0:516], Cst[:].rearrange("p h j -> p (h j)"), reads=[Cst], writes=swr)
            P.dma("sp", sto[:, 516:520], lru_h[:], reads=[lru_h], writes=swr)
            P.dma("sp", sto[:, 520:556], tails[:].rearrange("p c j -> p (c j)"), reads=[tails], writes=swr)
            mend = P.sb([128, 4], F32, "mend")
            P.op("dve", lambda: nc.vector.memset(mend[:], 0.0), writes=[mend])
            tt("dve", mend, mend[0:4, 0:1], gcar[0:4, 1:2], gcar[0:4, 0:1], ALU.subtract, [gcar, mend])
            P.dma("sp", sto[:, 556:560], mend[:], reads=[mend], writes=swr)
            P.barrier()
        P.tes = es

    def ffn_phase(l, moe, final, buf_v, buf_blocks):
        with contextlib.ExitStack() as es2:
            P.tes = es2
            xF = P.sb([128, 8, TT], F32, "xF")
            hF = [P.sb([128, TT], BF16, "hF") for _ in range(8)]
            tmp_ring = Ring([P.sb([128, 512], F32, "tmp") for _ in range(3)])
            sq_ring = tmp_ring
            rstd = P.sb([128, 512], F32, "rstd")
            wg_ring = Ring([P.sb([128, 8, JGW], BF16, "wg") for _ in range(3)])
            wu_ring = Ring([P.sb([128, 8, JGW], BF16, "wu") for _ in range(3)])
            wd_ring = Ring([P.sb([128, 2, 1024], BF16, "wd") for _ in range(3)])
            sg_ring = Ring([P.sb([128, 512], F32, "sg") for _ in range(3)])
            act_ring = Ring([P.sb([128, 512], BF16, "actb") for _ in range(4 * NB)])
            if moe:
                hf32 = [P.sb([128, 512], F32, "hf32") for _ in range(8)]
                wr = P.sb([128, 8, 8], F32, "wr")
                P.dma("sp", wr[:], w_r_d.rearrange("(kc p) e -> p kc e", p=128), writes=[wr])
                brb = P.sb([128, 8], F32, "brb")
                P.dma("sp", brb[:], brb_d, writes=[brb])
                gTs = P.sb([8, TT], F32, "gTs")
                gb = [P.sb([128, TT], F32, "gb") for _ in range(NE)]
                lg = P.sb([128, 8], F32, "lg")
                lg2 = P.sb([128, 8], F32, "lg2")
                m1 = P.sb([128, 1], F32, "m1")
                m2 = P.sb([128, 1], F32, "m2")
                selt = P.sb([128, 8], F32, "selt")
                pe_ = P.sb([128, 8], F32, "pe_")
                dsum = P.sb([128, 1], F32, "dsum")
                sg2_ring = Ring([P.sb([128, 512], F32, "sg2") for _ in range(3)])
            n_exp = NE if moe else 1

            for tti in range(NTT):
                T0 = tti * TT
                blocks = [buf_blocks[(T0 // 512) + n] for n in range(NB)]
                P.dma("sp", xF[:], buf_v[:, :, T0:T0 + TT], reads=blocks, writes=[xF])
                for n in range(NB):
                    nb_ = slice(n * 512, (n + 1) * 512)
                    rms_rstd(xF, lambda kc: xF[:, kc, nb_], 8, onesD, sq_ring, rstd)
                    for kc in range(8):
                        tmp = tmp_ring.next()
                        tt("dve", tmp, tmp[:], xF[:, kc, nb_], rstd[:], ALU.mult, [xF, rstd])
                        if moe:
                            act(hf32[kc], hf32[kc][:], tmp[:], AF.Identity, [tmp, derived], scale=Dv(l, 24 + kc), bias=Dv(l, 32 + kc))
                            P.op("pool", lambda: nc.gpsimd.tensor_copy(out=hF[kc][:, nb_], in_=hf32[kc][:]), reads=[hf32[kc]], writes=[hF[kc]])
                        else:
                            act(hF[kc], hF[kc][:, nb_], tmp[:], AF.Identity, [tmp, derived], scale=Dv(l, 24 + kc), bias=Dv(l, 32 + kc))
                    if moe:
                        for tb in range(4):
                            tblk = slice(tb * 128, (tb + 1) * 128)
                            pr = psf.next()
                            for kc in range(8):
                                mm(pr, pr[:, 0:8], hf32[kc][:, tblk], wr[:, kc, :], [hf32[kc], wr], start=(kc == 0), stop=(kc == 7))
                            tt("dve", lg, lg[:], pr[:, 0:8], brb[:], ALU.add, [pr, brb])
                            P.op("dve", lambda: nc.vector.tensor_reduce(out=m1[:], in_=lg[:], axis=AX.X, op=ALU.max), reads=[lg], writes=[m1])
                            ts(selt, selt[:], lg[:], m1[:, 0:1], -1e30, ALU.is_ge, ALU.mult, [lg, m1])
                            tt("dve", lg2, lg2[:], lg[:], selt[:], ALU.add, [lg, selt])
                            P.op("dve", lambda: nc.vector.tensor_reduce(out=m2[:], in_=lg2[:], axis=AX.X, op=ALU.max), reads=[lg2], writes=[m2])
                            ts(selt, selt[:], lg[:], m2[:, 0:1], None, ALU.is_ge, None, [lg, m2])
                            ts(m1, m1[:], m1[:], -1.0, None, ALU.mult, None, [m1])
                            act(pe_, pe_[:], lg[:], AF.Exp, [lg, m1], bias=m1[:, 0:1], scale=1.0)
                            tt("dve", pe_, pe_[:], pe_[:], selt[:], ALU.mult, [pe_, selt])
                            P.op("dve", lambda: nc.vector.tensor_reduce(out=dsum[:], in_=pe_[:], axis=AX.X, op=ALU.add), reads=[pe_], writes=[dsum])
                            P.op("dve", lambda: nc.vector.reciprocal(out=dsum[:], in_=dsum[:]), reads=[dsum], writes=[dsum])
                            ts(pe_, pe_[:], pe_[:], dsum[:, 0:1], None, ALU.mult, None, [pe_, dsum])
                            pg = psf.next()
                            P.op("pe", lambda: nc.tensor.transpose(out=pg[0:8, 0:128], in_=pe_[:], identity=ident_f), reads=[pe_, consts], writes=[pg])
                            c0 = n * 512 + tb * 128
                            P.op("dve", lambda: nc.vector.tensor_copy(out=gTs[:, c0:c0 + 128], in_=pg[0:8, 0:128]), reads=[pg], writes=[gTs])
                        for e in range(NE):
                            pb = psf.next()
                            mm(pb, pb[:], consts[0:8, 256 + e * 128:256 + (e + 1) * 128], gTs[:, nb_], [consts, gTs])
                            act(gb[e], gb[e][:, nb_], pb[:], AF.Copy, [pb])

                stages = [(e, jg) for e in range(n_exp) for jg in range(NJG)]
                loaded = {}

                def load(k):
                    e, jg = stages[k]
                    wg = wg_ring.next()
                    wu = wu_ring.next()
                    wd = wd_ring.next()
                    if moe:
                        P.dma("pool", wg[:], exg_d[e, jg], writes=[wg])
                        P.dma("pool", wu[:], exu_d[e, jg], writes=[wu])
                        P.dma("pool", wd[:], exd_d[e, jg * JGW:(jg + 1) * JGW, :].rearrange("(c p) n -> p c n", p=128), writes=[wd])
                    else:
                        P.dma("pool", wg[:], ffg_d[jg], writes=[wg])
                        P.dma("pool", wu[:], ffu_d[jg], writes=[wu])
                        P.dma("pool", wd[:], ffd_d[jg * JGW:(jg + 1) * JGW, :].rearrange("(c p) n -> p c n", p=128), writes=[wd])
                    loaded[k] = (wg, wu, wd)

                load(0)
                if len(stages) > 1:
                    load(1)
                for k in range(len(stages)):
                    if k + 2 < len(stages):
                        load(k + 2)
                    e, jg = stages[k]
                    wg, wu, wd = loaded.pop(k)
                    acts = {}
                    for jj in range(2):
                        for n in range(NB):
                            nb_ = slice(n * 512, (n + 1) * 512)
                            pgp = psf.next()
                            for kc in range(8):
                                mm(pgp, pgp[:], wg[:, kc, jj * 128:(jj + 1) * 128], hF[kc][:, nb_], [wg, hF[kc]], start=(kc == 0), stop=(kc == 7))
                            pup = psf.next()
                            for kc in range(8):
                                mm(pup, pup[:], wu[:, kc, jj * 128:(jj + 1) * 128], hF[kc][:, nb_], [wu, hF[kc]], start=(kc == 0), stop=(kc == 7))
                            sg = sg_ring.next()
                            act(sg, sg[:], pgp[:], AF.Silu, [pgp])
                            ab = act_ring.next()
                            if moe:
                                sg2 = sg2_ring.next()
                                tt("dve", sg2, sg2[:], sg[:], pup[:], ALU.mult, [sg, pup])
                                tt("pool", ab, ab[:], sg2[:], gb[e][:, nb_], ALU.mult, [sg2, gb[e]])
                            else:
                                tt("dve", ab, ab[:], sg[:], pup[:], ALU.mult, [sg, pup])
                            acts[(jj, n)] = ab
                    for n in range(NB):
                        nb_ = slice(n * 512, (n + 1) * 512)
                        for dc in range(8):
                            py = psf.next()
                            for jj in range(2):
                                mm(py, py[:], wd[:, jj, dc * 128:(dc + 1) * 128], acts[(jj, n)][:], [wd, acts[(jj, n)]], start=(jj == 0), stop=(jj == 1))
                            stt(xF, xF[:, dc, nb_], py[:], Dv(l, 40 + dc), xF[:, dc, nb_], ALU.mult, ALU.add, [py, derived, xF])

                if final:
                    for n in range(NB):
                        nb_ = slice(n * 512, (n + 1) * 512)
                        rms_rstd(xF, lambda kc: xF[:, kc, nb_], 8, onesD, sq_ring, rstd)
                        for kc in range(8):
                            stt(xF, xF[:, kc, nb_], xF[:, kc, nb_], V("gfin", kc), rstd[:], ALU.mult, ALU.mult, [xF, vec, rstd])
                P.dma("sp", buf_v[:, :, T0:T0 + TT], xF[:], reads=[xF], writes=blocks)
            P.barrier()
        P.tes = es

    if prefix:
        for l in range(NL):
            mixer_phase(l, xpT_v if l == 0 else preT_v, None if l == 0 else dpre, preT_v, dpre,
                        st_in_d[l], None, st_pre_d[l], dstp[l], False)
            if l < NL - 1:
                ffn_phase(l, moe=(l in moe_layers), final=False, buf_v=preT_v, buf_blocks=dpre)
    for l in range(NL):
        if not _SKIP_MIX:
            if prefix:
                mixer_phase(l, xT_v if l == 0 else outT_v, None if l == 0 else dx, outT_v, dx,
                            st_pre_d[l], dstp[l], st_out_d[l], None, True)
            else:
                mixer_phase(l, xT_v if l == 0 else outT_v, None if l == 0 else dx, outT_v, dx,
                            st_in_d[l], None, st_out_d[l], None, False)
        if not _SKIP_FFN:
            ffn_phase(l, moe=(l in moe_layers), final=(l == NL - 1), buf_v=outT_v, buf_blocks=dx)
    P.barrier()
    es.close()
    return nc


_NC_CACHE = {}


def _prep_shared(inp, NL):
    sh = {}
    sh["w_ada"] = np.ascontiguousarray(inp["w_ada"], dtype=np.float32)
    sh["w_in"] = np.ascontiguousarray(inp["w_in"], dtype=np.float32)
    sh["w_out"] = np.ascontiguousarray(inp["w_out"], dtype=np.float32)
    sh["w_lru_a"] = np.ascontiguousarray(inp["w_lru_a"], dtype=np.float32)
    sh["w_lru_x"] = np.ascontiguousarray(inp["w_lru_x"], dtype=np.float32)

    def grp(w):
        return np.ascontiguousarray(w.reshape(8, 128, NJG, JGW).transpose(2, 1, 0, 3))

    sh["ffg"] = grp(np.asarray(inp["w_ff_gate"][0], np.float32))
    sh["ffu"] = grp(np.asarray(inp["w_ff_up"][0], np.float32))
    sh["ffd"] = np.ascontiguousarray(inp["w_ff_down"][0], dtype=np.float32)
    sh["w_router"] = np.ascontiguousarray(inp["w_router"][0], dtype=np.float32)
    sh["brb"] = np.ascontiguousarray(np.broadcast_to(np.asarray(inp["b_router"][0], np.float32)[None, :], (128, 8)))
    sh["exg"] = np.stack([grp(np.asarray(inp["w_exp_gate"][0, e], np.float32)) for e in range(NE)])
    sh["exu"] = np.stack([grp(np.asarray(inp["w_exp_up"][0, e], np.float32)) for e in range(NE)])
    sh["exd"] = np.ascontiguousarray(inp["w_exp_down"][0], dtype=np.float32)
    sh["gmixb"] = np.ascontiguousarray(np.broadcast_to(np.asarray(inp["g_mix_out"], np.float32)[:, None, :512], (NL, 128, 512)))
    consts = np.zeros((128, 256 + 1024), np.float32)
    consts[:, 0:128] = np.eye(128, dtype=np.float32)
    consts[:, 128:256] = np.triu(np.ones((128, 128), np.float32))
    for e in range(NE):
        consts[e, 256 + e * 128:256 + (e + 1) * 128] = 1.0
    sh["consts"] = consts
    return sh


def _vecs(inp, b, NL, flag=0.0):
    lay, NV = vec_layout(NL)
    v = np.zeros((128, NV), np.float32)

    def put(name, arr):
        o, n = lay[name]
        assert arr.shape == (128, n), (name, arr.shape, n)
        v[:, o:o + n] = arr

    put("cond", fm(inp["c"][b], 8))
    for l in range(NL):
        put(f"bada{l}", fm(inp["b_ada"][l], 48))
        put(f"gm{l}", fm(inp["g_norm_mix"][l], 8))
        put(f"gf{l}", fm(inp["g_norm_ffn"][l], 8))
        w = np.asarray(inp["w_conv_qk"][l], np.float32)
        put(f"wcqk{l}", np.ascontiguousarray(w.T.reshape(8, 128, 4).transpose(1, 0, 2).reshape(128, 32)))
        put(f"bcqk{l}", fm(inp["b_conv_qk"][l], 8))
        w = np.asarray(inp["w_conv_lru"][l], np.float32)
        put(f"wcl{l}", np.ascontiguousarray(w.T.reshape(4, 128, 4).transpose(1, 0, 2).reshape(128, 16)))
        put(f"bcl{l}", fm(inp["b_conv_lru"][l], 4))
        put(f"bla{l}", fm(inp["b_lru_a"][l], 4))
        put(f"blx{l}", fm(inp["b_lru_x"][l], 4))
        put(f"lam{l}", fm(inp["lru_lambda"][l], 4))
        put(f"gml{l}", fm(inp["g_mix_out"][l][512:], 4))
        bg = np.zeros((128, 2), np.float32)
        bg[0:4, 0] = inp["b_gates"][l][0:4]
        bg[0:4, 1] = inp["b_gates"][l][4:8]
        put(f"bg{l}", bg)
    put("gfin", fm(inp["g_final"], 8))
    put("flag", np.full((128, 1), flag, np.float32))
    return v


def kernel(**inputs):
    x = np.asarray(inputs["x"], np.float32)
    B, S, D = x.shape
    NL = int(np.asarray(inputs["w_in"]).shape[0])
    n_cores = 2 * B
    T = S // 2
    key = (T, NL)
    if key not in _NC_CACHE:
        _NC_CACHE[key] = build(T, NL)
    nc = _NC_CACHE[key]
    sh = _prep_shared(inputs, NL)
    zeros_st = np.zeros((NL, 128, SW), np.float32)
    zeros_x = np.zeros((D, T), np.float32)
    in_maps = []
    for cid in range(n_cores):
        b, half = cid // 2, cid % 2
        m = dict(sh)
        m["xT"] = np.ascontiguousarray(x[b, half * T:(half + 1) * T, :].T)
        m["xpT"] = np.ascontiguousarray(x[b, 0:T, :].T) if half == 1 else zeros_x
        m["vecs"] = _vecs(inputs, b, NL, flag=float(half))
        m["st_in"] = zeros_st
        in_maps.append(m)
    res = run_bass_kernel_spmd(nc, in_maps, core_ids=list(range(n_cores)))
    out = np.empty((B, S, D), np.float32)
    for cid in range(n_cores):
        b, half = cid // 2, cid % 2
        out[b, half * T:(half + 1) * T, :] = res.results[cid]["outT"].T
    return out
```

```python
import contextlib
import math
import numpy as np
import concourse.bass as bass
import concourse.mybir as mybir
from concourse.bass_utils import run_bass_kernel_spmd

F32 = mybir.dt.float32
BF16 = mybir.dt.bfloat16
ALU = mybir.AluOpType
AF = mybir.ActivationFunctionType
AX = mybir.AxisListType

D_MODEL = 1024
D_FF = 2816
NE = 8
EPS = 1e-6
D_IN = 3080
SW = 560
C_Q, C_K, C_V, C_O, C_IG, C_FG, C_XL, C_GL = 0, 512, 1024, 1536, 2048, 2052, 2056, 2568
JGW = 256
NJG = D_FF // JGW

SAME_ENGINE_SYNC = True
EPOCH = 16000
_SKIP_MIX = False
_DEBUG_MAP = None
_SKIP_FFN = False


class Tl:
    __slots__ = ("t", "w", "r", "name")

    def __init__(self, t, name):
        self.t = t
        self.w = None
        self.r = {}
        self.name = name

    def __getitem__(self, k):
        return self.t[k]


class Ring:
    def __init__(self, tiles):
        self.tiles = tiles
        self.i = 0

    def next(self):
        t = self.tiles[self.i % len(self.tiles)]
        self.i += 1
        return t


class Prog:
    def __init__(self, nc, es, nslots=40):
        self.nc = nc
        self.es = es
        self.tes = es
        self.eng = {"pe": nc.tensor, "act": nc.scalar, "dve": nc.vector, "pool": nc.gpsimd, "sp": nc.sync}
        self.sems = {}
        self.cnt = {e: 0 for e in self.eng}
        self.seen = {e: {} for e in self.eng}
        self.nslots = nslots
        self.slot_sem = [es.enter_context(nc.semaphore(f"dq{i}")) for i in range(nslots)]
        self.slot_cnt = [0] * nslots
        self.next_slot = 0
        self.uid = 0
        self.dbg = None

    def _esem(self, e, ep):
        k = (e, ep)
        if k not in self.sems:
            self.sems[k] = self.es.enter_context(self.nc.semaphore(f"s_{e}_{ep}"))
        return self.sems[k]

    def sb(self, shape, dtype, name=None):
        self.uid += 1
        name = f"{name or 't'}_{self.uid}"
        return Tl(self.tes.enter_context(self.nc.sbuf_tensor(name, list(shape), dtype)), name)

    def ps(self, shape, dtype, name=None):
        self.uid += 1
        name = f"{name or 'p'}_{self.uid}"
        return Tl(self.tes.enter_context(self.nc.psum_tensor(name, list(shape), dtype)), name)

    def pseudo(self, name):
        return Tl(None, name)

    def _wait(self, e, dep):
        kind, key, val = dep
        k = (kind, key)
        if self.seen[e].get(k, 0) >= val:
            return
        sem = self._esem(key[0], key[1]) if kind == "eng" else self.slot_sem[key]
        self.eng[e].wait_ge(sem, val)
        self.seen[e][k] = val

    def _deps(self, e, reads, writes, force_same=False):
        deps = []
        for t in reads:
            if t.w is not None:
                deps.append(t.w)
        for t in writes:
            if t.w is not None:
                deps.append(t.w)
            for k, v in t.r.items():
                deps.append((k[0], k[1], v))
        for d in deps:
            if d[0] == "eng" and d[1][0] == e and not force_same and (e == "pe" or not SAME_ENGINE_SYNC):
                continue
            self._wait(e, d)

    def _mark(self, me, reads, writes):
        k = (me[0], me[1])
        for t in reads:
            if t.r.get(k, 0) < me[2]:
                t.r[k] = me[2]
        for t in writes:
            t.w = me
            t.r = {}

    def _cur(self, e):
        c = self.cnt[e]
        ep = (c - 1) // EPOCH
        return ("eng", (e, ep), c - ep * EPOCH)

    def op(self, e, fn, reads=(), writes=()):
        self._deps(e, reads, writes)
        ins = fn()
        if self.dbg is not None:
            import sys as _sys
            f = _sys._getframe(1)
            chain = []
            while f is not None and len(chain) < 4:
                if f.f_code.co_name not in ("mm", "act", "tt", "ts", "stt"):
                    chain.append(f.f_lineno)
                f = f.f_back
            try:
                self.dbg[ins.ins.name] = chain
            except Exception:
                pass
        self.cnt[e] += 1
        me = self._cur(e)
        ins.then_inc(self._esem(e, me[1][1]), 1)
        self._mark(me, reads, writes)
        return me

    def dma(self, q, out, in_, reads=(), writes=(), slow=False):
        slot = self.next_slot
        self.next_slot = (slot + 1) % self.nslots
        if self.slot_cnt[slot] > 0:
            self._wait(q, ("dma", slot, 16 * self.slot_cnt[slot]))
        self._deps(q, reads, writes, force_same=True)
        if slow:
            ins = self.eng[q].dma_start(out=out, in_=in_, allow_slow_non_contiguous=True)
        else:
            ins = self.eng[q].dma_start(out=out, in_=in_)
        self.slot_cnt[slot] += 1
        ins.then_inc(self.slot_sem[slot], 16)
        me = ("dma", slot, 16 * self.slot_cnt[slot])
        self._mark(me, reads, writes)
        return me

    def barrier(self):
        deps = []
        for e in self.eng:
            if self.cnt[e] > 0:
                deps.append(self._cur(e))
        for s in range(self.nslots):
            if self.slot_cnt[s] > 0:
                deps.append(("dma", s, 16 * self.slot_cnt[s]))
        for e in self.eng:
            for d in deps:
                self._wait(e, d)


def vec_layout(NL):
    lay = {}
    off = 0

    def add(name, n):
        nonlocal off
        lay[name] = (off, n)
        off += n

    add("cond", 8)
    for l in range(NL):
        add(f"bada{l}", 48)
        add(f"gm{l}", 8)
        add(f"gf{l}", 8)
        add(f"wcqk{l}", 32)
        add(f"bcqk{l}", 8)
        add(f"wcl{l}", 16)
        add(f"bcl{l}", 4)
        add(f"bla{l}", 4)
        add(f"blx{l}", 4)
        add(f"lam{l}", 4)
        add(f"gml{l}", 4)
        add(f"bg{l}", 2)
    add("gfin", 8)
    add("flag", 1)
    return lay, off


def fm(v, nch):
    return np.ascontiguousarray(np.asarray(v, np.float32).reshape(nch, 128).T)


def build(T, NL, moe_layers=(1,), prefix=True):
    NSUB = T // 512
    TT = min(1024, T)
    NTT = T // TT
    NB = TT // 512
    lay, NV = vec_layout(NL)

    nc = bass.Bass("TRN2", target_bir_lowering=False)
    DT = nc.dram_tensor
    xT = DT("xT", [1024, T], F32, kind="ExternalInput").ap()
    xpT = DT("xpT", [1024, T], F32, kind="ExternalInput").ap()
    preT = DT("preT", [1024, T], F32, kind="Internal").ap()
    st_pre_d = DT("st_pre", [NL, 128, SW], F32, kind="Internal").ap()
    vecs_d = DT("vecs", [128, NV], F32, kind="ExternalInput").ap()
    consts_d = DT("consts", [128, 256 + 1024], F32, kind="ExternalInput").ap()
    gmixb_d = DT("gmixb", [NL, 128, 512], F32, kind="ExternalInput").ap()
    brb_d = DT("brb", [128, 8], F32, kind="ExternalInput").ap()
    w_ada_d = DT("w_ada", [NL, 1024, 6144], F32, kind="ExternalInput").ap()
    w_in_d = DT("w_in", [NL, 1024, D_IN], F32, kind="ExternalInput").ap()
    w_out_d = DT("w_out", [NL, 1024, 1024], F32, kind="ExternalInput").ap()
    w_la_d = DT("w_lru_a", [NL, 8, 64, 64], F32, kind="ExternalInput").ap()
    w_lx_d = DT("w_lru_x", [NL, 8, 64, 64], F32, kind="ExternalInput").ap()
    ffg_d = DT("ffg", [NJG, 128, 8, JGW], F32, kind="ExternalInput").ap()
    ffu_d = DT("ffu", [NJG, 128, 8, JGW], F32, kind="ExternalInput").ap()
    ffd_d = DT("ffd", [D_FF, 1024], F32, kind="ExternalInput").ap()
    w_r_d = DT("w_router", [1024, 8], F32, kind="ExternalInput").ap()
    exg_d = DT("exg", [NE, NJG, 128, 8, JGW], F32, kind="ExternalInput").ap()
    exu_d = DT("exu", [NE, NJG, 128, 8, JGW], F32, kind="ExternalInput").ap()
    exd_d = DT("exd", [NE, D_FF, 1024], F32, kind="ExternalInput").ap()
    st_in_d = DT("st_in", [NL, 128, SW], F32, kind="ExternalInput").ap()
    outT = DT("outT", [1024, T], F32, kind="ExternalOutput").ap()
    st_out_d = DT("st_out", [NL, 128, SW], F32, kind="ExternalOutput").ap()

    xT_v = xT.rearrange("(kc p) t -> p kc t", p=128)
    xpT_v = xpT.rearrange("(kc p) t -> p kc t", p=128)
    preT_v = preT.rearrange("(kc p) t -> p kc t", p=128)
    outT_v = outT.rearrange("(kc p) t -> p kc t", p=128)

    es = contextlib.ExitStack()
    P = Prog(nc, es)
    if _DEBUG_MAP is not None:
        P.dbg = _DEBUG_MAP
    dx = [P.pseudo(f"dx{s}") for s in range(NSUB)]
    dpre = [P.pseudo(f"dpre{s}") for s in range(NSUB)]
    dstp = [P.pseudo(f"dstp{l}") for l in range(NL)]

    consts = P.sb([128, 256 + 1024], F32, "consts")
    vec = P.sb([128, NV], F32, "vec")
    P.dma("sp", consts[:], consts_d, writes=[consts])
    P.dma("sp", vec[:], vecs_d, writes=[vec])
    ident_f = consts[:, 0:128]
    mask_f = consts[:, 128:256]
    ident_b = P.sb([128, 128], BF16, "identb")
    P.op("dve", lambda: nc.vector.tensor_copy(out=ident_b[:], in_=ident_f), reads=[consts], writes=[ident_b])
    onesD = P.sb([128, 128], F32, "onesD")
    onesL = P.sb([128, 128], F32, "onesL")
    ones1 = P.sb([128, 512], F32, "ones1")
    epsc = P.sb([128, 1], F32, "epsc")
    P.op("dve", lambda: nc.vector.memset(onesD[:], 1.0 / 1024), writes=[onesD])
    P.op("dve", lambda: nc.vector.memset(onesL[:], 1.0 / 512), writes=[onesL])
    P.op("dve", lambda: nc.vector.memset(ones1[:], 1.0), writes=[ones1])
    P.op("dve", lambda: nc.vector.memset(epsc[:], EPS), writes=[epsc])

    def V(name, j=0, n=1):
        o, _ = lay[name]
        return vec[:, o + j:o + j + n]

    psf = Ring([P.ps([128, 512], F32, "psf") for _ in range(6)])
    psb = Ring([P.ps([128, 1024], BF16, "psb") for _ in range(2)])

    def mm(o_t, o_ap, l_ap, r_ap, reads, start=True, stop=True):
        P.op("pe", lambda: nc.tensor.matmul(o_ap, l_ap, r_ap, start=start, stop=stop), reads=reads, writes=[o_t])

    def act(o_t, o_ap, i_ap, func, reads, scale=None, bias=None, accum=None, extra_w=()):
        kw = {}
        if scale is not None:
            kw["scale"] = scale
        if bias is not None:
            kw["bias"] = bias
        if accum is not None:
            kw["accum_out"] = accum
        P.op("act", lambda: nc.scalar.activation(out=o_ap, in_=i_ap, func=func, **kw), reads=reads,
             writes=[o_t] + list(extra_w))

    def tt(e, o_t, o_ap, a_ap, b_ap, op, reads):
        eng = nc.vector if e == "dve" else nc.gpsimd
        P.op(e, lambda: eng.tensor_tensor(out=o_ap, in0=a_ap, in1=b_ap, op=op), reads=reads, writes=[o_t])

    def ts(o_t, o_ap, a_ap, s1, s2, op0, op1, reads, e="dve"):
        eng = nc.vector if e == "dve" else nc.gpsimd
        if op1 is None:
            P.op(e, lambda: eng.tensor_scalar(out=o_ap, in0=a_ap, scalar1=s1, scalar2=None, op0=op0), reads=reads, writes=[o_t])
        else:
            P.op(e, lambda: eng.tensor_scalar(out=o_ap, in0=a_ap, scalar1=s1, scalar2=s2, op0=op0, op1=op1), reads=reads, writes=[o_t])

    def stt(o_t, o_ap, a_ap, s, b_ap, op0, op1, reads):
        P.op("dve", lambda: nc.vector.scalar_tensor_tensor(out=o_ap, in0=a_ap, scalar=s, in1=b_ap, op0=op0, op1=op1),
             reads=reads, writes=[o_t])

    mod = P.sb([128, NL * 48], F32, "mod")
    derived = P.sb([128, NL * 64], F32, "derived")
    with contextlib.ExitStack() as es2:
        P.tes = es2
        cond = P.sb([128, 8], F32, "cond")
        act(cond, cond[:], V("cond", 0, 8), AF.Silu, [vec])
        wa_ring = Ring([P.sb([128, 8, 1024], F32, "wada") for _ in range(2)])
        pmod = psf.next()
        for l in range(NL):
            for sl in range(6):
                wa = wa_ring.next()
                P.dma("sp", wa[:], w_ada_d[l].rearrange("(kc p) n -> p kc n", p=128)[:, :, sl * 1024:(sl + 1) * 1024], writes=[wa])
                for j in range(8):
                    col = l * 48 + sl * 8 + j
                    for kc in range(8):
                        mm(pmod, pmod[:, col:col + 1], wa[:, kc, j * 128:(j + 1) * 128], cond[:, kc:kc + 1], [wa, cond],
                           start=(kc == 0), stop=(kc == 7))
        for l in range(NL):
            o = lay[f"bada{l}"][0]
            tt("dve", mod, mod[:, l * 48:(l + 1) * 48], pmod[:, l * 48:(l + 1) * 48], vec[:, o:o + 48], ALU.add, [pmod, vec])
            m0 = l * 48
            d0 = l * 64
            stt(derived, derived[:, d0:d0 + 8], mod[:, m0 + 8:m0 + 16], 1.0, V(f"gm{l}", 0, 8), ALU.add, ALU.mult, [mod, vec])
            P.op("dve", lambda: nc.vector.tensor_copy(out=derived[:, d0 + 8:d0 + 16], in_=mod[:, m0:m0 + 8]), reads=[mod], writes=[derived])
            P.op("dve", lambda: nc.vector.tensor_copy(out=derived[:, d0 + 16:d0 + 24], in_=mod[:, m0 + 16:m0 + 24]), reads=[mod], writes=[derived])
            stt(derived, derived[:, d0 + 24:d0 + 32], mod[:, m0 + 32:m0 + 40], 1.0, V(f"gf{l}", 0, 8), ALU.add, ALU.mult, [mod, vec])
            P.op("dve", lambda: nc.vector.tensor_copy(out=derived[:, d0 + 32:d0 + 40], in_=mod[:, m0 + 24:m0 + 32]), reads=[mod], writes=[derived])
            P.op("dve", lambda: nc.vector.tensor_copy(out=derived[:, d0 + 40:d0 + 48], in_=mod[:, m0 + 40:m0 + 48]), reads=[mod], writes=[derived])
            tmp4 = P.sb([128, 4], F32, "tmp4")
            act(tmp4, tmp4[:], V(f"lam{l}", 0, 4), AF.Exp, [vec], scale=-1.0)
            act(tmp4, tmp4[:], tmp4[:], AF.Ln, [tmp4], bias=1.0)
            ts(derived, derived[:, d0 + 48:d0 + 52], tmp4[:], -8.0, None, ALU.mult, None, [tmp4])
            ts(derived, derived[:, d0 + 52:d0 + 56], tmp4[:], -16.0, None, ALU.mult, None, [tmp4])
            ts(derived, derived[:, d0 + 56:d0 + 57], V(f"bg{l}", 1, 1), -1.0, None, ALU.mult, None, [vec])
        P.barrier()
    P.tes = es

    def Dv(l, j, n=1):
        return derived[:, l * 64 + j:l * 64 + j + n]

    def rms_rstd(x_t, x_ap_of_kc, nk, ones_t, sq_ring, rstd_t, reads_extra=()):
        ss = psf.next()
        for kc in range(nk):
            sq = sq_ring.next()
            act(sq, sq[:], x_ap_of_kc(kc), AF.Square, ([x_t] if x_t is not None else []) + list(reads_extra))
            mm(ss, ss[:], ones_t[:], sq[:], [ones_t, sq], start=(kc == 0), stop=(kc == nk - 1))
        act(rstd_t, rstd_t[:], ss[:], AF.Sqrt, [ss, epsc], bias=epsc[:, 0:1], scale=1.0)
        P.op("dve", lambda: nc.vector.reciprocal(out=rstd_t[:], in_=rstd_t[:]), reads=[rstd_t], writes=[rstd_t])

    def mixer_phase(l, src_v, src_blocks, dst_v, dst_blocks, st_src, st_src_tile, st_dst, st_dst_tile, use_flag, so=False):
        with contextlib.ExitStack() as es2:
            P.tes = es2
            win = [P.sb([128, 2, D_IN], BF16, "win") for _ in range(4)]
            wout = [P.sb([128, 4, 1024], BF16, "wout") for _ in range(2)]
            w_in_v = w_in_d[l].rearrange("(kc p) n -> p kc n", p=128)
            w_out_v = w_out_d[l].rearrange("(kc p) n -> p kc n", p=128)
            for i in range(4):
                P.dma("pool", win[i][:], w_in_v[:, 2 * i:2 * i + 2, :], writes=[win[i]])
            for i in range(2):
                P.dma("pool", wout[i][:], w_out_v[:, 4 * i:4 * i + 4, :], writes=[wout[i]])
            bd_a = P.sb([128, 4, 128], BF16, "bda")
            bd_x = P.sb([128, 4, 128], BF16, "bdx")
            for bd, wd in ((bd_a, w_la_d), (bd_x, w_lx_d)):
                P.op("dve", lambda: nc.vector.memset(bd[:], 0.0), writes=[bd])
                wv = wd[l].rearrange("(c two) k m -> two k c m", two=2)
                P.dma("pool", bd[0:64, :, 0:64], wv[0], writes=[bd])
                P.dma("pool", bd[64:128, :, 64:128], wv[1], reads=[bd], writes=[bd])
            gmb = P.sb([128, 512], F32, "gmb")
            P.dma("sp", gmb[:], gmixb_d[l], writes=[gmb])

            def wcol(kc, c0, c1):
                return win[kc // 2][:, kc % 2, c0:c1]

            Cst = P.sb([128, 4, 129], F32, "Cst")
            Cbf = P.sb([128, 4, 129], BF16, "Cbf")
            lru_h = P.sb([128, 4], F32, "lruh")
            gcar = P.sb([4, 2], F32, "gcar")
            tails = P.sb([128, 12, 3], F32, "tails")
            cbuf = Ring([P.sb([128, 515], F32, "cbuf") for _ in range(3)])
            sti = st_src
            srd = [st_src_tile] if st_src_tile is not None else []
            P.dma("sp", Cst[:].rearrange("p h j -> p (h j)"), sti[:, 0:516], reads=srd, writes=[Cst])
            P.dma("sp", lru_h[:], sti[:, 516:520], reads=srd, writes=[lru_h])
            P.dma("sp", tails[:].rearrange("p c j -> p (c j)"), sti[:, 520:556], reads=srd, writes=[tails])
            P.op("dve", lambda: nc.vector.memset(gcar[:], 0.0), writes=[gcar])
            P.dma("sp", gcar[0:4, 1:2], sti[0:4, 556:557], reads=[gcar] + srd, writes=[gcar], slow=True)
            if use_flag:
                fo = lay["flag"][0]
                ts(Cst, Cst[:].rearrange("p h j -> p (h j)"), Cst[:].rearrange("p h j -> p (h j)"), vec[:, fo:fo + 1], None, ALU.mult, None, [Cst, vec])
                ts(lru_h, lru_h[:], lru_h[:], vec[:, fo:fo + 1], None, ALU.mult, None, [lru_h, vec])
                ts(tails, tails[:].rearrange("p c j -> p (c j)"), tails[:].rearrange("p c j -> p (c j)"), vec[:, fo:fo + 1], None, ALU.mult, None, [tails, vec])
                ts(gcar, gcar[0:4, 1:2], gcar[0:4, 1:2], vec[0:4, fo:fo + 1], None, ALU.mult, None, [gcar, vec])

            xring = Ring([P.sb([128, 8, 512], F32, "xt") for _ in range(1)])
            hT = [P.sb([128, 512], BF16, "hT") for _ in range(8)]
            tmp_ring = Ring([P.sb([128, 512], F32, "tmp") for _ in range(3)])
            sq_ring = tmp_ring
            rstd = P.sb([128, 512], F32, "rstd")
            qk_bf = [P.sb([128, 512], BF16, "qkbf") for _ in range(8)]
            acc_ring = Ring([P.sb([128, 512], F32, "acc") for _ in range(2)])
            vaug = [P.sb([128, 4, 129], BF16, "vaug") for _ in range(4)]
            for tb in range(4):
                P.op("dve", lambda: nc.vector.memset(vaug[tb][:], 1.0), writes=[vaug[tb]])
            go = [P.sb([128, 512], F32, "go") for _ in range(4)]
            nb = P.sb([4, 512], F32, "nb")
            wv_t = P.sb([4, 512], F32, "wv")
            mu = P.sb([4, 512], F32, "mu")
            e1 = P.sb([4, 512], F32, "e1")
            fl = P.sb([4, 512], F32, "fl")
            g_e = e1
            g_l = fl
            rp = P.sb([4, 4], F32, "rp")
            a_t = P.sb([4, 4], F32, "a_t")
            adiag = P.sb([4, 16], F32, "adiag")
            abc = P.sb([128, 16], F32, "abc")
            tsc = [P.sb([128, 8], F32, "tsc") for _ in range(4)]
            ke_ring = Ring([P.sb([128, 4, 128], BF16, "ke") for _ in range(2)])
            stm_ring = Ring([P.sb([128, 4, 128], BF16, "stm") for _ in range(2)])
            den_ring = Ring([P.sb([128, 4], F32, "den") for _ in range(2)])
            hm4_ring = Ring([P.sb([128, 512], F32, "hm4") for _ in range(3)])
            junk = P.sb([128, 128], F32, "junk")
            ssq_ring = Ring([P.sb([128, 4], F32, "ssq") for _ in range(2)])
            hmn_ring = Ring([P.sb([128, 512], BF16, "hmn") for _ in range(1)])
            hmix_m = P.sb([128, 4, 512], BF16, "hmixm")
            hmix_l = [P.sb([128, 512], BF16, "hmixl") for _ in range(4)]
            xcb_ring = Ring([P.sb([128, 512], BF16, "xcb") for _ in range(2)])
            lt = [P.sb([128, 512], F32, "lt") for _ in range(6)]
            hl = [P.sb([128, 512], F32, "hl") for _ in range(4)]
            rstl = P.sb([128, 512], F32, "rstl")

            for s in range(NSUB):
                t0 = s * 512
                xt = xring.next()
                P.dma("sp", xt[:], src_v[:, :, t0:t0 + 512], reads=([src_blocks[s]] if src_blocks is not None else []), writes=[xt])
                rms_rstd(xt, lambda kc: xt[:, kc, :], 8, onesD, sq_ring, rstd)
                for kc in range(8):
                    tmp = tmp_ring.next()
                    tt("dve", tmp, tmp[:], xt[:, kc, :], rstd[:], ALU.mult, [xt, rstd])
                    act(hT[kc], hT[kc][:], tmp[:], AF.Identity, [tmp, derived], scale=Dv(l, 0 + kc), bias=Dv(l, 8 + kc))

                def proj_fm(ps, M, col0):
                    for kc in range(8):
                        mm(ps, ps[0:M, :], wcol(kc, col0, col0 + M), hT[kc][:], [win[kc // 2], hT[kc]],
                           start=(kc == 0), stop=(kc == 7))

                def conv(ps, ci, wname, bname, c, acc):
                    buf = cbuf.next()
                    act(buf, buf[:, 3:515], ps[:], AF.Copy, [ps])
                    P.op("dve", lambda: nc.vector.tensor_copy(out=buf[:, 0:3], in_=tails[:, ci, :]), reads=[tails, buf], writes=[buf])
                    wo = lay[wname][0] + 4 * c
                    bo = lay[bname][0] + c
                    ts(acc, acc[:], buf[:, 3:515], vec[:, wo + 3:wo + 4], vec[:, bo:bo + 1], ALU.mult, ALU.add, [buf, vec])
                    for j in (2, 1, 0):
                        stt(acc, acc[:], buf[:, j:j + 512], vec[:, wo + j:wo + j + 1], acc[:], ALU.mult, ALU.add, [buf, vec, acc])
                    P.op("dve", lambda: nc.vector.tensor_copy(out=tails[:, ci, :], in_=buf[:, 512:515]), reads=[buf, tails], writes=[tails])

                def sec_qk():
                    for c in range(8):
                        if so and c < 4:
                            if s == NSUB - 1:
                                ps = psf.tiles[c % 2]
                                proj_fm(ps, 128, C_Q + c * 128)
                                P.op("dve", lambda: nc.vector.tensor_copy(out=tails[:, c, :], in_=ps[:, 509:512]), reads=[ps, tails], writes=[tails])
                            continue
                        ps = psf.tiles[c % 2]
                        proj_fm(ps, 128, C_Q + c * 128)
                        acc = acc_ring.next()
                        conv(ps, c, f"wcqk{l}", f"bcqk{l}", c, acc)
                        act(qk_bf[c], qk_bf[c][:], acc[:], AF.Silu, [acc])
                        yield


                    yield
                def sec_gates():
                    psi = psf.tiles[2]
                    proj_fm(psi, 4, C_IG)
                    yield
                    psg = psf.tiles[3]
                    proj_fm(psg, 4, C_FG)
                    yield
                    act(g_e, g_e[:], psg[0:4, :], AF.Exp, [psg, derived], scale=-1.0, bias=derived[0:4, l * 64 + 56:l * 64 + 57])
                    yield
                    act(g_l, g_l[:], g_e[:], AF.Ln, [g_e], bias=1.0)
                    yield
                    P.op("dve", lambda: nc.vector.tensor_tensor_scan(out=nb[:], data0=ones1[0:4, :], data1=g_l[:], initial=gcar[0:4, 0:1],
                                                                     op0=ALU.mult, op1=ALU.add), reads=[ones1, g_l, gcar], writes=[nb])
                    bgo = lay[f"bg{l}"][0]
                    stt(wv_t, wv_t[:], psi[0:4, :], vec[0:4, bgo:bgo + 1], nb[:], ALU.add, ALU.add, [psi, vec, nb])
                    yield
                    P.op("dve", lambda: nc.vector.tensor_tensor_scan(out=mu[:], data0=wv_t[:], data1=wv_t[:], initial=gcar[0:4, 1:2],
                                                                     op0=ALU.max, op1=ALU.max), reads=[wv_t, gcar], writes=[mu])
                    mu_v = mu[:].rearrange("p (c t) -> p c t", t=128)
                    yield
                    Rb = mu_v[:, :, 127:128].to_broadcast([4, 4, 128])
                    yield
                    P.op("dve", lambda: nc.vector.tensor_copy(out=rp[:, 0:1], in_=gcar[0:4, 1:2]), reads=[gcar], writes=[rp])
                    yield
                    P.op("dve", lambda: nc.vector.tensor_copy(out=rp[:, 1:4].unsqueeze(2), in_=mu_v[:, 0:3, 127:128]), reads=[mu, rp], writes=[rp])
                    yield
                    tt("dve", a_t, a_t[:].unsqueeze(2), rp[:].unsqueeze(2), mu_v[:, :, 127:128], ALU.subtract, [rp, mu])
                    yield
                    act(a_t, a_t[:], a_t[:], AF.Exp, [a_t])
                    yield
                    tt("dve", adiag, adiag[:].rearrange("p (c h) -> p c h", h=4), a_t[:].unsqueeze(2).to_broadcast([4, 4, 4]),
                       consts[0:4, 0:4].unsqueeze(1).to_broadcast([4, 4, 4]), ALU.mult, [a_t, consts])
                    pab = psf.tiles[2]
                    mm(pab, pab[:, 0:16], ones1[0:4, 0:128], adiag[:], [ones1, adiag])
                    yield
                    P.op("dve", lambda: nc.vector.tensor_copy(out=abc[:], in_=pab[:, 0:16]), reads=[pab], writes=[abc])
                    yield
                    tt("dve", e1, e1[:].rearrange("p (c t) -> p c t", t=128), wv_t[:].rearrange("p (c t) -> p c t", t=128), Rb, ALU.subtract, [wv_t, mu])
                    yield
                    ts(e1, e1[:], e1[:], -0.5 * math.log(128.0), None, ALU.add, None, [e1])
                    yield
                    act(e1, e1[:], e1[:], AF.Exp, [e1])
                    yield
                    tt("dve", fl, fl[:].rearrange("p (c t) -> p c t", t=128), nb[:].rearrange("p (c t) -> p c t", t=128), Rb, ALU.subtract, [nb, mu])
                    yield
                    act(fl, fl[:], fl[:], AF.Exp, [fl])
                    yield
                    P.op("dve", lambda: nc.vector.tensor_copy(out=gcar[0:4, 0:1], in_=nb[:, 511:512]), reads=[nb, gcar], writes=[gcar])
                    yield
                    P.op("dve", lambda: nc.vector.tensor_copy(out=gcar[0:4, 1:2], in_=mu[:, 511:512]), reads=[mu, gcar], writes=[gcar])
                    yield
                    for tb in range(4):
                        pt = psf.tiles[3]
                        P.op("pe", lambda: nc.tensor.transpose(out=pt[:, 0:4], in_=e1[0:4, tb * 128:(tb + 1) * 128], identity=consts[0:4, 0:4]),
                             reads=[e1, consts], writes=[pt])
                        P.op("pe", lambda: nc.tensor.transpose(out=pt[:, 4:8], in_=fl[0:4, tb * 128:(tb + 1) * 128], identity=consts[0:4, 0:4]),
                             reads=[fl, consts], writes=[pt])
                        P.op("dve", lambda: nc.vector.tensor_copy(out=tsc[tb][:], in_=pt[:, 0:8]), reads=[pt], writes=[tsc[tb]])


                    yield
                def sec_vo():
                    for tb in range(4):
                        blk = slice(tb * 128, (tb + 1) * 128)
                        psv = psf.tiles[4]
                        for kc in range(8):
                            mm(psv, psv[:], hT[kc][:, blk], wcol(kc, C_V, C_V + 512), [hT[kc], win[kc // 2]], start=(kc == 0), stop=(kc == 7))
                        act(vaug[tb], vaug[tb][:, :, 0:128], psv[:].rearrange("p (h d) -> p h d", d=128), AF.Copy, [psv])
                        if so:
                            yield
                            continue
                        pso = psf.tiles[5]
                        for kc in range(8):
                            mm(pso, pso[:], hT[kc][:, blk], wcol(kc, C_O, C_O + 512), [hT[kc], win[kc // 2]], start=(kc == 0), stop=(kc == 7))
                        act(go[tb], go[tb][:], pso[:], AF.Sigmoid, [pso])
                        tt("pool", go[tb], go[tb][:], go[tb][:], gmb[:], ALU.mult, [go[tb], gmb])
                        yield


                    yield
                gens = [sec_qk(), sec_gates(), sec_vo()]
                while gens:
                    for g_ in list(gens):
                        try:
                            next(g_)
                        except StopIteration:
                            gens.remove(g_)

                def lru_chunk(c):
                    ps = psf.tiles[3]
                    proj_fm(ps, 128, C_XL + c * 128)
                    yield
                    xc, r_t, a_, a2, i_t, hs = lt
                    gel = r_t
                    conv(ps, 8 + c, f"wcl{l}", f"bcl{l}", c, xc)
                    yield
                    xcb = xcb_ring.next()
                    yield
                    act(xcb, xcb[:], xc[:], AF.Copy, [xc])
                    yield
                    pa = psf.tiles[4]
                    yield
                    mm(pa, pa[:], bd_a[:, c, :], xcb[:], [bd_a, xcb])
                    yield
                    px = psf.tiles[5]
                    yield
                    mm(px, px[:], bd_x[:, c, :], xcb[:], [bd_x, xcb])
                    yield
                    act(r_t, r_t[:], pa[:], AF.Sigmoid, [pa, vec], bias=V(f"bla{l}", c))
                    yield
                    act(a_, a_[:], r_t[:], AF.Exp, [r_t, derived], scale=Dv(l, 48 + c))
                    yield
                    act(a2, a2[:], r_t[:], AF.Exp, [r_t, derived], scale=Dv(l, 52 + c))
                    yield
                    ts(a2, a2[:], a2[:], -1.0, 1.0, ALU.mult, ALU.add, [a2])
                    yield
                    ts(a2, a2[:], a2[:], 1e-30, None, ALU.max, None, [a2])
                    yield
                    act(a2, a2[:], a2[:], AF.Sqrt, [a2])
                    yield
                    act(i_t, i_t[:], px[:], AF.Sigmoid, [px, vec], bias=V(f"blx{l}", c))
                    yield
                    tt("pool", i_t, i_t[:], i_t[:], xc[:], ALU.mult, [i_t, xc])
                    yield
                    tt("pool", i_t, i_t[:], i_t[:], a2[:], ALU.mult, [i_t, a2])
                    yield
                    P.op("dve", lambda: nc.vector.tensor_tensor_scan(out=hs[:], data0=a_[:], data1=i_t[:], initial=lru_h[:, c:c + 1],
                                                                     op0=ALU.mult, op1=ALU.add), reads=[a_, i_t, lru_h], writes=[hs])
                    P.op("dve", lambda: nc.vector.tensor_copy(out=lru_h[:, c:c + 1], in_=hs[:, 511:512]), reads=[hs, lru_h], writes=[lru_h])
                    yield
                    if so:
                        return
                    psl = psf.tiles[3]
                    yield
                    proj_fm(psl, 128, C_GL + c * 128)
                    yield
                    act(gel, gel[:], psl[:], AF.Gelu_apprx_tanh, [psl])
                    yield
                    tt("pool", hl[c], hl[c][:], hs[:], gel[:], ALU.mult, [hs, gel])
                    yield

                def mlstm_tb(tb):
                    blk = slice(tb * 128, (tb + 1) * 128)
                    yield
                    e1b = tsc[tb][:, 0:4].unsqueeze(2).to_broadcast([128, 4, 128])
                    yield
                    pk = psb.next()
                    yield
                    for h in range(4):
                        P.op("pe", lambda: nc.tensor.transpose(out=pk[:, h * 128:(h + 1) * 128], in_=qk_bf[4 + h][:, blk], identity=ident_b[:]),
                             reads=[qk_bf[4 + h], ident_b], writes=[pk])
                    ke4 = ke_ring.next()
                    yield
                    tt("dve", ke4, ke4[:], pk[:, 0:512].rearrange("p (h d) -> p h d", d=128), e1b, ALU.mult, [pk, tsc[tb]])
                    yield
                    if not so:
                        pS = psf.tiles[0]
                        yield
                        for h in range(4):
                            mm(pS, pS[:, h * 128:(h + 1) * 128], qk_bf[4 + h][:, blk], qk_bf[h][:, blk], [qk_bf[4 + h], qk_bf[h]])
                        stmp = hm4_ring.next()
                        yield
                        tt("dve", stmp, stmp[:].rearrange("p (h d) -> p h d", d=128), pS[:].rearrange("p (h d) -> p h d", d=128), e1b, ALU.mult, [pS, tsc[tb]])
                        yield
                        stm4 = stm_ring.next()
                        yield
                        tt("dve", stm4, stm4[:], stmp[:].rearrange("p (h d) -> p h d", d=128),
                           mask_f.unsqueeze(1).to_broadcast([128, 4, 128]), ALU.mult, [stmp, consts])
                    tt("dve", Cst, Cst[:], Cst[:], abc[:, tb * 4:(tb + 1) * 4].unsqueeze(2).to_broadcast([128, 4, 129]), ALU.mult, [Cst, abc])
                    yield
                    if not so:
                        act(Cbf, Cbf[:], Cst[:], AF.Copy, [Cst])
                        yield
                        pOn = psf.tiles[1]
                        yield
                        pOd = psf.tiles[2]
                        yield
                        for h in range(4):
                            mm(pOn, pOn[:, h * 128:(h + 1) * 128], stm4[:, h, :], vaug[tb][:, h, 0:128], [stm4, vaug[tb]], start=True, stop=False)
                            mm(pOn, pOn[:, h * 128:(h + 1) * 128], qk_bf[h][:, blk], Cbf[:, h, 0:128], [qk_bf[h], Cbf], start=False, stop=True)
                        for h in range(4):
                            mm(pOd, pOd[:, 384 + h:385 + h], stm4[:, h, :], vaug[tb][:, h, 128:129], [stm4, vaug[tb]], start=True, stop=False)
                            mm(pOd, pOd[:, 384 + h:385 + h], qk_bf[h][:, blk], Cbf[:, h, 128:129], [qk_bf[h], Cbf], start=False, stop=True)
                    pU1 = psf.tiles[0]
                    yield
                    pU2 = psf.tiles[2]
                    yield
                    for h in range(4):
                        pu = pU1 if h < 2 else pU2
                        o_ = (h % 2) * 129
                        mm(pu, pu[:, o_:o_ + 129], ke4[:, h, :], vaug[tb][:, h, :], [ke4, vaug[tb]])
                    tt("dve", Cst, Cst[:, 0:2, :], Cst[:, 0:2, :], pU1[:, 0:258].rearrange("p (h j) -> p h j", j=129), ALU.add, [Cst, pU1])
                    yield
                    tt("dve", Cst, Cst[:, 2:4, :], Cst[:, 2:4, :], pU2[:, 0:258].rearrange("p (h j) -> p h j", j=129), ALU.add, [Cst, pU2])
                    yield
                    if so:
                        return
                    den = den_ring.next()
                    yield
                    act(den, den[:], pOd[:, 384:388], AF.Abs, [pOd])
                    yield
                    tt("dve", den, den[:], den[:], tsc[tb][:, 4:8], ALU.max, [den, tsc[tb]])
                    yield
                    P.op("dve", lambda: nc.vector.reciprocal(out=den[:], in_=den[:]), reads=[den], writes=[den])
                    yield
                    hm4 = hm4_ring.next()
                    yield
                    tt("dve", hm4, hm4[:].rearrange("p (h d) -> p h d", d=128), pOn[:].rearrange("p (h d) -> p h d", d=128),
                       den[:].unsqueeze(2).to_broadcast([128, 4, 128]), ALU.mult, [pOn, den])
                    sq4 = hm4_ring.next()
                    yield
                    act(sq4, sq4[:], hm4[:], AF.Square, [hm4])
                    yield
                    ssq = ssq_ring.next()
                    yield
                    P.op("dve", lambda: nc.vector.tensor_reduce(out=ssq[:], in_=sq4[:].rearrange("p (h d) -> p h d", d=128), axis=AX.X, op=ALU.add),
                         reads=[sq4], writes=[ssq])
                    ts(ssq, ssq[:], ssq[:], 1.0 / 128, EPS, ALU.mult, ALU.add, [ssq])
                    yield
                    act(ssq, ssq[:], ssq[:], AF.Sqrt, [ssq])
                    yield
                    P.op("dve", lambda: nc.vector.reciprocal(out=ssq[:], in_=ssq[:]), reads=[ssq], writes=[ssq])
                    yield
                    tt("dve", hm4, hm4[:].rearrange("p (h d) -> p h d", d=128), hm4[:].rearrange("p (h d) -> p h d", d=128),
                       ssq[:].unsqueeze(2).to_broadcast([128, 4, 128]), ALU.mult, [hm4, ssq])
                    hmn = hmn_ring.next()
                    yield
                    tt("pool", hmn, hmn[:], hm4[:], go[tb][:], ALU.mult, [hm4, go[tb]])
                    yield
                    ph = psb.next()
                    yield
                    for h in range(4):
                        P.op("pe", lambda: nc.tensor.transpose(out=ph[:, h * 128:(h + 1) * 128], in_=hmn[:, h * 128:(h + 1) * 128], identity=ident_b[:]),
                             reads=[hmn, ident_b], writes=[ph])
                    act(hmix_m, hmix_m[:, :, blk], ph[:, 0:512].rearrange("p (h d) -> p h d", d=128), AF.Copy, [ph])
                    yield

                for tb in range(4):
                    gens = [mlstm_tb(tb), lru_chunk(tb)]
                    while gens:
                        for g_ in list(gens):
                            try:
                                next(g_)
                            except StopIteration:
                                gens.remove(g_)
                if so:
                    continue
                rms_rstd(None, lambda c: hl[c][:], 4, onesL, sq_ring, rstl, reads_extra=hl)
                for c in range(4):
                    stt(hmix_l[c], hmix_l[c][:], hl[c][:], V(f"gml{l}", c), rstl[:], ALU.mult, ALU.mult, [hl[c], vec, rstl])

                for dc in range(8):
                    ps = psf.next()
                    for kc in range(8):
                        hx_t = hmix_m if kc < 4 else hmix_l[kc - 4]
                        hx_ap = hmix_m[:, kc, :] if kc < 4 else hmix_l[kc - 4][:]
                        mm(ps, ps[:], wout[kc // 4][:, kc % 4, dc * 128:(dc + 1) * 128], hx_ap, [wout[kc // 4], hx_t],
                           start=(kc == 0), stop=(kc == 7))
                    stt(xt, xt[:, dc, :], ps[:], Dv(l, 16 + dc), xt[:, dc, :], ALU.mult, ALU.add, [ps, derived, xt])
                P.dma("sp", dst_v[:, :, t0:t0 + 512], xt[:], reads=[xt], writes=[dst_blocks[s]])

            sto = st_dst
            swr = [st_dst_tile] if st_dst_tile is not None else []
            P.dma("sp", sto[:, 0:516], Cst[:].rearrange("p h j -> p (h j)"), reads=[Cst], writes=swr)
            P.dma("sp", sto[:, 516:520], lru_h[:], reads=[lru_h], writes=swr)
            P.dma("sp", sto[:, 520:556], tails[:].rearrange("p c j -> p (c j)"), reads=[tails], writes=swr)
            mend = P.sb([128, 4], F32, "mend")
            P.op("dve", lambda: nc.vector.memset(mend[:], 0.0), writes=[mend])
            tt("dve", mend, mend[0:4, 0:1], gcar[0:4, 1:2], gcar[0:4, 0:1], ALU.subtract, [gcar, mend])
            P.dma("sp", sto[:, 556:560], mend[:], reads=[mend], writes=swr)
            P.barrier()
        P.tes = es

    def ffn_phase(l, moe, final, buf_v, buf_blocks):
        with contextlib.ExitStack() as es2:
            P.tes = es2
            xF = P.sb([128, 8, TT], F32, "xF")
            hF = [P.sb([128, TT], BF16, "hF") for _ in range(8)]
            tmp_ring = Ring([P.sb([128, 512], F32, "tmp") for _ in range(3)])
            sq_ring = tmp_ring
            rstd = P.sb([128, 512], F32, "rstd")
            wg_ring = Ring([P.sb([128, 8, JGW], BF16, "wg") for _ in range(3)])
            wu_ring = Ring([P.sb([128, 8, JGW], BF16, "wu") for _ in range(3)])
            wd_ring = Ring([P.sb([128, 2, 1024], BF16, "wd") for _ in range(3)])
            sg_ring = Ring([P.sb([128, 512], F32, "sg") for _ in range(3)])
            act_ring = Ring([P.sb([128, 512], BF16, "actb") for _ in range(4 * NB)])
            if moe:
                hf32 = [P.sb([128, 512], F32, "hf32") for _ in range(8)]
                wr = P.sb([128, 8, 8], F32, "wr")
                P.dma("sp", wr[:], w_r_d.rearrange("(kc p) e -> p kc e", p=128), writes=[wr])
                brb = P.sb([128, 8], F32, "brb")
                P.dma("sp", brb[:], brb_d, writes=[brb])
                gTs = P.sb([8, TT], F32, "gTs")
                gb = [P.sb([128, TT], F32, "gb") for _ in range(NE)]
                lg = P.sb([128, 8], F32, "lg")
                lg2 = P.sb([128, 8], F32, "lg2")
                m1 = P.sb([128, 1], F32, "m1")
                m2 = P.sb([128, 1], F32, "m2")
                selt = P.sb([128, 8], F32, "selt")
                pe_ = P.sb([128, 8], F32, "pe_")
                dsum = P.sb([128, 1], F32, "dsum")
                sg2_ring = Ring([P.sb([128, 512], F32, "sg2") for _ in range(3)])
            n_exp = NE if moe else 1

            for tti in range(NTT):
                T0 = tti * TT
                blocks = [buf_blocks[(T0 // 512) + n] for n in range(NB)]
                P.dma("sp", xF[:], buf_v[:, :, T0:T0 + TT], reads=blocks, writes=[xF])
                for n in range(NB):
                    nb_ = slice(n * 512, (n + 1) * 512)
                    rms_rstd(xF, lambda kc: xF[:, kc, nb_], 8, onesD, sq_ring, rstd)
                    for kc in range(8):
                        tmp = tmp_ring.next()
                        tt("dve", tmp, tmp[:], xF[:, kc, nb_], rstd[:], ALU.mult, [xF, rstd])
                        if moe:
                            act(hf32[kc], hf32[kc][:], tmp[:], AF.Identity, [tmp, derived], scale=Dv(l, 24 + kc), bias=Dv(l, 32 + kc))
                            P.op("pool", lambda: nc.gpsimd.tensor_copy(out=hF[kc][:, nb_], in_=hf32[kc][:]), reads=[hf32[kc]], writes=[hF[kc]])
                        else:
                            act(hF[kc], hF[kc][:, nb_], tmp[:], AF.Identity, [tmp, derived], scale=Dv(l, 24 + kc), bias=Dv(l, 32 + kc))
                    if moe:
                        for tb in range(4):
                            tblk = slice(tb * 128, (tb + 1) * 128)
                            pr = psf.next()
                            for kc in range(8):
                                mm(pr, pr[:, 0:8], hf32[kc][:, tblk], wr[:, kc, :], [hf32[kc], wr], start=(kc == 0), stop=(kc == 7))
                            tt("dve", lg, lg[:], pr[:, 0:8], brb[:], ALU.add, [pr, brb])
                            P.op("dve", lambda: nc.vector.tensor_reduce(out=m1[:], in_=lg[:], axis=AX.X, op=ALU.max), reads=[lg], writes=[m1])
                            ts(selt, selt[:], lg[:], m1[:, 0:1], -1e30, ALU.is_ge, ALU.mult, [lg, m1])
                            tt("dve", lg2, lg2[:], lg[:], selt[:], ALU.add, [lg, selt])
                            P.op("dve", lambda: nc.vector.tensor_reduce(out=m2[:], in_=lg2[:], axis=AX.X, op=ALU.max), reads=[lg2], writes=[m2])
                            ts(selt, selt[:], lg[:], m2[:, 0:1], None, ALU.is_ge, None, [lg, m2])
                            ts(m1, m1[:], m1[:], -1.0, None, ALU.mult, None, [m1])
                            act(pe_, pe_[:], lg[:], AF.Exp, [lg, m1], bias=m1[:, 0:1], scale=1.0)
                            tt("dve", pe_, pe_[:], pe_[:], selt[:], ALU.mult, [pe_, selt])
                            P.op("dve", lambda: nc.vector.tensor_reduce(out=dsum[:], in_=pe_[:], axis=AX.X, op=ALU.add), reads=[pe_], writes=[dsum])
                            P.op("dve", lambda: nc.vector.reciprocal(out=dsum[:], in_=dsum[:]), reads=[dsum], writes=[dsum])
                            ts(pe_, pe_[:], pe_[:], dsum[:, 0:1], None, ALU.mult, None, [pe_, dsum])
                            pg = psf.next()
                            P.op("pe", lambda: nc.tensor.transpose(out=pg[0:8, 0:128], in_=pe_[:], identity=ident_f), reads=[pe_, consts], writes=[pg])
                            c0 = n * 512 + tb * 128
                            P.op("dve", lambda: nc.vector.tensor_copy(out=gTs[:, c0:c0 + 128], in_=pg[0:8, 0:128]), reads=[pg], writes=[gTs])
                        for e in range(NE):
                            pb = psf.next()
                            mm(pb, pb[:], consts[0:8, 256 + e * 128:256 + (e + 1) * 128], gTs[:, nb_], [consts, gTs])
                            act(gb[e], gb[e][:, nb_], pb[:], AF.Copy, [pb])

                stages = [(e, jg) for e in range(n_exp) for jg in range(NJG)]
                loaded = {}

                def load(k):
                    e, jg = stages[k]
                    wg = wg_ring.next()
                    wu = wu_ring.next()
                    wd = wd_ring.next()
                    if moe:
                        P.dma("pool", wg[:], exg_d[e, jg], writes=[wg])
                        P.dma("pool", wu[:], exu_d[e, jg], writes=[wu])
                        P.dma("pool", wd[:], exd_d[e, jg * JGW:(jg + 1) * JGW, :].rearrange("(c p) n -> p c n", p=128), writes=[wd])
                    else:
                        P.dma("pool", wg[:], ffg_d[jg], writes=[wg])
                        P.dma("pool", wu[:], ffu_d[jg], writes=[wu])
                        P.dma("pool", wd[:], ffd_d[jg * JGW:(jg + 1) * JGW, :].rearrange("(c p) n -> p c n", p=128), writes=[wd])
                    loaded[k] = (wg, wu, wd)

                load(0)
                if len(stages) > 1:
                    load(1)
                for k in range(len(stages)):
                    if k + 2 < len(stages):
                        load(k + 2)
                    e, jg = stages[k]
                    wg, wu, wd = loaded.pop(k)
                    acts = {}
                    for jj in range(2):
                        for n in range(NB):
                            nb_ = slice(n * 512, (n + 1) * 512)
                            pgp = psf.next()
                            for kc in range(8):
                                mm(pgp, pgp[:], wg[:, kc, jj * 128:(jj + 1) * 128], hF[kc][:, nb_], [wg, hF[kc]], start=(kc == 0), stop=(kc == 7))
                            pup = psf.next()
                            for kc in range(8):
                                mm(pup, pup[:], wu[:, kc, jj * 128:(jj + 1) * 128], hF[kc][:, nb_], [wu, hF[kc]], start=(kc == 0), stop=(kc == 7))
                            sg = sg_ring.next()
                            act(sg, sg[:], pgp[:], AF.Silu, [pgp])
                            ab = act_ring.next()
                            if moe:
                                sg2 = sg2_ring.next()
                                tt("dve", sg2, sg2[:], sg[:], pup[:], ALU.mult, [sg, pup])
                                tt("pool", ab, ab[:], sg2[:], gb[e][:, nb_], ALU.mult, [sg2, gb[e]])
                            else:
                                tt("dve", ab, ab[:], sg[:], pup[:], ALU.mult, [sg, pup])
                            acts[(jj, n)] = ab
                    for n in range(NB):
                        nb_ = slice(n * 512, (n + 1) * 512)
                        for dc in range(8):
                            py = psf.next()
                            for jj in range(2):
                                mm(py, py[:], wd[:, jj, dc * 128:(dc + 1) * 128], acts[(jj, n)][:], [wd, acts[(jj, n)]], start=(jj == 0), stop=(jj == 1))
                            stt(xF, xF[:, dc, nb_], py[:], Dv(l, 40 + dc), xF[:, dc, nb_], ALU.mult, ALU.add, [py, derived, xF])

                if final:
                    for n in range(NB):
                        nb_ = slice(n * 512, (n + 1) * 512)
                        rms_rstd(xF, lambda kc: xF[:, kc, nb_], 8, onesD, sq_ring, rstd)
                        for kc in range(8):
                            stt(xF, xF[:, kc, nb_], xF[:, kc, nb_], V("gfin", kc), rstd[:], ALU.mult, ALU.mult, [xF, vec, rstd])
                P.dma("sp", buf_v[:, :, T0:T0 + TT], xF[:], reads=[xF], writes=blocks)
            P.barrier()
        P.tes = es

    if prefix:
        for l in range(NL):
            mixer_phase(l, xpT_v if l == 0 else preT_v, None if l == 0 else dpre, preT_v, dpre,
                        st_in_d[l], None, st_pre_d[l], dstp[l], False, so=(l == NL - 1))
            if l < NL - 1:
                ffn_phase(l, moe=(l in moe_layers), final=False, buf_v=preT_v, buf_blocks=dpre)
    for l in range(NL):
        if not _SKIP_MIX:
            if prefix:
                mixer_phase(l, xT_v if l == 0 else outT_v, None if l == 0 else dx, outT_v, dx,
                            st_pre_d[l], dstp[l], st_out_d[l], None, True)
            else:
                mixer_phase(l, xT_v if l == 0 else outT_v, None if l == 0 else dx, outT_v, dx,
                            st_in_d[l], None, st_out_d[l], None, False)
        if not _SKIP_FFN:
            ffn_phase(l, moe=(l in moe_layers), final=(l == NL - 1), buf_v=outT_v, buf_blocks=dx)
    P.barrier()
    es.close()
    return nc


_NC_CACHE = {}


def _prep_shared(inp, NL):
    sh = {}
    sh["w_ada"] = np.ascontiguousarray(inp["w_ada"], dtype=np.float32)
    sh["w_in"] = np.ascontiguousarray(inp["w_in"], dtype=np.float32)
    sh["w_out"] = np.ascontiguousarray(inp["w_out"], dtype=np.float32)
    sh["w_lru_a"] = np.ascontiguousarray(inp["w_lru_a"], dtype=np.float32)
    sh["w_lru_x"] = np.ascontiguousarray(inp["w_lru_x"], dtype=np.float32)

    def grp(w):
        return np.ascontiguousarray(w.reshape(8, 128, NJG, JGW).transpose(2, 1, 0, 3))

    sh["ffg"] = grp(np.asarray(inp["w_ff_gate"][0], np.float32))
    sh["ffu"] = grp(np.asarray(inp["w_ff_up"][0], np.float32))
    sh["ffd"] = np.ascontiguousarray(inp["w_ff_down"][0], dtype=np.float32)
    sh["w_router"] = np.ascontiguousarray(inp["w_router"][0], dtype=np.float32)
    sh["brb"] = np.ascontiguousarray(np.broadcast_to(np.asarray(inp["b_router"][0], np.float32)[None, :], (128, 8)))
    sh["exg"] = np.stack([grp(np.asarray(inp["w_exp_gate"][0, e], np.float32)) for e in range(NE)])
    sh["exu"] = np.stack([grp(np.asarray(inp["w_exp_up"][0, e], np.float32)) for e in range(NE)])
    sh["exd"] = np.ascontiguousarray(inp["w_exp_down"][0], dtype=np.float32)
    sh["gmixb"] = np.ascontiguousarray(np.broadcast_to(np.asarray(inp["g_mix_out"], np.float32)[:, None, :512], (NL, 128, 512)))
    consts = np.zeros((128, 256 + 1024), np.float32)
    consts[:, 0:128] = np.eye(128, dtype=np.float32)
    consts[:, 128:256] = np.triu(np.ones((128, 128), np.float32))
    for e in range(NE):
        consts[e, 256 + e * 128:256 + (e + 1) * 128] = 1.0
    sh["consts"] = consts
    return sh


def _vecs(inp, b, NL, flag=0.0):
    lay, NV = vec_layout(NL)
    v = np.zeros((128, NV), np.float32)

    def put(name, arr):
        o, n = lay[name]
        assert arr.shape == (128, n), (name, arr.shape, n)
        v[:, o:o + n] = arr

    put("cond", fm(inp["c"][b], 8))
    for l in range(NL):
        put(f"bada{l}", fm(inp["b_ada"][l], 48))
        put(f"gm{l}", fm(inp["g_norm_mix"][l], 8))
        put(f"gf{l}", fm(inp["g_norm_ffn"][l], 8))
        w = np.asarray(inp["w_conv_qk"][l], np.float32)
        put(f"wcqk{l}", np.ascontiguousarray(w.T.reshape(8, 128, 4).transpose(1, 0, 2).reshape(128, 32)))
        put(f"bcqk{l}", fm(inp["b_conv_qk"][l], 8))
        w = np.asarray(inp["w_conv_lru"][l], np.float32)
        put(f"wcl{l}", np.ascontiguousarray(w.T.reshape(4, 128, 4).transpose(1, 0, 2).reshape(128, 16)))
        put(f"bcl{l}", fm(inp["b_conv_lru"][l], 4))
        put(f"bla{l}", fm(inp["b_lru_a"][l], 4))
        put(f"blx{l}", fm(inp["b_lru_x"][l], 4))
        put(f"lam{l}", fm(inp["lru_lambda"][l], 4))
        put(f"gml{l}", fm(inp["g_mix_out"][l][512:], 4))
        bg = np.zeros((128, 2), np.float32)
        bg[0:4, 0] = inp["b_gates"][l][0:4]
        bg[0:4, 1] = inp["b_gates"][l][4:8]
        put(f"bg{l}", bg)
    put("gfin", fm(inp["g_final"], 8))
    put("flag", np.full((128, 1), flag, np.float32))
    return v


def kernel(**inputs):
    x = np.asarray(inputs["x"], np.float32)
    B, S, D = x.shape
    NL = int(np.asarray(inputs["w_in"]).shape[0])
    n_cores = 2 * B
    T = S // 2
    key = (T, NL)
    if key not in _NC_CACHE:
        _NC_CACHE[key] = build(T, NL)
    nc = _NC_CACHE[key]
    sh = _prep_shared(inputs, NL)
    zeros_st = np.zeros((NL, 128, SW), np.float32)
    zeros_x = np.zeros((D, T), np.float32)
    in_maps = []
    for cid in range(n_cores):
        b, half = cid // 2, cid % 2
        m = dict(sh)
        m["xT"] = np.ascontiguousarray(x[b, half * T:(half + 1) * T, :].T)
        m["xpT"] = np.ascontiguousarray(x[b, 0:T, :].T) if half == 1 else zeros_x
        m["vecs"] = _vecs(inputs, b, NL, flag=float(half))
        m["st_in"] = zeros_st
        in_maps.append(m)
    res = run_bass_kernel_spmd(nc, in_maps, core_ids=list(range(n_cores)))
    out = np.empty((B, S, D), np.float32)
    for cid in range(n_cores):
        b, half = cid // 2, cid % 2
        out[b, half * T:(half + 1) * T, :] = res.results[cid]["outT"].T
    return out
```

```python
import contextlib
import math
import numpy as np
import concourse.bass as bass
import concourse.mybir as mybir
from concourse.bass_utils import run_bass_kernel_spmd

F32 = mybir.dt.float32
BF16 = mybir.dt.bfloat16
ALU = mybir.AluOpType
AF = mybir.ActivationFunctionType
AX = mybir.AxisListType

D_MODEL = 1024
D_FF = 2816
NE = 8
EPS = 1e-6
D_IN = 3080
SW = 560
C_Q, C_K, C_V, C_O, C_IG, C_FG, C_XL, C_GL = 0, 512, 1024, 1536, 2048, 2052, 2056, 2568
JGW = 256
NJG = D_FF // JGW

SAME_ENGINE_SYNC = True
EPOCH = 16000
_SKIP_MIX = False
_DEBUG_MAP = None
_SKIP_FFN = False


class Tl:
    __slots__ = ("t", "w", "r", "name")

    def __init__(self, t, name):
        self.t = t
        self.w = None
        self.r = {}
        self.name = name

    def __getitem__(self, k):
        return self.t[k]


class Ring:
    def __init__(self, tiles):
        self.tiles = tiles
        self.i = 0

    def next(self):
        t = self.tiles[self.i % len(self.tiles)]
        self.i += 1
        return t


class Prog:
    def __init__(self, nc, es, nslots=40):
        self.nc = nc
        self.es = es
        self.tes = es
        self.eng = {"pe": nc.tensor, "act": nc.scalar, "dve": nc.vector, "pool": nc.gpsimd, "sp": nc.sync}
        self.sems = {}
        self.cnt = {e: 0 for e in self.eng}
        self.seen = {e: {} for e in self.eng}
        self.nslots = nslots
        self.slot_sem = [es.enter_context(nc.semaphore(f"dq{i}")) for i in range(nslots)]
        self.slot_cnt = [0] * nslots
        self.next_slot = 0
        self.uid = 0
        self.dbg = None

    def _esem(self, e, ep):
        k = (e, ep)
        if k not in self.sems:
            self.sems[k] = self.es.enter_context(self.nc.semaphore(f"s_{e}_{ep}"))
        return self.sems[k]

    def sb(self, shape, dtype, name=None):
        self.uid += 1
        name = f"{name or 't'}_{self.uid}"
        return Tl(self.tes.enter_context(self.nc.sbuf_tensor(name, list(shape), dtype)), name)

    def ps(self, shape, dtype, name=None):
        self.uid += 1
        name = f"{name or 'p'}_{self.uid}"
        return Tl(self.tes.enter_context(self.nc.psum_tensor(name, list(shape), dtype)), name)

    def pseudo(self, name):
        return Tl(None, name)

    def _wait(self, e, dep):
        kind, key, val = dep
        k = (kind, key)
        if self.seen[e].get(k, 0) >= val:
            return
        sem = self._esem(key[0], key[1]) if kind == "eng" else self.slot_sem[key]
        self.eng[e].wait_ge(sem, val)
        self.seen[e][k] = val

    def _deps(self, e, reads, writes, force_same=False):
        deps = []
        for t in reads:
            if t.w is not None:
                deps.append(t.w)
        for t in writes:
            if t.w is not None:
                deps.append(t.w)
            for k, v in t.r.items():
                deps.append((k[0], k[1], v))
        for d in deps:
            if d[0] == "eng" and d[1][0] == e and not force_same and (e == "pe" or not SAME_ENGINE_SYNC):
                continue
            self._wait(e, d)

    def _mark(self, me, reads, writes):
        k = (me[0], me[1])
        for t in reads:
            if t.r.get(k, 0) < me[2]:
                t.r[k] = me[2]
        for t in writes:
            t.w = me
            t.r = {}

    def _cur(self, e):
        c = self.cnt[e]
        ep = (c - 1) // EPOCH
        return ("eng", (e, ep), c - ep * EPOCH)

    def op(self, e, fn, reads=(), writes=()):
        self._deps(e, reads, writes)
        ins = fn()
        if self.dbg is not None:
            import sys as _sys
            f = _sys._getframe(1)
            chain = []
            while f is not None and len(chain) < 4:
                if f.f_code.co_name not in ("mm", "act", "tt", "ts", "stt"):
                    chain.append(f.f_lineno)
                f = f.f_back
            try:
                self.dbg[ins.ins.name] = chain
            except Exception:
                pass
        self.cnt[e] += 1
        me = self._cur(e)
        ins.then_inc(self._esem(e, me[1][1]), 1)
        self._mark(me, reads, writes)
        return me

    def dma(self, q, out, in_, reads=(), writes=(), slow=False):
        slot = self.next_slot
        self.next_slot = (slot + 1) % self.nslots
        if self.slot_cnt[slot] > 0:
            self._wait(q, ("dma", slot, 16 * self.slot_cnt[slot]))
        self._deps(q, reads, writes, force_same=True)
        if slow:
            ins = self.eng[q].dma_start(out=out, in_=in_, allow_slow_non_contiguous=True)
        else:
            ins = self.eng[q].dma_start(out=out, in_=in_)
        self.slot_cnt[slot] += 1
        ins.then_inc(self.slot_sem[slot], 16)
        me = ("dma", slot, 16 * self.slot_cnt[slot])
        self._mark(me, reads, writes)
        return me

    def barrier(self):
        deps = []
        for e in self.eng:
            if self.cnt[e] > 0:
                deps.append(self._cur(e))
        for s in range(self.nslots):
            if self.slot_cnt[s] > 0:
                deps.append(("dma", s, 16 * self.slot_cnt[s]))
        for e in self.eng:
            for d in deps:
                self._wait(e, d)


def vec_layout(NL):
    lay = {}
    off = 0

    def add(name, n):
        nonlocal off
        lay[name] = (off, n)
        off += n

    add("cond", 8)
    for l in range(NL):
        add(f"bada{l}", 48)
        add(f"gm{l}", 8)
        add(f"gf{l}", 8)
        add(f"wcqk{l}", 32)
        add(f"bcqk{l}", 8)
        add(f"wcl{l}", 16)
        add(f"bcl{l}", 4)
        add(f"bla{l}", 4)
        add(f"blx{l}", 4)
        add(f"lam{l}", 4)
        add(f"gml{l}", 4)
        add(f"bg{l}", 2)
    add("gfin", 8)
    add("flag", 1)
    return lay, off


def fm(v, nch):
    return np.ascontiguousarray(np.asarray(v, np.float32).reshape(nch, 128).T)


def build(T, NL, moe_layers=(1,), prefix=True):
    NSUB = T // 512
    TT = min(1024, T)
    NTT = T // TT
    NB = TT // 512
    lay, NV = vec_layout(NL)

    nc = bass.Bass("TRN2", target_bir_lowering=False)
    DT = nc.dram_tensor
    xT = DT("xT", [1024, T], F32, kind="ExternalInput").ap()
    xpT = DT("xpT", [1024, T], F32, kind="ExternalInput").ap()
    preT = DT("preT", [1024, T], F32, kind="Internal").ap()
    st_pre_d = DT("st_pre", [NL, 128, SW], F32, kind="Internal").ap()
    vecs_d = DT("vecs", [128, NV], F32, kind="ExternalInput").ap()
    consts_d = DT("consts", [128, 256 + 1024], F32, kind="ExternalInput").ap()
    gmixb_d = DT("gmixb", [NL, 128, 512], F32, kind="ExternalInput").ap()
    brb_d = DT("brb", [128, 8], F32, kind="ExternalInput").ap()
    w_ada_d = DT("w_ada", [NL, 1024, 6144], F32, kind="ExternalInput").ap()
    w_in_d = DT("w_in", [NL, 1024, D_IN], F32, kind="ExternalInput").ap()
    w_out_d = DT("w_out", [NL, 1024, 1024], F32, kind="ExternalInput").ap()
    w_la_d = DT("w_lru_a", [NL, 8, 64, 64], F32, kind="ExternalInput").ap()
    w_lx_d = DT("w_lru_x", [NL, 8, 64, 64], F32, kind="ExternalInput").ap()
    ffg_d = DT("ffg", [NJG, 128, 8, JGW], F32, kind="ExternalInput").ap()
    ffu_d = DT("ffu", [NJG, 128, 8, JGW], F32, kind="ExternalInput").ap()
    ffd_d = DT("ffd", [D_FF, 1024], F32, kind="ExternalInput").ap()
    w_r_d = DT("w_router", [1024, 8], F32, kind="ExternalInput").ap()
    exg_d = DT("exg", [NE, NJG, 128, 8, JGW], F32, kind="ExternalInput").ap()
    exu_d = DT("exu", [NE, NJG, 128, 8, JGW], F32, kind="ExternalInput").ap()
    exd_d = DT("exd", [NE, D_FF, 1024], F32, kind="ExternalInput").ap()
    st_in_d = DT("st_in", [NL, 128, SW], F32, kind="ExternalInput").ap()
    outT = DT("outT", [1024, T], F32, kind="ExternalOutput").ap()
    st_out_d = DT("st_out", [NL, 128, SW], F32, kind="ExternalOutput").ap()

    xT_v = xT.rearrange("(kc p) t -> p kc t", p=128)
    xpT_v = xpT.rearrange("(kc p) t -> p kc t", p=128)
    preT_v = preT.rearrange("(kc p) t -> p kc t", p=128)
    outT_v = outT.rearrange("(kc p) t -> p kc t", p=128)

    es = contextlib.ExitStack()
    P = Prog(nc, es)
    if _DEBUG_MAP is not None:
        P.dbg = _DEBUG_MAP
    dx = [P.pseudo(f"dx{s}") for s in range(NSUB)]
    dpre = [P.pseudo(f"dpre{s}") for s in range(NSUB)]
    dstp = [P.pseudo(f"dstp{l}") for l in range(NL)]

    consts = P.sb([128, 256 + 1024], F32, "consts")
    vec = P.sb([128, NV], F32, "vec")
    P.dma("sp", consts[:], consts_d, writes=[consts])
    P.dma("sp", vec[:], vecs_d, writes=[vec])
    ident_f = consts[:, 0:128]
    mask_f = consts[:, 128:256]
    ident_b = P.sb([128, 128], BF16, "identb")
    P.op("dve", lambda: nc.vector.tensor_copy(out=ident_b[:], in_=ident_f), reads=[consts], writes=[ident_b])
    onesD = P.sb([128, 128], F32, "onesD")
    onesL = P.sb([128, 128], F32, "onesL")
    ones1 = P.sb([128, 512], F32, "ones1")
    epsc = P.sb([128, 1], F32, "epsc")
    P.op("dve", lambda: nc.vector.memset(onesD[:], 1.0 / 1024), writes=[onesD])
    P.op("dve", lambda: nc.vector.memset(onesL[:], 1.0 / 512), writes=[onesL])
    P.op("dve", lambda: nc.vector.memset(ones1[:], 1.0), writes=[ones1])
    P.op("dve", lambda: nc.vector.memset(epsc[:], EPS), writes=[epsc])

    def V(name, j=0, n=1):
        o, _ = lay[name]
        return vec[:, o + j:o + j + n]

    psf = Ring([P.ps([128, 512], F32, "psf") for _ in range(6)])
    psb = Ring([P.ps([128, 1024], BF16, "psb") for _ in range(2)])

    def mm(o_t, o_ap, l_ap, r_ap, reads, start=True, stop=True):
        P.op("pe", lambda: nc.tensor.matmul(o_ap, l_ap, r_ap, start=start, stop=stop), reads=reads, writes=[o_t])

    def act(o_t, o_ap, i_ap, func, reads, scale=None, bias=None, accum=None, extra_w=()):
        kw = {}
        if scale is not None:
            kw["scale"] = scale
        if bias is not None:
            kw["bias"] = bias
        if accum is not None:
            kw["accum_out"] = accum
        P.op("act", lambda: nc.scalar.activation(out=o_ap, in_=i_ap, func=func, **kw), reads=reads,
             writes=[o_t] + list(extra_w))

    def tt(e, o_t, o_ap, a_ap, b_ap, op, reads):
        eng = nc.vector if e == "dve" else nc.gpsimd
        P.op(e, lambda: eng.tensor_tensor(out=o_ap, in0=a_ap, in1=b_ap, op=op), reads=reads, writes=[o_t])

    def ts(o_t, o_ap, a_ap, s1, s2, op0, op1, reads, e="dve"):
        eng = nc.vector if e == "dve" else nc.gpsimd
        if op1 is None:
            P.op(e, lambda: eng.tensor_scalar(out=o_ap, in0=a_ap, scalar1=s1, scalar2=None, op0=op0), reads=reads, writes=[o_t])
        else:
            P.op(e, lambda: eng.tensor_scalar(out=o_ap, in0=a_ap, scalar1=s1, scalar2=s2, op0=op0, op1=op1), reads=reads, writes=[o_t])

    def stt(o_t, o_ap, a_ap, s, b_ap, op0, op1, reads):
        P.op("dve", lambda: nc.vector.scalar_tensor_tensor(out=o_ap, in0=a_ap, scalar=s, in1=b_ap, op0=op0, op1=op1),
             reads=reads, writes=[o_t])

    mod = P.sb([128, NL * 48], F32, "mod")
    derived = P.sb([128, NL * 64], F32, "derived")
    with contextlib.ExitStack() as es2:
        P.tes = es2
        cond = P.sb([128, 8], F32, "cond")
        act(cond, cond[:], V("cond", 0, 8), AF.Silu, [vec])
        wa_ring = Ring([P.sb([128, 8, 1024], F32, "wada") for _ in range(2)])
        pmod = psf.next()
        for l in range(NL):
            for sl in range(6):
                wa = wa_ring.next()
                P.dma("sp", wa[:], w_ada_d[l].rearrange("(kc p) n -> p kc n", p=128)[:, :, sl * 1024:(sl + 1) * 1024], writes=[wa])
                for j in range(8):
                    col = l * 48 + sl * 8 + j
                    for kc in range(8):
                        mm(pmod, pmod[:, col:col + 1], wa[:, kc, j * 128:(j + 1) * 128], cond[:, kc:kc + 1], [wa, cond],
                           start=(kc == 0), stop=(kc == 7))
        for l in range(NL):
            o = lay[f"bada{l}"][0]
            tt("dve", mod, mod[:, l * 48:(l + 1) * 48], pmod[:, l * 48:(l + 1) * 48], vec[:, o:o + 48], ALU.add, [pmod, vec])
            m0 = l * 48
            d0 = l * 64
            stt(derived, derived[:, d0:d0 + 8], mod[:, m0 + 8:m0 + 16], 1.0, V(f"gm{l}", 0, 8), ALU.add, ALU.mult, [mod, vec])
            P.op("dve", lambda: nc.vector.tensor_copy(out=derived[:, d0 + 8:d0 + 16], in_=mod[:, m0:m0 + 8]), reads=[mod], writes=[derived])
            P.op("dve", lambda: nc.vector.tensor_copy(out=derived[:, d0 + 16:d0 + 24], in_=mod[:, m0 + 16:m0 + 24]), reads=[mod], writes=[derived])
            stt(derived, derived[:, d0 + 24:d0 + 32], mod[:, m0 + 32:m0 + 40], 1.0, V(f"gf{l}", 0, 8), ALU.add, ALU.mult, [mod, vec])
            P.op("dve", lambda: nc.vector.tensor_copy(out=derived[:, d0 + 32:d0 + 40], in_=mod[:, m0 + 24:m0 + 32]), reads=[mod], writes=[derived])
            P.op("dve", lambda: nc.vector.tensor_copy(out=derived[:, d0 + 40:d0 + 48], in_=mod[:, m0 + 40:m0 + 48]), reads=[mod], writes=[derived])
            tmp4 = P.sb([128, 4], F32, "tmp4")
            act(tmp4, tmp4[:], V(f"lam{l}", 0, 4), AF.Exp, [vec], scale=-1.0)
            act(tmp4, tmp4[:], tmp4[:], AF.Ln, [tmp4], bias=1.0)
            ts(derived, derived[:, d0 + 48:d0 + 52], tmp4[:], -8.0, None, ALU.mult, None, [tmp4])
            ts(derived, derived[:, d0 + 52:d0 + 56], tmp4[:], -16.0, None, ALU.mult, None, [tmp4])
            ts(derived, derived[:, d0 + 56:d0 + 57], V(f"bg{l}", 1, 1), -1.0, None, ALU.mult, None, [vec])
        P.barrier()
    P.tes = es

    def Dv(l, j, n=1):
        return derived[:, l * 64 + j:l * 64 + j + n]

    def rms_rstd(x_t, x_ap_of_kc, nk, ones_t, sq_ring, rstd_t, reads_extra=()):
        ss = psf.next()
        for kc in range(nk):
            sq = sq_ring.next()
            act(sq, sq[:], x_ap_of_kc(kc), AF.Square, ([x_t] if x_t is not None else []) + list(reads_extra))
            mm(ss, ss[:], ones_t[:], sq[:], [ones_t, sq], start=(kc == 0), stop=(kc == nk - 1))
        act(rstd_t, rstd_t[:], ss[:], AF.Sqrt, [ss, epsc], bias=epsc[:, 0:1], scale=1.0)
        P.op("dve", lambda: nc.vector.reciprocal(out=rstd_t[:], in_=rstd_t[:]), reads=[rstd_t], writes=[rstd_t])

    def mixer_phase(l, src_v, src_blocks, dst_v, dst_blocks, st_src, st_src_tile, st_dst, st_dst_tile, use_flag, so=False):
        with contextlib.ExitStack() as es2:
            P.tes = es2
            win = [P.sb([128, 2, D_IN], BF16, "win") for _ in range(4)]
            wout = [P.sb([128, 4, 1024], BF16, "wout") for _ in range(2)]
            w_in_v = w_in_d[l].rearrange("(kc p) n -> p kc n", p=128)
            w_out_v = w_out_d[l].rearrange("(kc p) n -> p kc n", p=128)
            for i in range(4):
                P.dma("pool", win[i][:], w_in_v[:, 2 * i:2 * i + 2, :], writes=[win[i]])
            for i in range(2):
                P.dma("pool", wout[i][:], w_out_v[:, 4 * i:4 * i + 4, :], writes=[wout[i]])
            bd_a = P.sb([128, 4, 128], BF16, "bda")
            bd_x = P.sb([128, 4, 128], BF16, "bdx")
            for bd, wd in ((bd_a, w_la_d), (bd_x, w_lx_d)):
                P.op("dve", lambda: nc.vector.memset(bd[:], 0.0), writes=[bd])
                wv = wd[l].rearrange("(c two) k m -> two k c m", two=2)
                P.dma("pool", bd[0:64, :, 0:64], wv[0], writes=[bd])
                P.dma("pool", bd[64:128, :, 64:128], wv[1], reads=[bd], writes=[bd])
            gmb = P.sb([128, 512], F32, "gmb")
            P.dma("sp", gmb[:], gmixb_d[l], writes=[gmb])

            def wcol(kc, c0, c1):
                return win[kc // 2][:, kc % 2, c0:c1]

            Cst = P.sb([128, 4, 129], F32, "Cst")
            Cbf = P.sb([128, 4, 129], BF16, "Cbf")
            lru_h = P.sb([128, 4], F32, "lruh")
            gcar = P.sb([4, 2], F32, "gcar")
            tails = P.sb([128, 12, 3], F32, "tails")
            cbuf = Ring([P.sb([128, 515], F32, "cbuf") for _ in range(3)])
            sti = st_src
            srd = [st_src_tile] if st_src_tile is not None else []
            P.dma("sp", Cst[:].rearrange("p h j -> p (h j)"), sti[:, 0:516], reads=srd, writes=[Cst])
            P.dma("sp", lru_h[:], sti[:, 516:520], reads=srd, writes=[lru_h])
            P.dma("sp", tails[:].rearrange("p c j -> p (c j)"), sti[:, 520:556], reads=srd, writes=[tails])
            P.op("dve", lambda: nc.vector.memset(gcar[:], 0.0), writes=[gcar])
            P.dma("sp", gcar[0:4, 1:2], sti[0:4, 556:557], reads=[gcar] + srd, writes=[gcar], slow=True)
            if use_flag:
                fo = lay["flag"][0]
                ts(Cst, Cst[:].rearrange("p h j -> p (h j)"), Cst[:].rearrange("p h j -> p (h j)"), vec[:, fo:fo + 1], None, ALU.mult, None, [Cst, vec])
                ts(lru_h, lru_h[:], lru_h[:], vec[:, fo:fo + 1], None, ALU.mult, None, [lru_h, vec])
                ts(tails, tails[:].rearrange("p c j -> p (c j)"), tails[:].rearrange("p c j -> p (c j)"), vec[:, fo:fo + 1], None, ALU.mult, None, [tails, vec])
                ts(gcar, gcar[0:4, 1:2], gcar[0:4, 1:2], vec[0:4, fo:fo + 1], None, ALU.mult, None, [gcar, vec])

            xring = Ring([P.sb([128, 8, 512], F32, "xt") for _ in range(1)])
            hT = [P.sb([128, 512], BF16, "hT") for _ in range(8)]
            tmp_ring = Ring([P.sb([128, 512], F32, "tmp") for _ in range(3)])
            sq_ring = tmp_ring
            rstd = P.sb([128, 512], F32, "rstd")
            qk_bf = [P.sb([128, 512], BF16, "qkbf") for _ in range(8)]
            acc_ring = Ring([P.sb([128, 512], F32, "acc") for _ in range(2)])
            vaug = [P.sb([128, 4, 129], BF16, "vaug") for _ in range(4)]
            for tb in range(4):
                P.op("dve", lambda: nc.vector.memset(vaug[tb][:], 1.0), writes=[vaug[tb]])
            go = [P.sb([128, 512], F32, "go") for _ in range(4)]
            nb = P.sb([4, 512], F32, "nb")
            wv_t = P.sb([4, 512], F32, "wv")
            mu = P.sb([4, 512], F32, "mu")
            e1 = P.sb([4, 512], F32, "e1")
            fl = P.sb([4, 512], F32, "fl")
            g_e = e1
            g_l = fl
            rp = P.sb([4, 4], F32, "rp")
            a_t = P.sb([4, 4], F32, "a_t")
            adiag = P.sb([4, 16], F32, "adiag")
            abc = P.sb([128, 16], F32, "abc")
            tsc = [P.sb([128, 8], F32, "tsc") for _ in range(4)]
            ke_ring = Ring([P.sb([128, 4, 128], BF16, "ke") for _ in range(2)])
            stm_ring = Ring([P.sb([128, 4, 128], BF16, "stm") for _ in range(2)])
            den_ring = Ring([P.sb([128, 4], F32, "den") for _ in range(2)])
            hm4_ring = Ring([P.sb([128, 512], F32, "hm4") for _ in range(3)])
            junk = P.sb([128, 128], F32, "junk")
            ssq_ring = Ring([P.sb([128, 4], F32, "ssq") for _ in range(2)])
            hmn_ring = Ring([P.sb([128, 512], BF16, "hmn") for _ in range(1)])
            hmix_m = P.sb([128, 4, 512], BF16, "hmixm")
            hmix_l = [P.sb([128, 512], BF16, "hmixl") for _ in range(4)]
            xcb_ring = Ring([P.sb([128, 512], BF16, "xcb") for _ in range(2)])
            lt = [P.sb([128, 512], F32, "lt") for _ in range(6)]
            hl = [P.sb([128, 512], F32, "hl") for _ in range(4)]
            rstl = P.sb([128, 512], F32, "rstl")

            for s in range(NSUB):
                t0 = s * 512
                xt = xring.next()
                P.dma("sp", xt[:], src_v[:, :, t0:t0 + 512], reads=([src_blocks[s]] if src_blocks is not None else []), writes=[xt])
                rms_rstd(xt, lambda kc: xt[:, kc, :], 8, onesD, sq_ring, rstd)
                for kc in range(8):
                    tmp = tmp_ring.next()
                    tt("dve", tmp, tmp[:], xt[:, kc, :], rstd[:], ALU.mult, [xt, rstd])
                    act(hT[kc], hT[kc][:], tmp[:], AF.Identity, [tmp, derived], scale=Dv(l, 0 + kc), bias=Dv(l, 8 + kc))

                def proj_fm(ps, M, col0):
                    for kc in range(8):
                        mm(ps, ps[0:M, :], wcol(kc, col0, col0 + M), hT[kc][:], [win[kc // 2], hT[kc]],
                           start=(kc == 0), stop=(kc == 7))

                def conv(ps, ci, wname, bname, c, acc):
                    buf = cbuf.next()
                    act(buf, buf[:, 3:515], ps[:], AF.Copy, [ps])
                    P.op("dve", lambda: nc.vector.tensor_copy(out=buf[:, 0:3], in_=tails[:, ci, :]), reads=[tails, buf], writes=[buf])
                    wo = lay[wname][0] + 4 * c
                    bo = lay[bname][0] + c
                    ts(acc, acc[:], buf[:, 3:515], vec[:, wo + 3:wo + 4], vec[:, bo:bo + 1], ALU.mult, ALU.add, [buf, vec])
                    for j in (2, 1, 0):
                        stt(acc, acc[:], buf[:, j:j + 512], vec[:, wo + j:wo + j + 1], acc[:], ALU.mult, ALU.add, [buf, vec, acc])
                    P.op("dve", lambda: nc.vector.tensor_copy(out=tails[:, ci, :], in_=buf[:, 512:515]), reads=[buf, tails], writes=[tails])

                def sec_qk():
                    for c in range(8):
                        if so and c < 4:
                            if s == NSUB - 1:
                                ps = psf.tiles[c % 2]
                                proj_fm(ps, 128, C_Q + c * 128)
                                P.op("dve", lambda: nc.vector.tensor_copy(out=tails[:, c, :], in_=ps[:, 509:512]), reads=[ps, tails], writes=[tails])
                            continue
                        ps = psf.tiles[c % 2]
                        proj_fm(ps, 128, C_Q + c * 128)
                        acc = acc_ring.next()
                        conv(ps, c, f"wcqk{l}", f"bcqk{l}", c, acc)
                        act(qk_bf[c], qk_bf[c][:], acc[:], AF.Silu, [acc])
                        yield


                    yield
                def sec_gates():
                    psi = psf.tiles[2]
                    proj_fm(psi, 4, C_IG)
                    yield
                    psg = psf.tiles[3]
                    proj_fm(psg, 4, C_FG)
                    yield
                    act(g_e, g_e[:], psg[0:4, :], AF.Exp, [psg, derived], scale=-1.0, bias=derived[0:4, l * 64 + 56:l * 64 + 57])
                    yield
                    act(g_l, g_l[:], g_e[:], AF.Ln, [g_e], bias=1.0)
                    yield
                    P.op("dve", lambda: nc.vector.tensor_tensor_scan(out=nb[:], data0=ones1[0:4, :], data1=g_l[:], initial=gcar[0:4, 0:1],
                                                                     op0=ALU.mult, op1=ALU.add), reads=[ones1, g_l, gcar], writes=[nb])
                    bgo = lay[f"bg{l}"][0]
                    stt(wv_t, wv_t[:], psi[0:4, :], vec[0:4, bgo:bgo + 1], nb[:], ALU.add, ALU.add, [psi, vec, nb])
                    yield
                    P.op("dve", lambda: nc.vector.tensor_tensor_scan(out=mu[:], data0=wv_t[:], data1=wv_t[:], initial=gcar[0:4, 1:2],
                                                                     op0=ALU.max, op1=ALU.max), reads=[wv_t, gcar], writes=[mu])
                    mu_v = mu[:].rearrange("p (c t) -> p c t", t=128)
                    yield
                    Rb = mu_v[:, :, 127:128].to_broadcast([4, 4, 128])
                    yield
                    P.op("dve", lambda: nc.vector.tensor_copy(out=rp[:, 0:1], in_=gcar[0:4, 1:2]), reads=[gcar], writes=[rp])
                    yield
                    P.op("dve", lambda: nc.vector.tensor_copy(out=rp[:, 1:4].unsqueeze(2), in_=mu_v[:, 0:3, 127:128]), reads=[mu, rp], writes=[rp])
                    yield
                    tt("dve", a_t, a_t[:].unsqueeze(2), rp[:].unsqueeze(2), mu_v[:, :, 127:128], ALU.subtract, [rp, mu])
                    yield
                    act(a_t, a_t[:], a_t[:], AF.Exp, [a_t])
                    yield
                    tt("dve", adiag, adiag[:].rearrange("p (c h) -> p c h", h=4), a_t[:].unsqueeze(2).to_broadcast([4, 4, 4]),
                       consts[0:4, 0:4].unsqueeze(1).to_broadcast([4, 4, 4]), ALU.mult, [a_t, consts])
                    pab = psf.tiles[2]
                    mm(pab, pab[:, 0:16], ones1[0:4, 0:128], adiag[:], [ones1, adiag])
                    yield
                    P.op("dve", lambda: nc.vector.tensor_copy(out=abc[:], in_=pab[:, 0:16]), reads=[pab], writes=[abc])
                    yield
                    tt("dve", e1, e1[:].rearrange("p (c t) -> p c t", t=128), wv_t[:].rearrange("p (c t) -> p c t", t=128), Rb, ALU.subtract, [wv_t, mu])
                    yield
                    ts(e1, e1[:], e1[:], -0.5 * math.log(128.0), None, ALU.add, None, [e1])
                    yield
                    act(e1, e1[:], e1[:], AF.Exp, [e1])
                    yield
                    tt("dve", fl, fl[:].rearrange("p (c t) -> p c t", t=128), nb[:].rearrange("p (c t) -> p c t", t=128), Rb, ALU.subtract, [nb, mu])
                    yield
                    act(fl, fl[:], fl[:], AF.Exp, [fl])
                    yield
                    P.op("dve", lambda: nc.vector.tensor_copy(out=gcar[0:4, 0:1], in_=nb[:, 511:512]), reads=[nb, gcar], writes=[gcar])
                    yield
                    P.op("dve", lambda: nc.vector.tensor_copy(out=gcar[0:4, 1:2], in_=mu[:, 511:512]), reads=[mu, gcar], writes=[gcar])
                    yield
                    for tb in range(4):
                        pt = psf.tiles[3]
                        P.op("pe", lambda: nc.tensor.transpose(out=pt[:, 0:4], in_=e1[0:4, tb * 128:(tb + 1) * 128], identity=consts[0:4, 0:4]),
                             reads=[e1, consts], writes=[pt])
                        P.op("pe", lambda: nc.tensor.transpose(out=pt[:, 4:8], in_=fl[0:4, tb * 128:(tb + 1) * 128], identity=consts[0:4, 0:4]),
                             reads=[fl, consts], writes=[pt])
                        P.op("dve", lambda: nc.vector.tensor_copy(out=tsc[tb][:], in_=pt[:, 0:8]), reads=[pt], writes=[tsc[tb]])


                    yield
                def sec_vo():
                    for tb in range(4):
                        blk = slice(tb * 128, (tb + 1) * 128)
                        psv = psf.tiles[4]
                        for kc in range(8):
                            mm(psv, psv[:], hT[kc][:, blk], wcol(kc, C_V, C_V + 512), [hT[kc], win[kc // 2]], start=(kc == 0), stop=(kc == 7))
                        act(vaug[tb], vaug[tb][:, :, 0:128], psv[:].rearrange("p (h d) -> p h d", d=128), AF.Copy, [psv])
                        if so:
                            yield
                            continue
                        pso = psf.tiles[5]
                        for kc in range(8):
                            mm(pso, pso[:], hT[kc][:, blk], wcol(kc, C_O, C_O + 512), [hT[kc], win[kc // 2]], start=(kc == 0), stop=(kc == 7))
                        act(go[tb], go[tb][:], pso[:], AF.Sigmoid, [pso])
                        tt("pool", go[tb], go[tb][:], go[tb][:], gmb[:], ALU.mult, [go[tb], gmb])
                        yield


                    yield
                gens = [sec_qk(), sec_gates(), sec_vo()]
                while gens:
                    for g_ in list(gens):
                        try:
                            next(g_)
                        except StopIteration:
                            gens.remove(g_)

                def lru_chunk(c):
                    ps = psf.tiles[3]
                    proj_fm(ps, 128, C_XL + c * 128)
                    yield
                    xc, r_t, a_, a2, i_t, hs = lt
                    gel = r_t
                    conv(ps, 8 + c, f"wcl{l}", f"bcl{l}", c, xc)
                    yield
                    xcb = xcb_ring.next()
                    yield
                    act(xcb, xcb[:], xc[:], AF.Copy, [xc])
                    yield
                    pa = psf.tiles[4]
                    yield
                    mm(pa, pa[:], bd_a[:, c, :], xcb[:], [bd_a, xcb])
                    yield
                    px = psf.tiles[5]
                    yield
                    mm(px, px[:], bd_x[:, c, :], xcb[:], [bd_x, xcb])
                    yield
                    act(r_t, r_t[:], pa[:], AF.Sigmoid, [pa, vec], bias=V(f"bla{l}", c))
                    yield
                    act(a_, a_[:], r_t[:], AF.Exp, [r_t, derived], scale=Dv(l, 48 + c))
                    yield
                    act(a2, a2[:], r_t[:], AF.Exp, [r_t, derived], scale=Dv(l, 52 + c))
                    yield
                    act(a2, a2[:], a2[:], AF.Sqrt, [a2], scale=-1.0, bias=1.0)
                    yield
                    act(i_t, i_t[:], px[:], AF.Sigmoid, [px, vec], bias=V(f"blx{l}", c))
                    yield
                    tt("pool", i_t, i_t[:], i_t[:], xc[:], ALU.mult, [i_t, xc])
                    yield
                    tt("pool", i_t, i_t[:], i_t[:], a2[:], ALU.mult, [i_t, a2])
                    yield
                    P.op("dve", lambda: nc.vector.tensor_tensor_scan(out=hs[:], data0=a_[:], data1=i_t[:], initial=lru_h[:, c:c + 1],
                                                                     op0=ALU.mult, op1=ALU.add), reads=[a_, i_t, lru_h], writes=[hs])
                    P.op("dve", lambda: nc.vector.tensor_copy(out=lru_h[:, c:c + 1], in_=hs[:, 511:512]), reads=[hs, lru_h], writes=[lru_h])
                    yield
                    if so:
                        return
                    psl = psf.tiles[3]
                    yield
                    proj_fm(psl, 128, C_GL + c * 128)
                    yield
                    act(gel, gel[:], psl[:], AF.Gelu_apprx_tanh, [psl])
                    yield
                    tt("pool", hl[c], hl[c][:], hs[:], gel[:], ALU.mult, [hs, gel])
                    yield

                def mlstm_tb(tb):
                    blk = slice(tb * 128, (tb + 1) * 128)
                    yield
                    e1b = tsc[tb][:, 0:4].unsqueeze(2).to_broadcast([128, 4, 128])
                    yield
                    pk = psb.next()
                    yield
                    for h in range(4):
                        P.op("pe", lambda: nc.tensor.transpose(out=pk[:, h * 128:(h + 1) * 128], in_=qk_bf[4 + h][:, blk], identity=ident_b[:]),
                             reads=[qk_bf[4 + h], ident_b], writes=[pk])
                    ke4 = ke_ring.next()
                    yield
                    tt("dve", ke4, ke4[:], pk[:, 0:512].rearrange("p (h d) -> p h d", d=128), e1b, ALU.mult, [pk, tsc[tb]])
                    yield
                    if not so:
                        pS = psf.tiles[0]
                        yield
                        for h in range(4):
                            mm(pS, pS[:, h * 128:(h + 1) * 128], qk_bf[4 + h][:, blk], qk_bf[h][:, blk], [qk_bf[4 + h], qk_bf[h]])
                        stmp = hm4_ring.next()
                        yield
                        tt("dve", stmp, stmp[:].rearrange("p (h d) -> p h d", d=128), pS[:].rearrange("p (h d) -> p h d", d=128), e1b, ALU.mult, [pS, tsc[tb]])
                        yield
                        stm4 = stm_ring.next()
                        yield
                        tt("dve", stm4, stm4[:], stmp[:].rearrange("p (h d) -> p h d", d=128),
                           mask_f.unsqueeze(1).to_broadcast([128, 4, 128]), ALU.mult, [stmp, consts])
                    tt("dve", Cst, Cst[:], Cst[:], abc[:, tb * 4:(tb + 1) * 4].unsqueeze(2).to_broadcast([128, 4, 129]), ALU.mult, [Cst, abc])
                    yield
                    if not so:
                        act(Cbf, Cbf[:], Cst[:], AF.Copy, [Cst])
                        yield
                        pOn = psf.tiles[1]
                        yield
                        pOd = psf.tiles[2]
                        yield
                        for h in range(4):
                            mm(pOn, pOn[:, h * 128:(h + 1) * 128], stm4[:, h, :], vaug[tb][:, h, 0:128], [stm4, vaug[tb]], start=True, stop=False)
                            mm(pOn, pOn[:, h * 128:(h + 1) * 128], qk_bf[h][:, blk], Cbf[:, h, 0:128], [qk_bf[h], Cbf], start=False, stop=True)
                        for h in range(4):
                            mm(pOd, pOd[:, 384 + h:385 + h], stm4[:, h, :], vaug[tb][:, h, 128:129], [stm4, vaug[tb]], start=True, stop=False)
                            mm(pOd, pOd[:, 384 + h:385 + h], qk_bf[h][:, blk], Cbf[:, h, 128:129], [qk_bf[h], Cbf], start=False, stop=True)
                    pU1 = psf.tiles[0]
                    yield
                    pU2 = psf.tiles[2]
                    yield
                    for h in range(4):
                        pu = pU1 if h < 2 else pU2
                        o_ = (h % 2) * 129
                        mm(pu, pu[:, o_:o_ + 129], ke4[:, h, :], vaug[tb][:, h, :], [ke4, vaug[tb]])
                    tt("dve", Cst, Cst[:, 0:2, :], Cst[:, 0:2, :], pU1[:, 0:258].rearrange("p (h j) -> p h j", j=129), ALU.add, [Cst, pU1])
                    yield
                    tt("dve", Cst, Cst[:, 2:4, :], Cst[:, 2:4, :], pU2[:, 0:258].rearrange("p (h j) -> p h j", j=129), ALU.add, [Cst, pU2])
                    yield
                    if so:
                        return
                    den = den_ring.next()
                    yield
                    act(den, den[:], pOd[:, 384:388], AF.Abs, [pOd])
                    yield
                    tt("dve", den, den[:], den[:], tsc[tb][:, 4:8], ALU.max, [den, tsc[tb]])
                    yield
                    P.op("dve", lambda: nc.vector.reciprocal(out=den[:], in_=den[:]), reads=[den], writes=[den])
                    yield
                    hm4 = hm4_ring.next()
                    yield
                    tt("dve", hm4, hm4[:].rearrange("p (h d) -> p h d", d=128), pOn[:].rearrange("p (h d) -> p h d", d=128),
                       den[:].unsqueeze(2).to_broadcast([128, 4, 128]), ALU.mult, [pOn, den])
                    sq4 = hm4_ring.next()
                    yield
                    act(sq4, sq4[:], hm4[:], AF.Square, [hm4])
                    yield
                    ssq = ssq_ring.next()
                    yield
                    P.op("dve", lambda: nc.vector.tensor_reduce(out=ssq[:], in_=sq4[:].rearrange("p (h d) -> p h d", d=128), axis=AX.X, op=ALU.add),
                         reads=[sq4], writes=[ssq])
                    act(ssq, ssq[:], ssq[:], AF.Sqrt, [ssq, epsc], scale=1.0 / 128, bias=epsc[:, 0:1])
                    yield
                    P.op("dve", lambda: nc.vector.reciprocal(out=ssq[:], in_=ssq[:]), reads=[ssq], writes=[ssq])
                    yield
                    tt("dve", hm4, hm4[:].rearrange("p (h d) -> p h d", d=128), hm4[:].rearrange("p (h d) -> p h d", d=128),
                       ssq[:].unsqueeze(2).to_broadcast([128, 4, 128]), ALU.mult, [hm4, ssq])
                    hmn = hmn_ring.next()
                    yield
                    tt("pool", hmn, hmn[:], hm4[:], go[tb][:], ALU.mult, [hm4, go[tb]])
                    yield
                    ph = psb.next()
                    yield
                    for h in range(4):
                        P.op("pe", lambda: nc.tensor.transpose(out=ph[:, h * 128:(h + 1) * 128], in_=hmn[:, h * 128:(h + 1) * 128], identity=ident_b[:]),
                             reads=[hmn, ident_b], writes=[ph])
                    act(hmix_m, hmix_m[:, :, blk], ph[:, 0:512].rearrange("p (h d) -> p h d", d=128), AF.Copy, [ph])
                    yield

                for tb in range(4):
                    gens = [mlstm_tb(tb), lru_chunk(tb)]
                    while gens:
                        for g_ in list(gens):
                            try:
                                next(g_)
                            except StopIteration:
                                gens.remove(g_)
                if so:
                    continue
                rms_rstd(None, lambda c: hl[c][:], 4, onesL, sq_ring, rstl, reads_extra=hl)
                for c in range(4):
                    stt(hmix_l[c], hmix_l[c][:], hl[c][:], V(f"gml{l}", c), rstl[:], ALU.mult, ALU.mult, [hl[c], vec, rstl])

                for dc in range(8):
                    ps = psf.next()
                    for kc in range(8):
                        hx_t = hmix_m if kc < 4 else hmix_l[kc - 4]
                        hx_ap = hmix_m[:, kc, :] if kc < 4 else hmix_l[kc - 4][:]
                        mm(ps, ps[:], wout[kc // 4][:, kc % 4, dc * 128:(dc + 1) * 128], hx_ap, [wout[kc // 4], hx_t],
                           start=(kc == 0), stop=(kc == 7))
                    stt(xt, xt[:, dc, :], ps[:], Dv(l, 16 + dc), xt[:, dc, :], ALU.mult, ALU.add, [ps, derived, xt])
                P.dma("sp", dst_v[:, :, t0:t0 + 512], xt[:], reads=[xt], writes=[dst_blocks[s]])

            sto = st_dst
            swr = [st_dst_tile] if st_dst_tile is not None else []
            P.dma("sp", sto[:, 0:516], Cst[:].rearrange("p h j -> p (h j)"), reads=[Cst], writes=swr)
            P.dma("sp", sto[:, 516:520], lru_h[:], reads=[lru_h], writes=swr)
            P.dma("sp", sto[:, 520:556], tails[:].rearrange("p c j -> p (c j)"), reads=[tails], writes=swr)
            mend = P.sb([128, 4], F32, "mend")
            P.op("dve", lambda: nc.vector.memset(mend[:], 0.0), writes=[mend])
            tt("dve", mend, mend[0:4, 0:1], gcar[0:4, 1:2], gcar[0:4, 0:1], ALU.subtract, [gcar, mend])
            P.dma("sp", sto[:, 556:560], mend[:], reads=[mend], writes=swr)
            P.barrier()
        P.tes = es

    def ffn_phase(l, moe, final, buf_v, buf_blocks):
        with contextlib.ExitStack() as es2:
            P.tes = es2
            xF = P.sb([128, 8, TT], F32, "xF")
            hF = [P.sb([128, TT], BF16, "hF") for _ in range(8)]
            tmp_ring = Ring([P.sb([128, 512], F32, "tmp") for _ in range(3)])
            sq_ring = tmp_ring
            rstd = P.sb([128, 512], F32, "rstd")
            wg_ring = Ring([P.sb([128, 8, JGW], BF16, "wg") for _ in range(3)])
            wu_ring = Ring([P.sb([128, 8, JGW], BF16, "wu") for _ in range(3)])
            wd_ring = Ring([P.sb([128, 2, 1024], BF16, "wd") for _ in range(4)])
            sg_ring = Ring([P.sb([128, 512], F32, "sg") for _ in range(3)])
            act_ring = Ring([P.sb([128, 512], BF16, "actb") for _ in range(4 * NB)])
            if moe:
                hf32 = [P.sb([128, 512], F32, "hf32") for _ in range(8)]
                wr = P.sb([128, 8, 8], F32, "wr")
                P.dma("sp", wr[:], w_r_d.rearrange("(kc p) e -> p kc e", p=128), writes=[wr])
                brb = P.sb([128, 8], F32, "brb")
                P.dma("sp", brb[:], brb_d, writes=[brb])
                gTs = P.sb([8, TT], F32, "gTs")
                gb = [P.sb([128, TT], F32, "gb") for _ in range(NE)]
                lg = P.sb([128, 8], F32, "lg")
                lg2 = P.sb([128, 8], F32, "lg2")
                m1 = P.sb([128, 1], F32, "m1")
                m2 = P.sb([128, 1], F32, "m2")
                selt = P.sb([128, 8], F32, "selt")
                pe_ = P.sb([128, 8], F32, "pe_")
                dsum = P.sb([128, 1], F32, "dsum")
                sg2_ring = Ring([P.sb([128, 512], F32, "sg2") for _ in range(3)])
            n_exp = NE if moe else 1

            for tti in range(NTT):
                T0 = tti * TT
                blocks = [buf_blocks[(T0 // 512) + n] for n in range(NB)]
                P.dma("sp", xF[:], buf_v[:, :, T0:T0 + TT], reads=blocks, writes=[xF])
                for n in range(NB):
                    nb_ = slice(n * 512, (n + 1) * 512)
                    rms_rstd(xF, lambda kc: xF[:, kc, nb_], 8, onesD, sq_ring, rstd)
                    for kc in range(8):
                        tmp = tmp_ring.next()
                        tt("dve", tmp, tmp[:], xF[:, kc, nb_], rstd[:], ALU.mult, [xF, rstd])
                        if moe:
                            act(hf32[kc], hf32[kc][:], tmp[:], AF.Identity, [tmp, derived], scale=Dv(l, 24 + kc), bias=Dv(l, 32 + kc))
                            P.op("pool", lambda: nc.gpsimd.tensor_copy(out=hF[kc][:, nb_], in_=hf32[kc][:]), reads=[hf32[kc]], writes=[hF[kc]])
                        else:
                            act(hF[kc], hF[kc][:, nb_], tmp[:], AF.Identity, [tmp, derived], scale=Dv(l, 24 + kc), bias=Dv(l, 32 + kc))
                    if moe:
                        for tb in range(4):
                            tblk = slice(tb * 128, (tb + 1) * 128)
                            pr = psf.next()
                            for kc in range(8):
                                mm(pr, pr[:, 0:8], hf32[kc][:, tblk], wr[:, kc, :], [hf32[kc], wr], start=(kc == 0), stop=(kc == 7))
                            tt("dve", lg, lg[:], pr[:, 0:8], brb[:], ALU.add, [pr, brb])
                            P.op("dve", lambda: nc.vector.tensor_reduce(out=m1[:], in_=lg[:], axis=AX.X, op=ALU.max), reads=[lg], writes=[m1])
                            ts(selt, selt[:], lg[:], m1[:, 0:1], -1e30, ALU.is_ge, ALU.mult, [lg, m1])
                            tt("dve", lg2, lg2[:], lg[:], selt[:], ALU.add, [lg, selt])
                            P.op("dve", lambda: nc.vector.tensor_reduce(out=m2[:], in_=lg2[:], axis=AX.X, op=ALU.max), reads=[lg2], writes=[m2])
                            ts(selt, selt[:], lg[:], m2[:, 0:1], None, ALU.is_ge, None, [lg, m2])
                            ts(m1, m1[:], m1[:], -1.0, None, ALU.mult, None, [m1])
                            act(pe_, pe_[:], lg[:], AF.Exp, [lg, m1], bias=m1[:, 0:1], scale=1.0)
                            tt("dve", pe_, pe_[:], pe_[:], selt[:], ALU.mult, [pe_, selt])
                            P.op("dve", lambda: nc.vector.tensor_reduce(out=dsum[:], in_=pe_[:], axis=AX.X, op=ALU.add), reads=[pe_], writes=[dsum])
                            P.op("dve", lambda: nc.vector.reciprocal(out=dsum[:], in_=dsum[:]), reads=[dsum], writes=[dsum])
                            ts(pe_, pe_[:], pe_[:], dsum[:, 0:1], None, ALU.mult, None, [pe_, dsum])
                            pg = psf.next()
                            P.op("pe", lambda: nc.tensor.transpose(out=pg[0:8, 0:128], in_=pe_[:], identity=ident_f), reads=[pe_, consts], writes=[pg])
                            c0 = n * 512 + tb * 128
                            P.op("dve", lambda: nc.vector.tensor_copy(out=gTs[:, c0:c0 + 128], in_=pg[0:8, 0:128]), reads=[pg], writes=[gTs])
                        for e in range(NE):
                            pb = psf.next()
                            mm(pb, pb[:], consts[0:8, 256 + e * 128:256 + (e + 1) * 128], gTs[:, nb_], [consts, gTs])
                            act(gb[e], gb[e][:, nb_], pb[:], AF.Copy, [pb])

                stages = [(e, jg) for e in range(n_exp) for jg in range(NJG)]
                loaded = {}

                def load(k):
                    e, jg = stages[k]
                    wg = wg_ring.next()
                    wu = wu_ring.next()
                    wd = wd_ring.next()
                    if moe:
                        P.dma("pool", wg[:], exg_d[e, jg], writes=[wg])
                        P.dma("pool", wu[:], exu_d[e, jg], writes=[wu])
                        P.dma("pool", wd[:], exd_d[e, jg * JGW:(jg + 1) * JGW, :].rearrange("(c p) n -> p c n", p=128), writes=[wd])
                    else:
                        P.dma("pool", wg[:], ffg_d[jg], writes=[wg])
                        P.dma("pool", wu[:], ffu_d[jg], writes=[wu])
                        P.dma("pool", wd[:], ffd_d[jg * JGW:(jg + 1) * JGW, :].rearrange("(c p) n -> p c n", p=128), writes=[wd])
                    loaded[k] = (wg, wu, wd)

                load(0)
                if len(stages) > 1:
                    load(1)
                def down(wd, acts):
                    for n in range(NB):
                        nb_ = slice(n * 512, (n + 1) * 512)
                        for dc in range(8):
                            py = psf.next()
                            for jj in range(2):
                                mm(py, py[:], wd[:, jj, dc * 128:(dc + 1) * 128], acts[(jj, n)][:], [wd, acts[(jj, n)]], start=(jj == 0), stop=(jj == 1))
                            stt(xF, xF[:, dc, nb_], py[:], Dv(l, 40 + dc), xF[:, dc, nb_], ALU.mult, ALU.add, [py, derived, xF])

                pending = None
                for k in range(len(stages)):
                    if k + 2 < len(stages):
                        load(k + 2)
                    e, jg = stages[k]
                    wg, wu, wd = loaded.pop(k)
                    acts = {}
                    for jj in range(2):
                        for n in range(NB):
                            nb_ = slice(n * 512, (n + 1) * 512)
                            pgp = psf.next()
                            for kc in range(8):
                                mm(pgp, pgp[:], wg[:, kc, jj * 128:(jj + 1) * 128], hF[kc][:, nb_], [wg, hF[kc]], start=(kc == 0), stop=(kc == 7))
                            pup = psf.next()
                            for kc in range(8):
                                mm(pup, pup[:], wu[:, kc, jj * 128:(jj + 1) * 128], hF[kc][:, nb_], [wu, hF[kc]], start=(kc == 0), stop=(kc == 7))
                            sg = sg_ring.next()
                            act(sg, sg[:], pgp[:], AF.Silu, [pgp])
                            ab = act_ring.next()
                            if moe:
                                sg2 = sg2_ring.next()
                                tt("dve", sg2, sg2[:], sg[:], pup[:], ALU.mult, [sg, pup])
                                tt("pool", ab, ab[:], sg2[:], gb[e][:, nb_], ALU.mult, [sg2, gb[e]])
                            else:
                                tt("dve", ab, ab[:], sg[:], pup[:], ALU.mult, [sg, pup])
                            acts[(jj, n)] = ab
                    if pending is not None:
                        down(*pending)
                    pending = (wd, acts)
                down(*pending)

                if final:
                    for n in range(NB):
                        nb_ = slice(n * 512, (n + 1) * 512)
                        rms_rstd(xF, lambda kc: xF[:, kc, nb_], 8, onesD, sq_ring, rstd)
                        for kc in range(8):
                            stt(xF, xF[:, kc, nb_], xF[:, kc, nb_], V("gfin", kc), rstd[:], ALU.mult, ALU.mult, [xF, vec, rstd])
                P.dma("sp", buf_v[:, :, T0:T0 + TT], xF[:], reads=[xF], writes=blocks)
            P.barrier()
        P.tes = es

    if prefix:
        for l in range(NL):
            mixer_phase(l, xpT_v if l == 0 else preT_v, None if l == 0 else dpre, preT_v, dpre,
                        st_in_d[l], None, st_pre_d[l], dstp[l], False, so=(l == NL - 1))
            if l < NL - 1:
                ffn_phase(l, moe=(l in moe_layers), final=False, buf_v=preT_v, buf_blocks=dpre)
    for l in range(NL):
        if not _SKIP_MIX:
            if prefix:
                mixer_phase(l, xT_v if l == 0 else outT_v, None if l == 0 else dx, outT_v, dx,
                            st_pre_d[l], dstp[l], st_out_d[l], None, True)
            else:
                mixer_phase(l, xT_v if l == 0 else outT_v, None if l == 0 else dx, outT_v, dx,
                            st_in_d[l], None, st_out_d[l], None, False)
        if not _SKIP_FFN:
            ffn_phase(l, moe=(l in moe_layers), final=(l == NL - 1), buf_v=outT_v, buf_blocks=dx)
    P.barrier()
    es.close()
    return nc


_NC_CACHE = {}


def _prep_shared(inp, NL):
    sh = {}
    sh["w_ada"] = np.ascontiguousarray(inp["w_ada"], dtype=np.float32)
    sh["w_in"] = np.ascontiguousarray(inp["w_in"], dtype=np.float32)
    sh["w_out"] = np.ascontiguousarray(inp["w_out"], dtype=np.float32)
    sh["w_lru_a"] = np.ascontiguousarray(inp["w_lru_a"], dtype=np.float32)
    sh["w_lru_x"] = np.ascontiguousarray(inp["w_lru_x"], dtype=np.float32)

    def grp(w):
        return np.ascontiguousarray(w.reshape(8, 128, NJG, JGW).transpose(2, 1, 0, 3))

    sh["ffg"] = grp(np.asarray(inp["w_ff_gate"][0], np.float32))
    sh["ffu"] = grp(np.asarray(inp["w_ff_up"][0], np.float32))
    sh["ffd"] = np.ascontiguousarray(inp["w_ff_down"][0], dtype=np.float32)
    sh["w_router"] = np.ascontiguousarray(inp["w_router"][0], dtype=np.float32)
    sh["brb"] = np.ascontiguousarray(np.broadcast_to(np.asarray(inp["b_router"][0], np.float32)[None, :], (128, 8)))
    sh["exg"] = np.stack([grp(np.asarray(inp["w_exp_gate"][0, e], np.float32)) for e in range(NE)])
    sh["exu"] = np.stack([grp(np.asarray(inp["w_exp_up"][0, e], np.float32)) for e in range(NE)])
    sh["exd"] = np.ascontiguousarray(inp["w_exp_down"][0], dtype=np.float32)
    sh["gmixb"] = np.ascontiguousarray(np.broadcast_to(np.asarray(inp["g_mix_out"], np.float32)[:, None, :512], (NL, 128, 512)))
    consts = np.zeros((128, 256 + 1024), np.float32)
    consts[:, 0:128] = np.eye(128, dtype=np.float32)
    consts[:, 128:256] = np.triu(np.ones((128, 128), np.float32))
    for e in range(NE):
        consts[e, 256 + e * 128:256 + (e + 1) * 128] = 1.0
    sh["consts"] = consts
    return sh


def _vecs(inp, b, NL, flag=0.0):
    lay, NV = vec_layout(NL)
    v = np.zeros((128, NV), np.float32)

    def put(name, arr):
        o, n = lay[name]
        assert arr.shape == (128, n), (name, arr.shape, n)
        v[:, o:o + n] = arr

    put("cond", fm(inp["c"][b], 8))
    for l in range(NL):
        put(f"bada{l}", fm(inp["b_ada"][l], 48))
        put(f"gm{l}", fm(inp["g_norm_mix"][l], 8))
        put(f"gf{l}", fm(inp["g_norm_ffn"][l], 8))
        w = np.asarray(inp["w_conv_qk"][l], np.float32)
        put(f"wcqk{l}", np.ascontiguousarray(w.T.reshape(8, 128, 4).transpose(1, 0, 2).reshape(128, 32)))
        put(f"bcqk{l}", fm(inp["b_conv_qk"][l], 8))
        w = np.asarray(inp["w_conv_lru"][l], np.float32)
        put(f"wcl{l}", np.ascontiguousarray(w.T.reshape(4, 128, 4).transpose(1, 0, 2).reshape(128, 16)))
        put(f"bcl{l}", fm(inp["b_conv_lru"][l], 4))
        put(f"bla{l}", fm(inp["b_lru_a"][l], 4))
        put(f"blx{l}", fm(inp["b_lru_x"][l], 4))
        put(f"lam{l}", fm(inp["lru_lambda"][l], 4))
        put(f"gml{l}", fm(inp["g_mix_out"][l][512:], 4))
        bg = np.zeros((128, 2), np.float32)
        bg[0:4, 0] = inp["b_gates"][l][0:4]
        bg[0:4, 1] = inp["b_gates"][l][4:8]
        put(f"bg{l}", bg)
    put("gfin", fm(inp["g_final"], 8))
    put("flag", np.full((128, 1), flag, np.float32))
    return v


def kernel(**inputs):
    x = np.asarray(inputs["x"], np.float32)
    B, S, D = x.shape
    NL = int(np.asarray(inputs["w_in"]).shape[0])
    n_cores = 2 * B
    T = S // 2
    key = (T, NL)
    if key not in _NC_CACHE:
        _NC_CACHE[key] = build(T, NL)
    nc = _NC_CACHE[key]
    sh = _prep_shared(inputs, NL)
    zeros_st = np.zeros((NL, 128, SW), np.float32)
    zeros_x = np.zeros((D, T), np.float32)
    in_maps = []
    for cid in range(n_cores):
        b, half = cid // 2, cid % 2
        m = dict(sh)
        m["xT"] = np.ascontiguousarray(x[b, half * T:(half + 1) * T, :].T)
        m["xpT"] = np.ascontiguousarray(x[b, 0:T, :].T) if half == 1 else zeros_x
        m["vecs"] = _vecs(inputs, b, NL, flag=float(half))
        m["st_in"] = zeros_st
        in_maps.append(m)
    res = run_bass_kernel_spmd(nc, in_maps, core_ids=list(range(n_cores)))
    out = np.empty((B, S, D), np.float32)
    for cid in range(n_cores):
        b, half = cid // 2, cid % 2
        out[b, half * T:(half + 1) * T, :] = res.results[cid]["outT"].T
    return out
```

```python
import contextlib
import math
import numpy as np
import concourse.bass as bass
import concourse.mybir as mybir
from concourse.bass_utils import run_bass_kernel_spmd

F32 = mybir.dt.float32
BF16 = mybir.dt.bfloat16
ALU = mybir.AluOpType
AF = mybir.ActivationFunctionType
AX = mybir.AxisListType

D_MODEL = 1024
D_FF = 2816
NE = 8
EPS = 1e-6
D_IN = 3080
SW = 560
C_Q, C_K, C_V, C_O, C_IG, C_FG, C_XL, C_GL = 0, 512, 1024, 1536, 2048, 2052, 2056, 2568
JGW = 256
NJG = D_FF // JGW

SAME_ENGINE_SYNC = True
EPOCH = 16000
_SKIP_MIX = False
_DEBUG_MAP = None
_SKIP_FFN = False


class Tl:
    __slots__ = ("t", "w", "r", "name")

    def __init__(self, t, name):
        self.t = t
        self.w = None
        self.r = {}
        self.name = name

    def __getitem__(self, k):
        return self.t[k]


class Ring:
    def __init__(self, tiles):
        self.tiles = tiles
        self.i = 0

    def next(self):
        t = self.tiles[self.i % len(self.tiles)]
        self.i += 1
        return t


class Prog:
    def __init__(self, nc, es, nslots=24):
        self.nc = nc
        self.es = es
        self.tes = es
        self.eng = {"pe": nc.tensor, "act": nc.scalar, "dve": nc.vector, "pool": nc.gpsimd, "sp": nc.sync}
        self.sems = {}
        self.cnt = {e: 0 for e in self.eng}
        self.seen = {e: {} for e in self.eng}
        self.nslots = 2 * nslots
        self.slot_sem = [es.enter_context(nc.semaphore(f"dq{i}")) for i in range(2 * nslots)]
        self.slot_cnt = [0] * (2 * nslots)
        self.q_slots = {"sp": list(range(0, nslots)), "pool": list(range(nslots, 2 * nslots))}
        self.q_next = {"sp": 0, "pool": 0}
        self.uid = 0
        self.dbg = None

    def _esem(self, e, ep):
        k = (e, ep)
        if k not in self.sems:
            self.sems[k] = self.es.enter_context(self.nc.semaphore(f"s_{e}_{ep}"))
        return self.sems[k]

    def sb(self, shape, dtype, name=None):
        self.uid += 1
        name = f"{name or 't'}_{self.uid}"
        return Tl(self.tes.enter_context(self.nc.sbuf_tensor(name, list(shape), dtype)), name)

    def ps(self, shape, dtype, name=None):
        self.uid += 1
        name = f"{name or 'p'}_{self.uid}"
        return Tl(self.tes.enter_context(self.nc.psum_tensor(name, list(shape), dtype)), name)

    def pseudo(self, name):
        return Tl(None, name)

    def _wait(self, e, dep):
        kind, key, val = dep
        k = (kind, key)
        if self.seen[e].get(k, 0) >= val:
            return
        sem = self._esem(key[0], key[1]) if kind == "eng" else self.slot_sem[key]
        self.eng[e].wait_ge(sem, val)
        self.seen[e][k] = val

    def _deps(self, e, reads, writes, force_same=False):
        deps = []
        for t in reads:
            if t.w is not None:
                deps.append(t.w)
        for t in writes:
            if t.w is not None:
                deps.append(t.w)
            for k, v in t.r.items():
                deps.append((k[0], k[1], v))
        for d in deps:
            if d[0] == "eng" and d[1][0] == e and not force_same and (e == "pe" or not SAME_ENGINE_SYNC):
                continue
            self._wait(e, d)

    def _mark(self, me, reads, writes):
        k = (me[0], me[1])
        for t in reads:
            if t.r.get(k, 0) < me[2]:
                t.r[k] = me[2]
        for t in writes:
            t.w = me
            t.r = {}

    def _cur(self, e):
        c = self.cnt[e]
        ep = (c - 1) // EPOCH
        return ("eng", (e, ep), c - ep * EPOCH)

    def op(self, e, fn, reads=(), writes=()):
        self._deps(e, reads, writes)
        ins = fn()
        if self.dbg is not None:
            import sys as _sys
            f = _sys._getframe(1)
            chain = []
            while f is not None and len(chain) < 4:
                if f.f_code.co_name not in ("mm", "act", "tt", "ts", "stt"):
                    chain.append(f.f_lineno)
                f = f.f_back
            try:
                self.dbg[ins.ins.name] = chain
            except Exception:
                pass
        self.cnt[e] += 1
        me = self._cur(e)
        ins.then_inc(self._esem(e, me[1][1]), 1)
        self._mark(me, reads, writes)
        return me

    def dma(self, q, out, in_, reads=(), writes=(), slow=False):
        pool_ = self.q_slots[q]
        slot = pool_[self.q_next[q] % len(pool_)]
        self.q_next[q] += 1
        if self.slot_cnt[slot] > 0:
            self._wait(q, ("dma", slot, 16 * self.slot_cnt[slot]))
        self._deps(q, reads, writes, force_same=True)
        if slow:
            ins = self.eng[q].dma_start(out=out, in_=in_, allow_slow_non_contiguous=True)
        else:
            ins = self.eng[q].dma_start(out=out, in_=in_)
        self.slot_cnt[slot] += 1
        ins.then_inc(self.slot_sem[slot], 16)
        me = ("dma", slot, 16 * self.slot_cnt[slot])
        self._mark(me, reads, writes)
        return me

    def barrier(self):
        deps = []
        for e in self.eng:
            if self.cnt[e] > 0:
                deps.append(self._cur(e))
        for s in range(self.nslots):
            if self.slot_cnt[s] > 0:
                deps.append(("dma", s, 16 * self.slot_cnt[s]))
        for e in self.eng:
            for d in deps:
                self._wait(e, d)


def vec_layout(NL):
    lay = {}
    off = 0

    def add(name, n):
        nonlocal off
        lay[name] = (off, n)
        off += n

    add("cond", 8)
    for l in range(NL):
        add(f"bada{l}", 48)
        add(f"gm{l}", 8)
        add(f"gf{l}", 8)
        add(f"wcqk{l}", 32)
        add(f"bcqk{l}", 8)
        add(f"wcl{l}", 16)
        add(f"bcl{l}", 4)
        add(f"bla{l}", 4)
        add(f"blx{l}", 4)
        add(f"lam{l}", 4)
        add(f"gml{l}", 4)
        add(f"bg{l}", 2)
    add("gfin", 8)
    add("flag", 1)
    return lay, off


def fm(v, nch):
    return np.ascontiguousarray(np.asarray(v, np.float32).reshape(nch, 128).T)


def build(T, NL, moe_layers=(1,), prefix=True):
    NSUB = T // 512
    TT = min(1024, T)
    NTT = T // TT
    NB = TT // 512
    lay, NV = vec_layout(NL)

    nc = bass.Bass("TRN2", target_bir_lowering=False)
    DT = nc.dram_tensor
    xT = DT("xT", [1024, T], F32, kind="ExternalInput").ap()
    xpT = DT("xpT", [1024, T], F32, kind="ExternalInput").ap()
    preT = DT("preT", [1024, T], F32, kind="Internal").ap()
    st_pre_d = DT("st_pre", [NL, 128, SW], F32, kind="Internal").ap()
    vecs_d = DT("vecs", [128, NV], F32, kind="ExternalInput").ap()
    consts_d = DT("consts", [128, 256 + 1024], F32, kind="ExternalInput").ap()
    gmixb_d = DT("gmixb", [NL, 128, 512], F32, kind="ExternalInput").ap()
    brb_d = DT("brb", [128, 8], F32, kind="ExternalInput").ap()
    w_ada_d = DT("w_ada", [NL, 1024, 6144], F32, kind="ExternalInput").ap()
    w_in_d = DT("w_in", [NL, 1024, D_IN], F32, kind="ExternalInput").ap()
    w_out_d = DT("w_out", [NL, 1024, 1024], F32, kind="ExternalInput").ap()
    w_la_d = DT("w_lru_a", [NL, 8, 64, 64], F32, kind="ExternalInput").ap()
    w_lx_d = DT("w_lru_x", [NL, 8, 64, 64], F32, kind="ExternalInput").ap()
    ffg_d = DT("ffg", [NJG, 128, 8, JGW], F32, kind="ExternalInput").ap()
    ffu_d = DT("ffu", [NJG, 128, 8, JGW], F32, kind="ExternalInput").ap()
    ffd_d = DT("ffd", [D_FF, 1024], F32, kind="ExternalInput").ap()
    w_r_d = DT("w_router", [1024, 8], F32, kind="ExternalInput").ap()
    exg_d = DT("exg", [NE, NJG, 128, 8, JGW], F32, kind="ExternalInput").ap()
    exu_d = DT("exu", [NE, NJG, 128, 8, JGW], F32, kind="ExternalInput").ap()
    exd_d = DT("exd", [NE, D_FF, 1024], F32, kind="ExternalInput").ap()
    st_in_d = DT("st_in", [NL, 128, SW], F32, kind="ExternalInput").ap()
    outT = DT("outT", [1024, T], F32, kind="ExternalOutput").ap()
    st_out_d = DT("st_out", [NL, 128, SW], F32, kind="ExternalOutput").ap()

    xT_v = xT.rearrange("(kc p) t -> p kc t", p=128)
    xpT_v = xpT.rearrange("(kc p) t -> p kc t", p=128)
    preT_v = preT.rearrange("(kc p) t -> p kc t", p=128)
    outT_v = outT.rearrange("(kc p) t -> p kc t", p=128)

    es = contextlib.ExitStack()
    P = Prog(nc, es)
    if _DEBUG_MAP is not None:
        P.dbg = _DEBUG_MAP
    dx = [P.pseudo(f"dx{s}") for s in range(NSUB)]
    dpre = [P.pseudo(f"dpre{s}") for s in range(NSUB)]
    dstp = [P.pseudo(f"dstp{l}") for l in range(NL)]

    consts = P.sb([128, 256 + 1024], F32, "consts")
    vec = P.sb([128, NV], F32, "vec")
    P.dma("sp", consts[:], consts_d, writes=[consts])
    P.dma("sp", vec[:], vecs_d, writes=[vec])
    ident_f = consts[:, 0:128]
    mask_f = consts[:, 128:256]
    ident_b = P.sb([128, 128], BF16, "identb")
    P.op("dve", lambda: nc.vector.tensor_copy(out=ident_b[:], in_=ident_f), reads=[consts], writes=[ident_b])
    onesD = P.sb([128, 128], F32, "onesD")
    onesL = P.sb([128, 128], F32, "onesL")
    ones1 = P.sb([128, 512], F32, "ones1")
    epsc = P.sb([128, 1], F32, "epsc")
    P.op("dve", lambda: nc.vector.memset(onesD[:], 1.0 / 1024), writes=[onesD])
    P.op("dve", lambda: nc.vector.memset(onesL[:], 1.0 / 512), writes=[onesL])
    P.op("dve", lambda: nc.vector.memset(ones1[:], 1.0), writes=[ones1])
    P.op("dve", lambda: nc.vector.memset(epsc[:], EPS), writes=[epsc])

    def V(name, j=0, n=1):
        o, _ = lay[name]
        return vec[:, o + j:o + j + n]

    psf = Ring([P.ps([128, 512], F32, "psf") for _ in range(6)])
    psb = Ring([P.ps([128, 1024], BF16, "psb") for _ in range(2)])

    def mm(o_t, o_ap, l_ap, r_ap, reads, start=True, stop=True):
        P.op("pe", lambda: nc.tensor.matmul(o_ap, l_ap, r_ap, start=start, stop=stop), reads=reads, writes=[o_t])

    def act(o_t, o_ap, i_ap, func, reads, scale=None, bias=None, accum=None, extra_w=()):
        kw = {}
        if scale is not None:
            kw["scale"] = scale
        if bias is not None:
            kw["bias"] = bias
        if accum is not None:
            kw["accum_out"] = accum
        P.op("act", lambda: nc.scalar.activation(out=o_ap, in_=i_ap, func=func, **kw), reads=reads,
             writes=[o_t] + list(extra_w))

    def tt(e, o_t, o_ap, a_ap, b_ap, op, reads):
        eng = nc.vector if e == "dve" else nc.gpsimd
        P.op(e, lambda: eng.tensor_tensor(out=o_ap, in0=a_ap, in1=b_ap, op=op), reads=reads, writes=[o_t])

    def ts(o_t, o_ap, a_ap, s1, s2, op0, op1, reads, e="dve"):
        eng = nc.vector if e == "dve" else nc.gpsimd
        if op1 is None:
            P.op(e, lambda: eng.tensor_scalar(out=o_ap, in0=a_ap, scalar1=s1, scalar2=None, op0=op0), reads=reads, writes=[o_t])
        else:
            P.op(e, lambda: eng.tensor_scalar(out=o_ap, in0=a_ap, scalar1=s1, scalar2=s2, op0=op0, op1=op1), reads=reads, writes=[o_t])

    def stt(o_t, o_ap, a_ap, s, b_ap, op0, op1, reads):
        P.op("dve", lambda: nc.vector.scalar_tensor_tensor(out=o_ap, in0=a_ap, scalar=s, in1=b_ap, op0=op0, op1=op1),
             reads=reads, writes=[o_t])

    mod = P.sb([128, NL * 48], F32, "mod")
    derived = P.sb([128, NL * 64], F32, "derived")
    with contextlib.ExitStack() as es2:
        P.tes = es2
        cond = P.sb([128, 8], F32, "cond")
        act(cond, cond[:], V("cond", 0, 8), AF.Silu, [vec])
        wa_ring = Ring([P.sb([128, 8, 1024], F32, "wada") for _ in range(2)])
        pmod = psf.next()
        for l in range(NL):
            for sl in range(6):
                wa = wa_ring.next()
                P.dma("sp", wa[:], w_ada_d[l].rearrange("(kc p) n -> p kc n", p=128)[:, :, sl * 1024:(sl + 1) * 1024], writes=[wa])
                for j in range(8):
                    col = l * 48 + sl * 8 + j
                    for kc in range(8):
                        mm(pmod, pmod[:, col:col + 1], wa[:, kc, j * 128:(j + 1) * 128], cond[:, kc:kc + 1], [wa, cond],
                           start=(kc == 0), stop=(kc == 7))
        for l in range(NL):
            o = lay[f"bada{l}"][0]
            tt("dve", mod, mod[:, l * 48:(l + 1) * 48], pmod[:, l * 48:(l + 1) * 48], vec[:, o:o + 48], ALU.add, [pmod, vec])
            m0 = l * 48
            d0 = l * 64
            stt(derived, derived[:, d0:d0 + 8], mod[:, m0 + 8:m0 + 16], 1.0, V(f"gm{l}", 0, 8), ALU.add, ALU.mult, [mod, vec])
            P.op("dve", lambda: nc.vector.tensor_copy(out=derived[:, d0 + 8:d0 + 16], in_=mod[:, m0:m0 + 8]), reads=[mod], writes=[derived])
            P.op("dve", lambda: nc.vector.tensor_copy(out=derived[:, d0 + 16:d0 + 24], in_=mod[:, m0 + 16:m0 + 24]), reads=[mod], writes=[derived])
            stt(derived, derived[:, d0 + 24:d0 + 32], mod[:, m0 + 32:m0 + 40], 1.0, V(f"gf{l}", 0, 8), ALU.add, ALU.mult, [mod, vec])
            P.op("dve", lambda: nc.vector.tensor_copy(out=derived[:, d0 + 32:d0 + 40], in_=mod[:, m0 + 24:m0 + 32]), reads=[mod], writes=[derived])
            P.op("dve", lambda: nc.vector.tensor_copy(out=derived[:, d0 + 40:d0 + 48], in_=mod[:, m0 + 40:m0 + 48]), reads=[mod], writes=[derived])
            tmp4 = P.sb([128, 4], F32, "tmp4")
            act(tmp4, tmp4[:], V(f"lam{l}", 0, 4), AF.Exp, [vec], scale=-1.0)
            act(tmp4, tmp4[:], tmp4[:], AF.Ln, [tmp4], bias=1.0)
            ts(derived, derived[:, d0 + 48:d0 + 52], tmp4[:], -8.0, None, ALU.mult, None, [tmp4])
            ts(derived, derived[:, d0 + 52:d0 + 56], tmp4[:], -16.0, None, ALU.mult, None, [tmp4])
            ts(derived, derived[:, d0 + 56:d0 + 57], V(f"bg{l}", 1, 1), -1.0, None, ALU.mult, None, [vec])
        P.barrier()
    P.tes = es

    def Dv(l, j, n=1):
        return derived[:, l * 64 + j:l * 64 + j + n]

    def rms_rstd(x_t, x_ap_of_kc, nk, ones_t, sq_ring, rstd_t, reads_extra=()):
        ss = psf.next()
        for kc in range(nk):
            sq = sq_ring.next()
            act(sq, sq[:], x_ap_of_kc(kc), AF.Square, ([x_t] if x_t is not None else []) + list(reads_extra))
            mm(ss, ss[:], ones_t[:], sq[:], [ones_t, sq], start=(kc == 0), stop=(kc == nk - 1))
        act(rstd_t, rstd_t[:], ss[:], AF.Sqrt, [ss, epsc], bias=epsc[:, 0:1], scale=1.0)
        P.op("dve", lambda: nc.vector.reciprocal(out=rstd_t[:], in_=rstd_t[:]), reads=[rstd_t], writes=[rstd_t])

    def mixer_phase(l, src_v, src_blocks, dst_v, dst_blocks, st_src, st_src_tile, st_dst, st_dst_tile, use_flag, so=False):
        with contextlib.ExitStack() as es2:
            P.tes = es2
            win = [P.sb([128, 2, D_IN], BF16, "win") for _ in range(4)]
            wout = [P.sb([128, 4, 1024], BF16, "wout") for _ in range(2)]
            w_in_v = w_in_d[l].rearrange("(kc p) n -> p kc n", p=128)
            w_out_v = w_out_d[l].rearrange("(kc p) n -> p kc n", p=128)
            for i in range(4):
                P.dma("pool", win[i][:], w_in_v[:, 2 * i:2 * i + 2, :], writes=[win[i]])
            for i in range(2):
                P.dma("pool", wout[i][:], w_out_v[:, 4 * i:4 * i + 4, :], writes=[wout[i]])
            bd_a = P.sb([128, 4, 128], BF16, "bda")
            bd_x = P.sb([128, 4, 128], BF16, "bdx")
            for bd, wd in ((bd_a, w_la_d), (bd_x, w_lx_d)):
                P.op("dve", lambda: nc.vector.memset(bd[:], 0.0), writes=[bd])
                wv = wd[l].rearrange("(c two) k m -> two k c m", two=2)
                P.dma("pool", bd[0:64, :, 0:64], wv[0], writes=[bd])
                P.dma("pool", bd[64:128, :, 64:128], wv[1], reads=[bd], writes=[bd])
            gmb = P.sb([128, 512], F32, "gmb")
            P.dma("sp", gmb[:], gmixb_d[l], writes=[gmb])

            def wcol(kc, c0, c1):
                return win[kc // 2][:, kc % 2, c0:c1]

            Cst = P.sb([128, 4, 129], F32, "Cst")
            Cbf = P.sb([128, 4, 129], BF16, "Cbf")
            lru_h = P.sb([128, 4], F32, "lruh")
            gcar = P.sb([4, 2], F32, "gcar")
            tails = P.sb([128, 12, 3], F32, "tails")
            cbuf = Ring([P.sb([128, 515], F32, "cbuf") for _ in range(3)])
            sti = st_src
            srd = [st_src_tile] if st_src_tile is not None else []
            P.dma("sp", Cst[:].rearrange("p h j -> p (h j)"), sti[:, 0:516], reads=srd, writes=[Cst])
            P.dma("sp", lru_h[:], sti[:, 516:520], reads=srd, writes=[lru_h])
            P.dma("sp", tails[:].rearrange("p c j -> p (c j)"), sti[:, 520:556], reads=srd, writes=[tails])
            P.op("dve", lambda: nc.vector.memset(gcar[:], 0.0), writes=[gcar])
            P.dma("sp", gcar[0:4, 1:2], sti[0:4, 556:557], reads=[gcar] + srd, writes=[gcar], slow=True)
            if use_flag:
                fo = lay["flag"][0]
                ts(Cst, Cst[:].rearrange("p h j -> p (h j)"), Cst[:].rearrange("p h j -> p (h j)"), vec[:, fo:fo + 1], None, ALU.mult, None, [Cst, vec])
                ts(lru_h, lru_h[:], lru_h[:], vec[:, fo:fo + 1], None, ALU.mult, None, [lru_h, vec])
                ts(tails, tails[:].rearrange("p c j -> p (c j)"), tails[:].rearrange("p c j -> p (c j)"), vec[:, fo:fo + 1], None, ALU.mult, None, [tails, vec])
                ts(gcar, gcar[0:4, 1:2], gcar[0:4, 1:2], vec[0:4, fo:fo + 1], None, ALU.mult, None, [gcar, vec])

            xring = Ring([P.sb([128, 8, 512], F32, "xt") for _ in range(1)])
            hT = [P.sb([128, 512], BF16, "hT") for _ in range(8)]
            tmp_ring = Ring([P.sb([128, 512], F32, "tmp") for _ in range(3)])
            sq_ring = tmp_ring
            rstd = P.sb([128, 512], F32, "rstd")
            qk_bf = [P.sb([128, 512], BF16, "qkbf") for _ in range(8)]
            acc_ring = Ring([P.sb([128, 512], F32, "acc") for _ in range(2)])
            vaug = [P.sb([128, 4, 129], BF16, "vaug") for _ in range(4)]
            for tb in range(4):
                P.op("dve", lambda: nc.vector.memset(vaug[tb][:], 1.0), writes=[vaug[tb]])
            go = [P.sb([128, 512], F32, "go") for _ in range(4)]
            nb = P.sb([4, 512], F32, "nb")
            wv_t = P.sb([4, 512], F32, "wv")
            mu = P.sb([4, 512], F32, "mu")
            e1 = P.sb([4, 512], F32, "e1")
            fl = P.sb([4, 512], F32, "fl")
            g_e = e1
            g_l = fl
            rp = P.sb([4, 4], F32, "rp")
            a_t = P.sb([4, 4], F32, "a_t")
            adiag = P.sb([4, 16], F32, "adiag")
            abc = P.sb([128, 16], F32, "abc")
            tsc = [P.sb([128, 8], F32, "tsc") for _ in range(4)]
            ke_ring = Ring([P.sb([128, 4, 128], BF16, "ke") for _ in range(2)])
            stm_ring = Ring([P.sb([128, 4, 128], BF16, "stm") for _ in range(2)])
            den_ring = Ring([P.sb([128, 4], F32, "den") for _ in range(2)])
            hm4_ring = Ring([P.sb([128, 512], F32, "hm4") for _ in range(3)])
            junk = P.sb([128, 128], F32, "junk")
            ssq_ring = Ring([P.sb([128, 4], F32, "ssq") for _ in range(2)])
            hmn_ring = Ring([P.sb([128, 512], BF16, "hmn") for _ in range(1)])
            hmix_m = P.sb([128, 4, 512], BF16, "hmixm")
            hmix_l = [P.sb([128, 512], BF16, "hmixl") for _ in range(4)]
            xcb_ring = Ring([P.sb([128, 512], BF16, "xcb") for _ in range(2)])
            lt = [P.sb([128, 512], F32, "lt") for _ in range(6)]
            hl = [P.sb([128, 512], F32, "hl") for _ in range(4)]
            rstl = P.sb([128, 512], F32, "rstl")

            for s in range(NSUB):
                t0 = s * 512
                xt = xring.next()
                P.dma("sp", xt[:], src_v[:, :, t0:t0 + 512], reads=([src_blocks[s]] if src_blocks is not None else []), writes=[xt])
                rms_rstd(xt, lambda kc: xt[:, kc, :], 8, onesD, sq_ring, rstd)
                for kc in range(8):
                    tmp = tmp_ring.next()
                    tt("dve", tmp, tmp[:], xt[:, kc, :], rstd[:], ALU.mult, [xt, rstd])
                    act(hT[kc], hT[kc][:], tmp[:], AF.Identity, [tmp, derived], scale=Dv(l, 0 + kc), bias=Dv(l, 8 + kc))

                def proj_fm(ps, M, col0):
                    for kc in range(8):
                        mm(ps, ps[0:M, :], wcol(kc, col0, col0 + M), hT[kc][:], [win[kc // 2], hT[kc]],
                           start=(kc == 0), stop=(kc == 7))

                def conv(ps, ci, wname, bname, c, acc):
                    buf = cbuf.next()
                    act(buf, buf[:, 3:515], ps[:], AF.Copy, [ps])
                    P.op("dve", lambda: nc.vector.tensor_copy(out=buf[:, 0:3], in_=tails[:, ci, :]), reads=[tails, buf], writes=[buf])
                    wo = lay[wname][0] + 4 * c
                    bo = lay[bname][0] + c
                    ts(acc, acc[:], buf[:, 3:515], vec[:, wo + 3:wo + 4], vec[:, bo:bo + 1], ALU.mult, ALU.add, [buf, vec])
                    for j in (2, 1, 0):
                        stt(acc, acc[:], buf[:, j:j + 512], vec[:, wo + j:wo + j + 1], acc[:], ALU.mult, ALU.add, [buf, vec, acc])
                    P.op("dve", lambda: nc.vector.tensor_copy(out=tails[:, ci, :], in_=buf[:, 512:515]), reads=[buf, tails], writes=[tails])

                def sec_qk():
                    for c in range(8):
                        if so and c < 4:
                            if s == NSUB - 1:
                                ps = psf.tiles[c % 2]
                                proj_fm(ps, 128, C_Q + c * 128)
                                P.op("dve", lambda: nc.vector.tensor_copy(out=tails[:, c, :], in_=ps[:, 509:512]), reads=[ps, tails], writes=[tails])
                            continue
                        ps = psf.tiles[c % 2]
                        proj_fm(ps, 128, C_Q + c * 128)
                        acc = acc_ring.next()
                        conv(ps, c, f"wcqk{l}", f"bcqk{l}", c, acc)
                        act(qk_bf[c], qk_bf[c][:], acc[:], AF.Silu, [acc])
                        yield


                    yield
                def sec_gates():
                    psi = psf.tiles[2]
                    proj_fm(psi, 4, C_IG)
                    yield
                    psg = psf.tiles[3]
                    proj_fm(psg, 4, C_FG)
                    yield
                    act(g_e, g_e[:], psg[0:4, :], AF.Exp, [psg, derived], scale=-1.0, bias=derived[0:4, l * 64 + 56:l * 64 + 57])
                    yield
                    act(g_l, g_l[:], g_e[:], AF.Ln, [g_e], bias=1.0)
                    yield
                    P.op("dve", lambda: nc.vector.tensor_tensor_scan(out=nb[:], data0=ones1[0:4, :], data1=g_l[:], initial=gcar[0:4, 0:1],
                                                                     op0=ALU.mult, op1=ALU.add), reads=[ones1, g_l, gcar], writes=[nb])
                    bgo = lay[f"bg{l}"][0]
                    stt(wv_t, wv_t[:], psi[0:4, :], vec[0:4, bgo:bgo + 1], nb[:], ALU.add, ALU.add, [psi, vec, nb])
                    yield
                    P.op("dve", lambda: nc.vector.tensor_tensor_scan(out=mu[:], data0=wv_t[:], data1=wv_t[:], initial=gcar[0:4, 1:2],
                                                                     op0=ALU.max, op1=ALU.max), reads=[wv_t, gcar], writes=[mu])
                    mu_v = mu[:].rearrange("p (c t) -> p c t", t=128)
                    yield
                    Rb = mu_v[:, :, 127:128].to_broadcast([4, 4, 128])
                    yield
                    P.op("dve", lambda: nc.vector.tensor_copy(out=rp[:, 0:1], in_=gcar[0:4, 1:2]), reads=[gcar], writes=[rp])
                    yield
                    P.op("dve", lambda: nc.vector.tensor_copy(out=rp[:, 1:4].unsqueeze(2), in_=mu_v[:, 0:3, 127:128]), reads=[mu, rp], writes=[rp])
                    yield
                    tt("dve", a_t, a_t[:].unsqueeze(2), rp[:].unsqueeze(2), mu_v[:, :, 127:128], ALU.subtract, [rp, mu])
                    yield
                    act(a_t, a_t[:], a_t[:], AF.Exp, [a_t])
                    yield
                    tt("dve", adiag, adiag[:].rearrange("p (c h) -> p c h", h=4), a_t[:].unsqueeze(2).to_broadcast([4, 4, 4]),
                       consts[0:4, 0:4].unsqueeze(1).to_broadcast([4, 4, 4]), ALU.mult, [a_t, consts])
                    pab = psf.tiles[2]
                    mm(pab, pab[:, 0:16], ones1[0:4, 0:128], adiag[:], [ones1, adiag])
                    yield
                    P.op("dve", lambda: nc.vector.tensor_copy(out=abc[:], in_=pab[:, 0:16]), reads=[pab], writes=[abc])
                    yield
                    tt("dve", e1, e1[:].rearrange("p (c t) -> p c t", t=128), wv_t[:].rearrange("p (c t) -> p c t", t=128), Rb, ALU.subtract, [wv_t, mu])
                    yield
                    ts(e1, e1[:], e1[:], -0.5 * math.log(128.0), None, ALU.add, None, [e1])
                    yield
                    act(e1, e1[:], e1[:], AF.Exp, [e1])
                    yield
                    tt("dve", fl, fl[:].rearrange("p (c t) -> p c t", t=128), nb[:].rearrange("p (c t) -> p c t", t=128), Rb, ALU.subtract, [nb, mu])
                    yield
                    act(fl, fl[:], fl[:], AF.Exp, [fl])
                    yield
                    P.op("dve", lambda: nc.vector.tensor_copy(out=gcar[0:4, 0:1], in_=nb[:, 511:512]), reads=[nb, gcar], writes=[gcar])
                    yield
                    P.op("dve", lambda: nc.vector.tensor_copy(out=gcar[0:4, 1:2], in_=mu[:, 511:512]), reads=[mu, gcar], writes=[gcar])
                    yield
                    for tb in range(4):
                        pt = psf.tiles[3]
                        P.op("pe", lambda: nc.tensor.transpose(out=pt[:, 0:4], in_=e1[0:4, tb * 128:(tb + 1) * 128], identity=consts[0:4, 0:4]),
                             reads=[e1, consts], writes=[pt])
                        P.op("pe", lambda: nc.tensor.transpose(out=pt[:, 4:8], in_=fl[0:4, tb * 128:(tb + 1) * 128], identity=consts[0:4, 0:4]),
                             reads=[fl, consts], writes=[pt])
                        P.op("dve", lambda: nc.vector.tensor_copy(out=tsc[tb][:], in_=pt[:, 0:8]), reads=[pt], writes=[tsc[tb]])


                    yield
                def sec_vo():
                    for tb in range(4):
                        blk = slice(tb * 128, (tb + 1) * 128)
                        psv = psf.tiles[4]
                        for kc in range(8):
                            mm(psv, psv[:], hT[kc][:, blk], wcol(kc, C_V, C_V + 512), [hT[kc], win[kc // 2]], start=(kc == 0), stop=(kc == 7))
                        act(vaug[tb], vaug[tb][:, :, 0:128], psv[:].rearrange("p (h d) -> p h d", d=128), AF.Copy, [psv])
                        if so:
                            yield
                            continue
                        pso = psf.tiles[5]
                        for kc in range(8):
                            mm(pso, pso[:], hT[kc][:, blk], wcol(kc, C_O, C_O + 512), [hT[kc], win[kc // 2]], start=(kc == 0), stop=(kc == 7))
                        act(go[tb], go[tb][:], pso[:], AF.Sigmoid, [pso])
                        tt("pool", go[tb], go[tb][:], go[tb][:], gmb[:], ALU.mult, [go[tb], gmb])
                        yield


                    yield
                gens = [sec_qk(), sec_gates(), sec_vo()]
                while gens:
                    for g_ in list(gens):
                        try:
                            next(g_)
                        except StopIteration:
                            gens.remove(g_)

                def lru_chunk(c):
                    ps = psf.tiles[3]
                    proj_fm(ps, 128, C_XL + c * 128)
                    yield
                    xc, r_t, a_, a2, i_t, hs = lt
                    gel = r_t
                    conv(ps, 8 + c, f"wcl{l}", f"bcl{l}", c, xc)
                    yield
                    xcb = xcb_ring.next()
                    yield
                    act(xcb, xcb[:], xc[:], AF.Copy, [xc])
                    yield
                    pa = psf.tiles[4]
                    yield
                    mm(pa, pa[:], bd_a[:, c, :], xcb[:], [bd_a, xcb])
                    yield
                    px = psf.tiles[5]
                    yield
                    mm(px, px[:], bd_x[:, c, :], xcb[:], [bd_x, xcb])
                    yield
                    act(r_t, r_t[:], pa[:], AF.Sigmoid, [pa, vec], bias=V(f"bla{l}", c))
                    yield
                    act(a_, a_[:], r_t[:], AF.Exp, [r_t, derived], scale=Dv(l, 48 + c))
                    yield
                    act(a2, a2[:], r_t[:], AF.Exp, [r_t, derived], scale=Dv(l, 52 + c))
                    yield
                    ts(a2, a2[:], a2[:], -1.0, 1.0, ALU.mult, ALU.add, [a2])
                    yield
                    ts(a2, a2[:], a2[:], 1e-30, None, ALU.max, None, [a2])
                    yield
                    act(a2, a2[:], a2[:], AF.Sqrt, [a2])
                    yield
                    act(i_t, i_t[:], px[:], AF.Sigmoid, [px, vec], bias=V(f"blx{l}", c))
                    yield
                    tt("pool", i_t, i_t[:], i_t[:], xc[:], ALU.mult, [i_t, xc])
                    yield
                    tt("pool", i_t, i_t[:], i_t[:], a2[:], ALU.mult, [i_t, a2])
                    yield
                    P.op("dve", lambda: nc.vector.tensor_tensor_scan(out=hs[:], data0=a_[:], data1=i_t[:], initial=lru_h[:, c:c + 1],
                                                                     op0=ALU.mult, op1=ALU.add), reads=[a_, i_t, lru_h], writes=[hs])
                    P.op("dve", lambda: nc.vector.tensor_copy(out=lru_h[:, c:c + 1], in_=hs[:, 511:512]), reads=[hs, lru_h], writes=[lru_h])
                    yield
                    if so:
                        return
                    psl = psf.tiles[3]
                    yield
                    proj_fm(psl, 128, C_GL + c * 128)
                    yield
                    act(gel, gel[:], psl[:], AF.Gelu_apprx_tanh, [psl])
                    yield
                    tt("pool", hl[c], hl[c][:], hs[:], gel[:], ALU.mult, [hs, gel])
                    yield

                def mlstm_tb(tb):
                    blk = slice(tb * 128, (tb + 1) * 128)
                    yield
                    e1b = tsc[tb][:, 0:4].unsqueeze(2).to_broadcast([128, 4, 128])
                    yield
                    pk = psb.next()
                    yield
                    for h in range(4):
                        P.op("pe", lambda: nc.tensor.transpose(out=pk[:, h * 128:(h + 1) * 128], in_=qk_bf[4 + h][:, blk], identity=ident_b[:]),
                             reads=[qk_bf[4 + h], ident_b], writes=[pk])
                    ke4 = ke_ring.next()
                    yield
                    tt("dve", ke4, ke4[:], pk[:, 0:512].rearrange("p (h d) -> p h d", d=128), e1b, ALU.mult, [pk, tsc[tb]])
                    yield
                    if not so:
                        pS = psf.tiles[0]
                        yield
                        for h in range(4):
                            mm(pS, pS[:, h * 128:(h + 1) * 128], qk_bf[4 + h][:, blk], qk_bf[h][:, blk], [qk_bf[4 + h], qk_bf[h]])
                        stmp = hm4_ring.next()
                        yield
                        tt("dve", stmp, stmp[:].rearrange("p (h d) -> p h d", d=128), pS[:].rearrange("p (h d) -> p h d", d=128), e1b, ALU.mult, [pS, tsc[tb]])
                        yield
                        stm4 = stm_ring.next()
                        yield
                        tt("dve", stm4, stm4[:], stmp[:].rearrange("p (h d) -> p h d", d=128),
                           mask_f.unsqueeze(1).to_broadcast([128, 4, 128]), ALU.mult, [stmp, consts])
                    tt("dve", Cst, Cst[:], Cst[:], abc[:, tb * 4:(tb + 1) * 4].unsqueeze(2).to_broadcast([128, 4, 129]), ALU.mult, [Cst, abc])
                    yield
                    if not so:
                        act(Cbf, Cbf[:], Cst[:], AF.Copy, [Cst])
                        yield
                        pOn = psf.tiles[1]
                        yield
                        pOd = psf.tiles[2]
                        yield
                        for h in range(4):
                            mm(pOn, pOn[:, h * 128:(h + 1) * 128], stm4[:, h, :], vaug[tb][:, h, 0:128], [stm4, vaug[tb]], start=True, stop=False)
                            mm(pOn, pOn[:, h * 128:(h + 1) * 128], qk_bf[h][:, blk], Cbf[:, h, 0:128], [qk_bf[h], Cbf], start=False, stop=True)
                        for h in range(4):
                            mm(pOd, pOd[:, 384 + h:385 + h], stm4[:, h, :], vaug[tb][:, h, 128:129], [stm4, vaug[tb]], start=True, stop=False)
                            mm(pOd, pOd[:, 384 + h:385 + h], qk_bf[h][:, blk], Cbf[:, h, 128:129], [qk_bf[h], Cbf], start=False, stop=True)
                    pU1 = psf.tiles[0]
                    yield
                    pU2 = psf.tiles[2]
                    yield
                    for h in range(4):
                        pu = pU1 if h < 2 else pU2
                        o_ = (h % 2) * 129
                        mm(pu, pu[:, o_:o_ + 129], ke4[:, h, :], vaug[tb][:, h, :], [ke4, vaug[tb]])
                    tt("dve", Cst, Cst[:, 0:2, :], Cst[:, 0:2, :], pU1[:, 0:258].rearrange("p (h j) -> p h j", j=129), ALU.add, [Cst, pU1])
                    yield
                    tt("dve", Cst, Cst[:, 2:4, :], Cst[:, 2:4, :], pU2[:, 0:258].rearrange("p (h j) -> p h j", j=129), ALU.add, [Cst, pU2])
                    yield
                    if so:
                        return
                    den = den_ring.next()
                    yield
                    act(den, den[:], pOd[:, 384:388], AF.Abs, [pOd])
                    yield
                    tt("dve", den, den[:], den[:], tsc[tb][:, 4:8], ALU.max, [den, tsc[tb]])
                    yield
                    P.op("dve", lambda: nc.vector.reciprocal(out=den[:], in_=den[:]), reads=[den], writes=[den])
                    yield
                    hm4 = hm4_ring.next()
                    yield
                    tt("dve", hm4, hm4[:].rearrange("p (h d) -> p h d", d=128), pOn[:].rearrange("p (h d) -> p h d", d=128),
                       den[:].unsqueeze(2).to_broadcast([128, 4, 128]), ALU.mult, [pOn, den])
                    sq4 = hm4_ring.next()
                    yield
                    act(sq4, sq4[:], hm4[:], AF.Square, [hm4])
                    yield
                    ssq = ssq_ring.next()
                    yield
                    P.op("dve", lambda: nc.vector.tensor_reduce(out=ssq[:], in_=sq4[:].rearrange("p (h d) -> p h d", d=128), axis=AX.X, op=ALU.add),
                         reads=[sq4], writes=[ssq])
                    ts(ssq, ssq[:], ssq[:], 1.0 / 128, EPS, ALU.mult, ALU.add, [ssq])
                    yield
                    act(ssq, ssq[:], ssq[:], AF.Sqrt, [ssq])
                    yield
                    P.op("dve", lambda: nc.vector.reciprocal(out=ssq[:], in_=ssq[:]), reads=[ssq], writes=[ssq])
                    yield
                    tt("dve", hm4, hm4[:].rearrange("p (h d) -> p h d", d=128), hm4[:].rearrange("p (h d) -> p h d", d=128),
                       ssq[:].unsqueeze(2).to_broadcast([128, 4, 128]), ALU.mult, [hm4, ssq])
                    hmn = hmn_ring.next()
                    yield
                    tt("pool", hmn, hmn[:], hm4[:], go[tb][:], ALU.mult, [hm4, go[tb]])
                    yield
                    ph = psb.next()
                    yield
                    for h in range(4):
                        P.op("pe", lambda: nc.tensor.transpose(out=ph[:, h * 128:(h + 1) * 128], in_=hmn[:, h * 128:(h + 1) * 128], identity=ident_b[:]),
                             reads=[hmn, ident_b], writes=[ph])
                    act(hmix_m, hmix_m[:, :, blk], ph[:, 0:512].rearrange("p (h d) -> p h d", d=128), AF.Copy, [ph])
                    yield

                for tb in range(4):
                    gens = [mlstm_tb(tb), lru_chunk(tb)]
                    while gens:
                        for g_ in list(gens):
                            try:
                                next(g_)
                            except StopIteration:
                                gens.remove(g_)
                if so:
                    continue
                rms_rstd(None, lambda c: hl[c][:], 4, onesL, sq_ring, rstl, reads_extra=hl)
                for c in range(4):
                    stt(hmix_l[c], hmix_l[c][:], hl[c][:], V(f"gml{l}", c), rstl[:], ALU.mult, ALU.mult, [hl[c], vec, rstl])

                for dc in range(8):
                    ps = psf.next()
                    for kc in range(8):
                        hx_t = hmix_m if kc < 4 else hmix_l[kc - 4]
                        hx_ap = hmix_m[:, kc, :] if kc < 4 else hmix_l[kc - 4][:]
                        mm(ps, ps[:], wout[kc // 4][:, kc % 4, dc * 128:(dc + 1) * 128], hx_ap, [wout[kc // 4], hx_t],
                           start=(kc == 0), stop=(kc == 7))
                    stt(xt, xt[:, dc, :], ps[:], Dv(l, 16 + dc), xt[:, dc, :], ALU.mult, ALU.add, [ps, derived, xt])
                P.dma("sp", dst_v[:, :, t0:t0 + 512], xt[:], reads=[xt], writes=[dst_blocks[s]])

            sto = st_dst
            swr = [st_dst_tile] if st_dst_tile is not None else []
            P.dma("sp", sto[:, 0:516], Cst[:].rearrange("p h j -> p (h j)"), reads=[Cst], writes=swr)
            P.dma("sp", sto[:, 516:520], lru_h[:], reads=[lru_h], writes=swr)
            P.dma("sp", sto[:, 520:556], tails[:].rearrange("p c j -> p (c j)"), reads=[tails], writes=swr)
            mend = P.sb([128, 4], F32, "mend")
            P.op("dve", lambda: nc.vector.memset(mend[:], 0.0), writes=[mend])
            tt("dve", mend, mend[0:4, 0:1], gcar[0:4, 1:2], gcar[0:4, 0:1], ALU.subtract, [gcar, mend])
            P.dma("sp", sto[:, 556:560], mend[:], reads=[mend], writes=swr)
            P.barrier()
        P.tes = es

    def ffn_phase(l, moe, final, buf_v, buf_blocks):
        with contextlib.ExitStack() as es2:
            P.tes = es2
            xF = P.sb([128, 8, TT], F32, "xF")
            hF = [P.sb([128, TT], BF16, "hF") for _ in range(8)]
            tmp_ring = Ring([P.sb([128, 512], F32, "tmp") for _ in range(3)])
            sq_ring = tmp_ring
            rstd = P.sb([128, 512], F32, "rstd")
            wg_ring = Ring([P.sb([128, 8, JGW], BF16, "wg") for _ in range(3)])
            wu_ring = Ring([P.sb([128, 8, JGW], BF16, "wu") for _ in range(3)])
            wd_ring = Ring([P.sb([128, 2, 1024], BF16, "wd") for _ in range(3)])
            sg_ring = Ring([P.sb([128, 512], F32, "sg") for _ in range(3)])
            act_ring = Ring([P.sb([128, 512], BF16, "actb") for _ in range(4 * NB)])
            if moe:
                hf32 = [P.sb([128, 512], F32, "hf32") for _ in range(8)]
                wr = P.sb([128, 8, 8], F32, "wr")
                P.dma("sp", wr[:], w_r_d.rearrange("(kc p) e -> p kc e", p=128), writes=[wr])
                brb = P.sb([128, 8], F32, "brb")
                P.dma("sp", brb[:], brb_d, writes=[brb])
                gTs = P.sb([8, TT], F32, "gTs")
                gb = [P.sb([128, TT], F32, "gb") for _ in range(NE)]
                lg = P.sb([128, 8], F32, "lg")
                lg2 = P.sb([128, 8], F32, "lg2")
                m1 = P.sb([128, 1], F32, "m1")
                m2 = P.sb([128, 1], F32, "m2")
                selt = P.sb([128, 8], F32, "selt")
                pe_ = P.sb([128, 8], F32, "pe_")
                dsum = P.sb([128, 1], F32, "dsum")
                sg2_ring = Ring([P.sb([128, 512], F32, "sg2") for _ in range(3)])
            n_exp = NE if moe else 1

            for tti in range(NTT):
                T0 = tti * TT
                blocks = [buf_blocks[(T0 // 512) + n] for n in range(NB)]
                P.dma("sp", xF[:], buf_v[:, :, T0:T0 + TT], reads=blocks, writes=[xF])
                for n in range(NB):
                    nb_ = slice(n * 512, (n + 1) * 512)
                    rms_rstd(xF, lambda kc: xF[:, kc, nb_], 8, onesD, sq_ring, rstd)
                    for kc in range(8):
                        tmp = tmp_ring.next()
                        tt("dve", tmp, tmp[:], xF[:, kc, nb_], rstd[:], ALU.mult, [xF, rstd])
                        if moe:
                            act(hf32[kc], hf32[kc][:], tmp[:], AF.Identity, [tmp, derived], scale=Dv(l, 24 + kc), bias=Dv(l, 32 + kc))
                            P.op("pool", lambda: nc.gpsimd.tensor_copy(out=hF[kc][:, nb_], in_=hf32[kc][:]), reads=[hf32[kc]], writes=[hF[kc]])
                        else:
                            act(hF[kc], hF[kc][:, nb_], tmp[:], AF.Identity, [tmp, derived], scale=Dv(l, 24 + kc), bias=Dv(l, 32 + kc))
                    if moe:
                        for tb in range(4):
                            tblk = slice(tb * 128, (tb + 1) * 128)
                            pr = psf.next()
                            for kc in range(8):
                                mm(pr, pr[:, 0:8], hf32[kc][:, tblk], wr[:, kc, :], [hf32[kc], wr], start=(kc == 0), stop=(kc == 7))
                            tt("dve", lg, lg[:], pr[:, 0:8], brb[:], ALU.add, [pr, brb])
                            P.op("dve", lambda: nc.vector.tensor_reduce(out=m1[:], in_=lg[:], axis=AX.X, op=ALU.max), reads=[lg], writes=[m1])
                            ts(selt, selt[:], lg[:], m1[:, 0:1], -1e30, ALU.is_ge, ALU.mult, [lg, m1])
                            tt("dve", lg2, lg2[:], lg[:], selt[:], ALU.add, [lg, selt])
                            P.op("dve", lambda: nc.vector.tensor_reduce(out=m2[:], in_=lg2[:], axis=AX.X, op=ALU.max), reads=[lg2], writes=[m2])
                            ts(selt, selt[:], lg[:], m2[:, 0:1], None, ALU.is_ge, None, [lg, m2])
                            ts(m1, m1[:], m1[:], -1.0, None, ALU.mult, None, [m1])
                            act(pe_, pe_[:], lg[:], AF.Exp, [lg, m1], bias=m1[:, 0:1], scale=1.0)
                            tt("dve", pe_, pe_[:], pe_[:], selt[:], ALU.mult, [pe_, selt])
                            P.op("dve", lambda: nc.vector.tensor_reduce(out=dsum[:], in_=pe_[:], axis=AX.X, op=ALU.add), reads=[pe_], writes=[dsum])
                            P.op("dve", lambda: nc.vector.reciprocal(out=dsum[:], in_=dsum[:]), reads=[dsum], writes=[dsum])
                            ts(pe_, pe_[:], pe_[:], dsum[:, 0:1], None, ALU.mult, None, [pe_, dsum])
                            pg = psf.next()
                            P.op("pe", lambda: nc.tensor.transpose(out=pg[0:8, 0:128], in_=pe_[:], identity=ident_f), reads=[pe_, consts], writes=[pg])
                            c0 = n * 512 + tb * 128
                            P.op("dve", lambda: nc.vector.tensor_copy(out=gTs[:, c0:c0 + 128], in_=pg[0:8, 0:128]), reads=[pg], writes=[gTs])
                        for e in range(NE):
                            pb = psf.next()
                            mm(pb, pb[:], consts[0:8, 256 + e * 128:256 + (e + 1) * 128], gTs[:, nb_], [consts, gTs])
                            act(gb[e], gb[e][:, nb_], pb[:], AF.Copy, [pb])

                stages = [(e, jg) for e in range(n_exp) for jg in range(NJG)]
                loaded = {}

                def load(k):
                    e, jg = stages[k]
                    wg = wg_ring.next()
                    wu = wu_ring.next()
                    wd = wd_ring.next()
                    if moe:
                        P.dma("pool", wg[:], exg_d[e, jg], writes=[wg])
                        P.dma("pool", wu[:], exu_d[e, jg], writes=[wu])
                        P.dma("pool", wd[:], exd_d[e, jg * JGW:(jg + 1) * JGW, :].rearrange("(c p) n -> p c n", p=128), writes=[wd])
                    else:
                        P.dma("pool", wg[:], ffg_d[jg], writes=[wg])
                        P.dma("pool", wu[:], ffu_d[jg], writes=[wu])
                        P.dma("pool", wd[:], ffd_d[jg * JGW:(jg + 1) * JGW, :].rearrange("(c p) n -> p c n", p=128), writes=[wd])
                    loaded[k] = (wg, wu, wd)

                load(0)
                if len(stages) > 1:
                    load(1)
                for k in range(len(stages)):
                    if k + 2 < len(stages):
                        load(k + 2)
                    e, jg = stages[k]
                    wg, wu, wd = loaded.pop(k)
                    acts = {}
                    for jj in range(2):
                        for n in range(NB):
                            nb_ = slice(n * 512, (n + 1) * 512)
                            pgp = psf.next()
                            for kc in range(8):
                                mm(pgp, pgp[:], wg[:, kc, jj * 128:(jj + 1) * 128], hF[kc][:, nb_], [wg, hF[kc]], start=(kc == 0), stop=(kc == 7))
                            pup = psf.next()
                            for kc in range(8):
                                mm(pup, pup[:], wu[:, kc, jj * 128:(jj + 1) * 128], hF[kc][:, nb_], [wu, hF[kc]], start=(kc == 0), stop=(kc == 7))
                            sg = sg_ring.next()
                            act(sg, sg[:], pgp[:], AF.Silu, [pgp])
                            ab = act_ring.next()
                            if moe:
                                sg2 = sg2_ring.next()
                                tt("dve", sg2, sg2[:], sg[:], pup[:], ALU.mult, [sg, pup])
                                tt("pool", ab, ab[:], sg2[:], gb[e][:, nb_], ALU.mult, [sg2, gb[e]])
                            else:
                                tt("dve", ab, ab[:], sg[:], pup[:], ALU.mult, [sg, pup])
                            acts[(jj, n)] = ab
                    for n in range(NB):
                        nb_ = slice(n * 512, (n + 1) * 512)
                        for dc in range(8):
                            py = psf.next()
                            for jj in range(2):
                                mm(py, py[:], wd[:, jj, dc * 128:(dc + 1) * 128], acts[(jj, n)][:], [wd, acts[(jj, n)]], start=(jj == 0), stop=(jj == 1))
                            stt(xF, xF[:, dc, nb_], py[:], Dv(l, 40 + dc), xF[:, dc, nb_], ALU.mult, ALU.add, [py, derived, xF])

                if final:
                    for n in range(NB):
                        nb_ = slice(n * 512, (n + 1) * 512)
                        rms_rstd(xF, lambda kc: xF[:, kc, nb_], 8, onesD, sq_ring, rstd)
                        for kc in range(8):
                            stt(xF, xF[:, kc, nb_], xF[:, kc, nb_], V("gfin", kc), rstd[:], ALU.mult, ALU.mult, [xF, vec, rstd])
                P.dma("sp", buf_v[:, :, T0:T0 + TT], xF[:], reads=[xF], writes=blocks)
            P.barrier()
        P.tes = es

    if prefix:
        for l in range(NL):
            mixer_phase(l, xpT_v if l == 0 else preT_v, None if l == 0 else dpre, preT_v, dpre,
                        st_in_d[l], None, st_pre_d[l], dstp[l], False, so=(l == NL - 1))
            if l < NL - 1:
                ffn_phase(l, moe=(l in moe_layers), final=False, buf_v=preT_v, buf_blocks=dpre)
    for l in range(NL):
        if not _SKIP_MIX:
            if prefix:
                mixer_phase(l, xT_v if l == 0 else outT_v, None if l == 0 else dx, outT_v, dx,
                            st_pre_d[l], dstp[l], st_out_d[l], None, True)
            else:
                mixer_phase(l, xT_v if l == 0 else outT_v, None if l == 0 else dx, outT_v, dx,
                            st_in_d[l], None, st_out_d[l], None, False)
        if not _SKIP_FFN:
            ffn_phase(l, moe=(l in moe_layers), final=(l == NL - 1), buf_v=outT_v, buf_blocks=dx)
    P.barrier()
    es.close()
    return nc


_NC_CACHE = {}


def _prep_shared(inp, NL):
    sh = {}
    sh["w_ada"] = np.ascontiguousarray(inp["w_ada"], dtype=np.float32)
    sh["w_in"] = np.ascontiguousarray(inp["w_in"], dtype=np.float32)
    sh["w_out"] = np.ascontiguousarray(inp["w_out"], dtype=np.float32)
    sh["w_lru_a"] = np.ascontiguousarray(inp["w_lru_a"], dtype=np.float32)
    sh["w_lru_x"] = np.ascontiguousarray(inp["w_lru_x"], dtype=np.float32)

    def grp(w):
        return np.ascontiguousarray(w.reshape(8, 128, NJG, JGW).transpose(2, 1, 0, 3))

    sh["ffg"] = grp(np.asarray(inp["w_ff_gate"][0], np.float32))
    sh["ffu"] = grp(np.asarray(inp["w_ff_up"][0], np.float32))
    sh["ffd"] = np.ascontiguousarray(inp["w_ff_down"][0], dtype=np.float32)
    sh["w_router"] = np.ascontiguousarray(inp["w_router"][0], dtype=np.float32)
    sh["brb"] = np.ascontiguousarray(np.broadcast_to(np.asarray(inp["b_router"][0], np.float32)[None, :], (128, 8)))
    sh["exg"] = np.stack([grp(np.asarray(inp["w_exp_gate"][0, e], np.float32)) for e in range(NE)])
    sh["exu"] = np.stack([grp(np.asarray(inp["w_exp_up"][0, e], np.float32)) for e in range(NE)])
    sh["exd"] = np.ascontiguousarray(inp["w_exp_down"][0], dtype=np.float32)
    sh["gmixb"] = np.ascontiguousarray(np.broadcast_to(np.asarray(inp["g_mix_out"], np.float32)[:, None, :512], (NL, 128, 512)))
    consts = np.zeros((128, 256 + 1024), np.float32)
    consts[:, 0:128] = np.eye(128, dtype=np.float32)
    consts[:, 128:256] = np.triu(np.ones((128, 128), np.float32))
    for e in range(NE):
        consts[e, 256 + e * 128:256 + (e + 1) * 128] = 1.0
    sh["consts"] = consts
    return sh


def _vecs(inp, b, NL, flag=0.0):
    lay, NV = vec_layout(NL)
    v = np.zeros((128, NV), np.float32)

    def put(name, arr):
        o, n = lay[name]
        assert arr.shape == (128, n), (name, arr.shape, n)
        v[:, o:o + n] = arr

    put("cond", fm(inp["c"][b], 8))
    for l in range(NL):
        put(f"bada{l}", fm(inp["b_ada"][l], 48))
        put(f"gm{l}", fm(inp["g_norm_mix"][l], 8))
        put(f"gf{l}", fm(inp["g_norm_ffn"][l], 8))
        w = np.asarray(inp["w_conv_qk"][l], np.float32)
        put(f"wcqk{l}", np.ascontiguousarray(w.T.reshape(8, 128, 4).transpose(1, 0, 2).reshape(128, 32)))
        put(f"bcqk{l}", fm(inp["b_conv_qk"][l], 8))
        w = np.asarray(inp["w_conv_lru"][l], np.float32)
        put(f"wcl{l}", np.ascontiguousarray(w.T.reshape(4, 128, 4).transpose(1, 0, 2).reshape(128, 16)))
        put(f"bcl{l}", fm(inp["b_conv_lru"][l], 4))
        put(f"bla{l}", fm(inp["b_lru_a"][l], 4))
        put(f"blx{l}", fm(inp["b_lru_x"][l], 4))
        put(f"lam{l}", fm(inp["lru_lambda"][l], 4))
        put(f"gml{l}", fm(inp["g_mix_out"][l][512:], 4))
        bg = np.zeros((128, 2), np.float32)
        bg[0:4, 0] = inp["b_gates"][l][0:4]
        bg[0:4, 1] = inp["b_gates"][l][4:8]
        put(f"bg{l}", bg)
    put("gfin", fm(inp["g_final"], 8))
    put("flag", np.full((128, 1), flag, np.float32))
    return v


def kernel(**inputs):
    x = np.asarray(inputs["x"], np.float32)
    B, S, D = x.shape
    NL = int(np.asarray(inputs["w_in"]).shape[0])
    n_cores = 2 * B
    T = S // 2
    key = (T, NL)
    if key not in _NC_CACHE:
        _NC_CACHE[key] = build(T, NL)
    nc = _NC_CACHE[key]
    sh = _prep_shared(inputs, NL)
    zeros_st = np.zeros((NL, 128, SW), np.float32)
    zeros_x = np.zeros((D, T), np.float32)
    in_maps = []
    for cid in range(n_cores):
        b, half = cid // 2, cid % 2
        m = dict(sh)
        m["xT"] = np.ascontiguousarray(x[b, half * T:(half + 1) * T, :].T)
        m["xpT"] = np.ascontiguousarray(x[b, 0:T, :].T) if half == 1 else zeros_x
        m["vecs"] = _vecs(inputs, b, NL, flag=float(half))
        m["st_in"] = zeros_st
        in_maps.append(m)
    res = run_bass_kernel_spmd(nc, in_maps, core_ids=list(range(n_cores)))
    out = np.empty((B, S, D), np.float32)
    for cid in range(n_cores):
        b, half = cid // 2, cid % 2
        out[b, half * T:(half + 1) * T, :] = res.results[cid]["outT"].T
    return out
```
